# Optimizing a Trainium2 kernel written in Bass

```python
import math
import jax
import jax.numpy as jnp
from jax import lax
import numpy as np

D_MODEL = 1024
BATCH = 8
SEQ = 2048
DEPTH = 2

GRID_W = 64
CTX_LEN = 256
N_EVEN = (DEPTH + 1) // 2
N_ODD = DEPTH // 2
EPS = 1e-6
NEG_INF = -1e30
BLOCK = 128
ROPE_BASE = 10000.0
HY_CH = D_MODEL // 2
HY_ORDER = 2
HY_EMB = 33
HY_FILT_HID = 64
HY_MAX_DECAY = math.log(1e-2) / 0.3
HY_MIN_DECAY = math.log(1e-2) / 1.5
DA_HEADS = 4
DA_HD = D_MODEL // 16
RET_HEADS = 4
RET_DK = D_MODEL // 8
RET_DV = D_MODEL // 8
RET_CHUNK = 128
GQ_KV = 2
GQ_GROUP = 4
GQ_HD = D_MODEL // 16
WINDOW = 128
N_GROUPS = 4
EXP_PER_GROUP = 8
TOP_K = 2
D_EXPERT = D_MODEL // 4

E_SPLIT = (3 * HY_CH, DA_HEADS * 2 * DA_HD, DA_HEADS * 2 * DA_HD, DA_HEADS * 2 * DA_HD)
E_IN = sum(E_SPLIT)
E_MIX = HY_CH + DA_HEADS * 2 * DA_HD
O_SPLIT = (RET_HEADS * RET_DK, RET_HEADS * RET_DK, RET_HEADS * RET_DV, RET_HEADS * RET_DV,
           GQ_KV * GQ_GROUP * GQ_HD, GQ_KV * GQ_HD, GQ_KV * GQ_HD)
O_IN = sum(O_SPLIT)
O_MIX = RET_HEADS * RET_DV + GQ_KV * GQ_GROUP * GQ_HD

kernel_name = "hybrid_hyena_diffattn_retention_swa_hmoe_dit"

f32 = jnp.float32


def split_cols(t, sizes):
    return jnp.split(t, [int(i) for i in np.cumsum(sizes)[:-1]], axis=-1)


def rms_norm(x):
    xf = x.astype(f32)
    return (xf * lax.rsqrt(jnp.mean(xf * xf, axis=-1, keepdims=True) + EPS)).astype(x.dtype)


def modulate(x, shift, scale):
    return x * (1 + scale) + shift


def axial_rope(rows, head_dim):
    row = jnp.repeat(jnp.arange(rows), GRID_W).astype(f32)
    col = jnp.tile(jnp.arange(GRID_W), rows).astype(f32)
    nf = head_dim // 4
    inv = ROPE_BASE ** (-jnp.arange(nf, dtype=f32) / nf)
    ang = jnp.stack([row[:, None] * inv, col[:, None] * inv], axis=1)
    return jnp.cos(ang), jnp.sin(ang)


def apply_axial_rope(x, cos, sin):
    shp = x.shape
    d = shp[-1]
    xr = x.astype(f32).reshape(shp[:-1] + (2, 2, d // 4))
    x1, x2 = xr[..., 0, :], xr[..., 1, :]
    bshape = (cos.shape[0],) + (1,) * (x.ndim - 3) + cos.shape[1:]
    c, s = cos.reshape(bshape), sin.reshape(bshape)
    out = jnp.stack([x1 * c - x2 * s, x2 * c + x1 * s], axis=-2)
    return out.reshape(shp).astype(x.dtype)


def seq_rope(n_tok, head_dim):
    inv = 1.0 / (ROPE_BASE ** jnp.linspace(0.0, 1.0, head_dim // 2, dtype=f32))
    ang = jnp.arange(n_tok, dtype=f32)[:, None] * inv
    return jnp.cos(ang), jnp.sin(ang)


def apply_rope_1d(x, cos, sin):
    x1, x2 = jnp.split(x.astype(f32), 2, axis=-1)
    c, s = cos[:, None, :], sin[:, None, :]
    return jnp.concatenate([x1 * c - x2 * s, x2 * c + x1 * s], axis=-1).astype(x.dtype)


def depthwise_conv3(u, w, b):
    y = lax.conv_general_dilated(u, w[:, None, :].astype(u.dtype), window_strides=(1,),
                                 padding=((1, 1),), dimension_numbers=('NWC', 'WIO', 'NWC'),
                                 feature_group_count=u.shape[-1])
    return y + b


def hyena_filters(L, w1, b1, w2, b2, w3, freq):
    bands = (HY_EMB - 1) // 2
    t = jnp.linspace(0.0, 1.0, L, dtype=f32)[:, None]
    w = (2.0 * math.pi / L) * jnp.arange(L, dtype=f32)[:, None]
    fb = jnp.linspace(1e-4, bands - 1, bands, dtype=f32)[None, :]
    z = jnp.concatenate([t, jnp.cos(fb * w), -jnp.sin(fb * w)], axis=-1)
    h = jnp.sin(freq[0] * (z @ w1 + b1))
    h = jnp.sin(freq[1] * (h @ w2 + b2))
    h = (h @ w3).astype(f32).reshape(L, HY_ORDER, 2, HY_CH)
    deltas = jnp.abs(jnp.linspace(HY_MIN_DECAY, HY_MAX_DECAY, HY_CH, dtype=f32))
    h = h * jnp.exp(-t * deltas)[:, None, None, :]
    fwd, bwd = h[:, :, 0], h[:, :, 1]
    full = jnp.concatenate([fwd, jnp.zeros_like(fwd[:1]), bwd[:0:-1]], axis=0)
    return jnp.fft.rfft(full, axis=0)


def hyena_mixer(u, conv_w, conv_b, filt_f, bias):
    L = u.shape[1]
    u = depthwise_conv3(u, conv_w, conv_b)
    v, x1, x2 = jnp.split(u, 3, axis=-1)
    z = v.astype(f32)
    for n, gate in enumerate((x1, x2)):
        zf = jnp.fft.rfft(z, n=2 * L, axis=1)
        z = jnp.fft.irfft(zf * filt_f[None, :, n, :], n=2 * L, axis=1)[:, :L] + z * bias[n].astype(f32)
        z = gate.astype(f32) * z
    return z.astype(u.dtype)


def diff_attention(q_l, k_l, v_l, q_c, k_c, v_c, lam, lam_init, subln):
    B, S = q_l.shape[:2]
    nb = S // BLOCK
    keys = jnp.concatenate([k_c, k_l], axis=1)
    vals = jnp.concatenate([v_c, v_l], axis=1)

    def attend(qb, kk, vv):
        s = jnp.einsum('bqhmd,bkhmd->bhmqk', qb, kk).astype(f32)
        p = jax.nn.softmax(s, axis=-1)
        a = p[:, :, 0] - lam * p[:, :, 1]
        return jnp.einsum('bhqk,bkhe->bqhe', a.astype(vv.dtype), vv)

    qb = jnp.moveaxis(q_l.reshape((B, nb, BLOCK) + q_l.shape[2:]), 1, 0)
    o = lax.map(lambda qq: attend(qq, keys, vals), qb)
    o_l = jnp.moveaxis(o, 0, 1).reshape((B, S) + o.shape[3:])

    def post(o):
        return (rms_norm(o) * subln * (1.0 - lam_init)).reshape(o.shape[:2] + (-1,))

    o_c = post(attend(q_c, k_c, v_c)) if q_c is not None else None
    return post(o_l), o_c


def sink_softmax(sink, *scores):
    sk = jnp.broadcast_to(sink, scores[0].shape[:-1] + (1,))
    p = jax.nn.softmax(jnp.concatenate((sk,) + scores, axis=-1), axis=-1)
    sizes = [t.shape[-1] for t in scores]
    return jnp.split(p[..., 1:], [int(i) for i in np.cumsum(sizes)[:-1]], axis=-1)


def window_gqa(q_l, k_l, v_l, q_c, k_c, v_c, sink):
    B, S = q_l.shape[:2]
    nb = S // BLOCK

    def bands(t):
        tp = jnp.pad(t, ((0, 0), (BLOCK, BLOCK), (0, 0), (0, 0))).reshape(B, nb + 2, BLOCK, GQ_KV, GQ_HD)
        w = jnp.concatenate([tp[:, :-2], tp[:, 1:-1], tp[:, 2:]], axis=2)
        return jnp.moveaxis(w, 1, 0)

    kw, vw = bands(k_l), bands(v_l)
    qb = jnp.moveaxis(q_l.reshape(B, nb, BLOCK, GQ_KV, GQ_GROUP, GQ_HD), 1, 0)
    i = jnp.arange(BLOCK)[:, None]
    j = jnp.arange(3 * BLOCK)[None, :]
    kpos = (jnp.arange(nb) * BLOCK)[:, None, None] - BLOCK + j[None]
    mask = (jnp.abs(j - BLOCK - i) <= WINDOW)[None] & (kpos >= 0) & (kpos < S)
    sink_col = sink.astype(f32).reshape(GQ_KV, GQ_GROUP, 1, 1)

    def block(args):
        qq, kk, vv, mm = args
        s_ctx = jnp.einsum('bqkgd,bckd->bkgqc', qq, k_c).astype(f32)
        s_loc = jnp.where(mm, jnp.einsum('bqkgd,bjkd->bkgqj', qq, kk).astype(f32), NEG_INF)
        p_ctx, p_loc = sink_softmax(sink_col, s_ctx, s_loc)
        return (jnp.einsum('bkgqc,bckd->bqkgd', p_ctx.astype(v_c.dtype), v_c)
                + jnp.einsum('bkgqj,bjkd->bqkgd', p_loc.astype(vv.dtype), vv))

    o = lax.map(block, (qb, kw, vw, mask))
    o_l = jnp.moveaxis(o, 0, 1).reshape(B, S, -1)
    o_c = None
    if q_c is not None:
        s = jnp.einsum('bqkgd,bckd->bkgqc', q_c, k_c).astype(f32)
        (p,) = sink_softmax(sink_col, s)
        o_c = jnp.einsum('bkgqc,bckd->bqkgd', p.astype(v_c.dtype), v_c).reshape(q_c.shape[0], q_c.shape[1], -1)
    return o_l, o_c


def retention_scan(q, k, v, log_g, s0):
    B, L, H, dk = k.shape
    dv = v.shape[-1]
    n = L // RET_CHUNK
    kc = k.reshape(B, n, RET_CHUNK, H, dk)
    vc = v.reshape(B, n, RET_CHUNK, H, dv)
    idx = jnp.arange(RET_CHUNK, dtype=f32)
    zeta = jnp.exp((RET_CHUNK - 1 - idx)[:, None] * log_g[None, :])
    u = jnp.einsum('bnjhd,jh,bnjhe->bnhde', kc, zeta, vc)
    g_chunk = jnp.exp(RET_CHUNK * log_g)[None, :, None, None]

    def step(s, u_i):
        return g_chunk * s + u_i, s

    s_fin, s_prev = lax.scan(step, s0, jnp.moveaxis(u, 1, 0))
    if q is None:
        return None, s_fin
    qc = q.reshape(B, n, RET_CHUNK, H, dk)
    rel = idx[:, None] - idx[None, :]
    dmat = jnp.where(rel >= 0, jnp.exp(jnp.maximum(rel, 0.0)[None] * log_g[:, None, None]), 0.0)
    att = jnp.einsum('bnihd,bnjhd->bnhij', qc, kc) * dmat
    o = jnp.einsum('bnhij,bnjhe->bnihe', att, vc)
    xi = jnp.exp((idx + 1)[:, None] * log_g[None, :])
    o = o + jnp.einsum('bnihd,nbhde->bnihe', qc, s_prev) * xi[None, None, :, :, None]
    return o.reshape(B, L, H, dv), s_fin


def bidir_retention(q_l, k_l, v_l, q_c, k_c, v_c, decay):
    lg = jax.nn.log_sigmoid(decay.astype(f32))
    cast = lambda t: None if t is None else t.astype(f32)
    flip = lambda t: None if t is None else t[:, ::-1]
    q_l, k_l, v_l, q_c, k_c, v_c = map(cast, (q_l, k_l, v_l, q_c, k_c, v_c))
    B, _, H, dk = k_l.shape
    s0 = jnp.zeros((B, H, dk, v_l.shape[-1]), f32)
    o_cf, s_cf = retention_scan(q_c, k_c, v_c, lg[0], s0)
    o_cb, s_cb = retention_scan(flip(q_c), flip(k_c), flip(v_c), lg[1], s0)
    o_lf, _ = retention_scan(q_l, k_l, v_l, lg[0], s_cf)
    o_lb, _ = retention_scan(flip(q_l), flip(k_l), flip(v_l), lg[1], s_cb)
    o_c = o_cf + flip(o_cb) if q_c is not None else None
    return o_lf + flip(o_lb), o_c


def retention_out(o, g, gn_w):
    B, L, H, dv = o.shape
    mu = jnp.mean(o, axis=-1, keepdims=True)
    var = jnp.mean(jnp.square(o - mu), axis=-1, keepdims=True)
    o = ((o - mu) * lax.rsqrt(var + EPS)).reshape(B, L, H * dv)
    return (o * gn_w.astype(f32) * jax.nn.silu(g.astype(f32))).astype(g.dtype)


def even_mixer(ul, uc, need_ctx, lam_init, w_in, w_out, conv_w, conv_b, f_w1, f_b1, f_w2, f_b2, f_w3,
               f_freq, hy_b, q_norm, k_norm, lam_p, subln, cos_ax, sin_ax):
    B, S, _ = ul.shape
    C = uc.shape[1]
    hy_l, q_l, k_l, v_l = split_cols(ul @ w_in, E_SPLIT)
    hy_c, q_c, k_c, v_c = split_cols(uc @ w_in, E_SPLIT)
    y_hy_l = hyena_mixer(hy_l, conv_w, conv_b, hyena_filters(S, f_w1, f_b1, f_w2, f_b2, f_w3, f_freq), hy_b)
    qk = lambda t, g, L: rms_norm(t.reshape(B, L, DA_HEADS, 2, DA_HD)) * g
    scale = DA_HD ** -0.5
    lam_p = lam_p.astype(f32)
    lam = jnp.exp(jnp.sum(lam_p[0] * lam_p[1])) - jnp.exp(jnp.sum(lam_p[2] * lam_p[3])) + lam_init
    ql = apply_axial_rope(qk(q_l, q_norm, S), cos_ax, sin_ax) * scale
    kl = apply_axial_rope(qk(k_l, k_norm, S), cos_ax, sin_ax)
    kc = qk(k_c, k_norm, C)
    vl = v_l.reshape(B, S, DA_HEADS, 2 * DA_HD)
    vc = v_c.reshape(B, C, DA_HEADS, 2 * DA_HD)
    qc = qk(q_c, q_norm, C) * scale if need_ctx else None
    o_l, o_c = diff_attention(ql, kl, vl, qc, kc, vc, lam, lam_init, subln)
    out_l = jnp.concatenate([y_hy_l, o_l], axis=-1) @ w_out
    out_c = None
    if need_ctx:
        y_hy_c = hyena_mixer(hy_c, conv_w, conv_b, hyena_filters(C, f_w1, f_b1, f_w2, f_b2, f_w3, f_freq), hy_b)
        out_c = jnp.concatenate([y_hy_c, o_c], axis=-1) @ w_out
    return out_l, out_c


def odd_mixer(ul, uc, need_ctx, w_in, w_out, decay, gn_w, q_norm, k_norm, sink, cos_ax, sin_ax, cos_rt, sin_rt):
    B, S, _ = ul.shape
    C = uc.shape[1]
    rq_l, rk_l, rv_l, rg_l, gq_l, gk_l, gv_l = split_cols(ul @ w_in, O_SPLIT)
    rq_c, rk_c, rv_c, rg_c, gq_c, gk_c, gv_c = split_cols(uc @ w_in, O_SPLIT)
    hd = lambda t, L, d: t.reshape(B, L, RET_HEADS, d)
    rscale = RET_DK ** -0.5
    rql = apply_rope_1d(hd(rq_l, S, RET_DK), cos_rt, sin_rt)
    rkl = apply_rope_1d(hd(rk_l, S, RET_DK), cos_rt, sin_rt) * rscale
    rqc = hd(rq_c, C, RET_DK) if need_ctx else None
    o_rl, o_rc = bidir_retention(rql, rkl, hd(rv_l, S, RET_DV), rqc, hd(rk_c, C, RET_DK) * rscale,
                                 hd(rv_c, C, RET_DV), decay)
    y_rl = retention_out(o_rl, rg_l, gn_w)
    gscale = GQ_HD ** -0.5
    gql = apply_axial_rope(rms_norm(gq_l.reshape(B, S, GQ_KV, GQ_GROUP, GQ_HD)) * q_norm, cos_ax, sin_ax) * gscale
    gkl = apply_axial_rope(rms_norm(gk_l.reshape(B, S, GQ_KV, GQ_HD)) * k_norm, cos_ax, sin_ax)
    gkc = rms_norm(gk_c.reshape(B, C, GQ_KV, GQ_HD)) * k_norm
    gqc = rms_norm(gq_c.reshape(B, C, GQ_KV, GQ_GROUP, GQ_HD)) * q_norm * gscale if need_ctx else None
    y_gl, y_gc = window_gqa(gql, gkl, gv_l.reshape(B, S, GQ_KV, GQ_HD), gqc, gkc,
                            gv_c.reshape(B, C, GQ_KV, GQ_HD), sink)
    out_l = jnp.concatenate([y_rl, y_gl], axis=-1) @ w_out
    out_c = None
    if need_ctx:
        out_c = jnp.concatenate([retention_out(o_rc, rg_c, gn_w), y_gc], axis=-1) @ w_out
    return out_l, out_c


def hier_moe(t, w_grp, b_grp, w_rt, b_rt, w_gate, w_up, w_down):
    n_tok = t.shape[0]
    lg = (t @ w_grp + b_grp).astype(f32)
    pg = jax.nn.softmax(lg, axis=-1)
    oh_g = jax.nn.one_hot(jnp.argmax(lg, axis=-1), N_GROUPS, dtype=f32)
    le = (t @ w_rt + b_rt).astype(f32).reshape(n_tok, N_GROUPS, EXP_PER_GROUP)
    le_sel = jnp.einsum('tge,tg->te', le, oh_g)
    top_v, top_i = lax.top_k(le_sel, TOP_K)
    w = jax.nn.softmax(top_v, axis=-1) * jnp.max(pg, axis=-1, keepdims=True)
    comb_e = jnp.einsum('tke,tk->te', jax.nn.one_hot(top_i, EXP_PER_GROUP, dtype=f32), w)
    comb = (oh_g[:, :, None] * comb_e[:, None, :]).astype(t.dtype)
    out = jnp.zeros_like(t)
    for g in range(N_GROUPS):
        a = jax.nn.silu(jnp.einsum('td,edf->tef', t, w_gate[g])) * jnp.einsum('td,edf->tef', t, w_up[g])
        out = out + jnp.einsum('tef,efd->td', a * comb[:, g, :, None], w_down[g])
    return out


def setup_inputs(seed: int = 0) -> dict:
    key = jax.random.key(seed)
    ks = iter(jax.random.split(key, 64))

    def nrm(shape, std):
        return jax.random.normal(next(ks), shape, jnp.float32) * std

    D = D_MODEL
    G, E, F = N_GROUPS, EXP_PER_GROUP, D_EXPERT
    ret_base = jnp.log(jnp.exp2(5.0 + jnp.arange(RET_HEADS, dtype=jnp.float32)) - 1.0)
    return {
        "x": nrm((BATCH, SEQ, D), 1.0),
        "c": nrm((BATCH, D), 1.0),
        "ctx": nrm((BATCH, CTX_LEN, D), 1.0),
        "c_ctx": nrm((D,), 1.0),
        "ada_w": nrm((DEPTH, D, 6 * D), 0.5 * D ** -0.5),
        "ada_b": nrm((DEPTH, 6 * D), 0.02),
        "e_w_in": nrm((N_EVEN, D, E_IN), D ** -0.5),
        "e_w_out": nrm((N_EVEN, E_MIX, D), E_MIX ** -0.5),
        "hy_conv_w": nrm((N_EVEN, 3, 3 * HY_CH), 3 ** -0.5),
        "hy_conv_b": nrm((N_EVEN, 3 * HY_CH), 0.02),
        "hy_f_w1": nrm((N_EVEN, HY_EMB, HY_FILT_HID), HY_EMB ** -0.5),
        "hy_f_b1": nrm((N_EVEN, HY_FILT_HID), 0.1),
        "hy_f_w2": nrm((N_EVEN, HY_FILT_HID, HY_FILT_HID), HY_FILT_HID ** -0.5),
        "hy_f_b2": nrm((N_EVEN, HY_FILT_HID), 0.1),
        "hy_f_w3": nrm((N_EVEN, HY_FILT_HID, HY_ORDER * 2 * HY_CH), 0.05 * HY_FILT_HID ** -0.5),
        "hy_f_freq": 1.0 + nrm((N_EVEN, 2, HY_FILT_HID), 0.1),
        "hy_bias": nrm((N_EVEN, HY_ORDER, HY_CH), 0.1),
        "da_q_norm": 1.0 + nrm((N_EVEN, DA_HD), 0.02),
        "da_k_norm": 1.0 + nrm((N_EVEN, DA_HD), 0.02),
        "da_lam": nrm((N_EVEN, 4, DA_HD), 0.1),
        "da_subln": 1.0 + nrm((N_EVEN, 2 * DA_HD), 0.02),
        "o_w_in": nrm((N_ODD, D, O_IN), D ** -0.5),
        "o_w_out": nrm((N_ODD, O_MIX, D), O_MIX ** -0.5),
        "ret_decay": ret_base[None, None, :] + nrm((N_ODD, 2, RET_HEADS), 0.1),
        "ret_gn": 1.0 + nrm((N_ODD, RET_HEADS * RET_DV), 0.02),
        "gq_q_norm": 1.0 + nrm((N_ODD, GQ_HD), 0.02),
        "gq_k_norm": 1.0 + nrm((N_ODD, GQ_HD), 0.02),
        "gq_sink": nrm((N_ODD, GQ_KV * GQ_GROUP), 0.5),
        "moe_w_grp": nrm((DEPTH, D, G), D ** -0.5),
        "moe_b_grp": nrm((DEPTH, G), 0.01),
        "moe_w_rt": nrm((DEPTH, D, G * E), D ** -0.5),
        "moe_b_rt": nrm((DEPTH, G * E), 0.01),
        "moe_w_gate": nrm((DEPTH, G, E, D, F), D ** -0.5),
        "moe_w_up": nrm((DEPTH, G, E, D, F), D ** -0.5),
        "moe_w_down": nrm((DEPTH, G, E, F, D), F ** -0.5),
    }


def reference(x, c, ctx, c_ctx, ada_w, ada_b, e_w_in, e_w_out, hy_conv_w, hy_conv_b, hy_f_w1, hy_f_b1,
              hy_f_w2, hy_f_b2, hy_f_w3, hy_f_freq, hy_bias, da_q_norm, da_k_norm, da_lam, da_subln,
              o_w_in, o_w_out, ret_decay, ret_gn, gq_q_norm, gq_k_norm, gq_sink, moe_w_grp, moe_b_grp,
              moe_w_rt, moe_b_rt, moe_w_gate, moe_w_up, moe_w_down):
    B, S, D = x.shape
    C = ctx.shape[1]
    ROWS = S // GRID_W
    cos_da, sin_da = axial_rope(ROWS, DA_HD)
    cos_gq, sin_gq = axial_rope(ROWS, GQ_HD)
    cos_rt, sin_rt = seq_rope(S, RET_DK)
    silu_c = jax.nn.silu(c)
    silu_cc = jax.nn.silu(c_ctx)
    hl, hc = x, ctx
    for l in range(DEPTH):
        last = l == DEPTH - 1
        i = l // 2
        mod_l = (silu_c @ ada_w[l] + ada_b[l])[:, None, :]
        mod_c = (silu_cc @ ada_w[l] + ada_b[l])[None, None, :]
        sh1, sc1, g1, sh2, sc2, g2 = jnp.split(mod_l, 6, axis=-1)
        csh1, csc1, cg1, csh2, csc2, cg2 = jnp.split(mod_c, 6, axis=-1)
        ul = modulate(rms_norm(hl), sh1, sc1)
        uc = modulate(rms_norm(hc), csh1, csc1)
        if l % 2 == 0:
            lam_init = 0.8 - 0.6 * math.exp(-0.3 * l)
            ml, mc = even_mixer(ul, uc, not last, lam_init, e_w_in[i], e_w_out[i], hy_conv_w[i], hy_conv_b[i],
                                hy_f_w1[i], hy_f_b1[i], hy_f_w2[i], hy_f_b2[i], hy_f_w3[i], hy_f_freq[i],
                                hy_bias[i], da_q_norm[i], da_k_norm[i], da_lam[i], da_subln[i], cos_da, sin_da)
        else:
            ml, mc = odd_mixer(ul, uc, not last, o_w_in[i], o_w_out[i], ret_decay[i], ret_gn[i],
                               gq_q_norm[i], gq_k_norm[i], gq_sink[i], cos_gq, sin_gq, cos_rt, sin_rt)
        hl = hl + g1 * ml
        vl = modulate(rms_norm(hl), sh2, sc2).reshape(B * S, D)
        moe_args = (moe_w_grp[l], moe_b_grp[l], moe_w_rt[l], moe_b_rt[l], moe_w_gate[l], moe_w_up[l], moe_w_down[l])
        if last:
            hl = hl + g2 * hier_moe(vl, *moe_args).reshape(B, S, D)
        else:
            hc = hc + cg1 * mc
            vc = modulate(rms_norm(hc), csh2, csc2).reshape(B * C, D)
            y = hier_moe(jnp.concatenate([vl, vc], axis=0), *moe_args)
            hl = hl + g2 * y[:B * S].reshape(B, S, D)
            hc = hc + cg2 * y[B * S:].reshape(B, C, D)
    return hl
```

```python
import contextlib
import math
import numpy as np
import ml_dtypes
import concourse.bass as bass
import concourse.mybir as mybir
from concourse.bass_utils import run_bass_kernel_spmd

F32 = mybir.dt.float32
BF16 = mybir.dt.bfloat16
I32 = mybir.dt.int32
AF = mybir.ActivationFunctionType
ALU = mybir.AluOpType
AX = mybir.AxisListType

D = 1024
SEQ = 2048
CTX = 256
T = SEQ + CTX
NT = T // 128
EPS = 1e-6
NEXP = 32
DEXP = 256

ENGS = ("pe", "act", "dve", "pool", "sp")
NDMA_SEM = 8


class _Op:
    __slots__ = ("eng", "fn", "deps", "need_inc", "sem", "val", "is_dma", "rot_dep")

    def __init__(self, eng, fn, is_dma):
        self.eng = eng
        self.fn = fn
        self.deps = []
        self.need_inc = False
        self.sem = None
        self.val = 0
        self.is_dma = is_dma
        self.rot_dep = None


class Sched:
    def __init__(self, nc):
        self.nc = nc
        self.ops = {e: [] for e in ENGS}
        self.last_writer = {}
        self.readers = {}
        self.dma_hist = {e: [] for e in ENGS}

    def _track(self, op, reads, writes):
        deps = []
        for k in reads:
            w = self.last_writer.get(k)
            if w is not None:
                deps.append(w)
        for k in writes:
            w = self.last_writer.get(k)
            if w is not None:
                deps.append(w)
            deps.extend(self.readers.get(k, ()))
        seen = set()
        for dp in deps:
            if dp is op or id(dp) in seen:
                continue
            if dp.eng == "pe" and op.eng == "pe" and not dp.is_dma and not op.is_dma:
                continue
            seen.add(id(dp))
            op.deps.append(dp)
            dp.need_inc = True
        for k in writes:
            self.last_writer[k] = op
            self.readers[k] = []
        for k in reads:
            self.readers.setdefault(k, []).append(op)

    def op(self, eng, fn, reads=(), writes=()):
        o = _Op(eng, fn, False)
        self._track(o, reads, writes)
        self.ops[eng].append(o)
        return o

    def dma(self, eng, out, in_, reads=(), writes=(), **kw):
        def fn(e, out=out, in_=in_, kw=kw):
            return e.dma_start(out=out, in_=in_, **kw)
        o = _Op(eng, fn, True)
        o.need_inc = True
        self._track(o, reads, writes)
        hist = self.dma_hist[eng]
        if len(hist) >= NDMA_SEM:
            o.rot_dep = hist[-NDMA_SEM]
        hist.append(o)
        self.ops[eng].append(o)
        return o

    def barrier(self):
        lasts = []
        for e in ENGS:
            for o in reversed(self.ops[e]):
                if not o.is_dma and o.fn is not None:
                    lasts.append(o)
                    break
            lasts.extend(self.dma_hist[e][-NDMA_SEM:])
        for e in ENGS:
            o = _Op(e, None, False)
            for dp in lasts:
                o.deps.append(dp)
                dp.need_inc = True
            self.ops[e].append(o)
        self.last_writer = {}
        self.readers = {}

    def emit(self, final_wait_ops=()):
        nc = self.nc
        sems = {e: nc.alloc_semaphore(f"s_{e}") for e in ENGS}
        dsems = {e: [nc.alloc_semaphore(f"d_{e}{i}") for i in range(NDMA_SEM)] for e in ENGS
                 if self.dma_hist[e]}
        for e in ENGS:
            cnt = 0
            dcnt = [0] * NDMA_SEM
            di = 0
            for o in self.ops[e]:
                if o.is_dma:
                    j = di % NDMA_SEM
                    dcnt[j] += 16
                    o.sem = dsems[e][j]
                    o.val = dcnt[j]
                    di += 1
                elif o.need_inc and o.fn is not None:
                    cnt += 1
                    o.sem = sems[e]
                    o.val = cnt
        engobj = {"pe": "tensor", "act": "scalar", "dve": "vector", "pool": "gpsimd", "sp": "sync"}
        final_wait_ops = list(final_wait_ops)
        with nc.Block() as block:
            for e in ENGS:
                def body(eng, e=e):
                    waited = {}

                    def wait(dp):
                        if dp.sem is None:
                            return
                        key = id(dp.sem)
                        if waited.get(key, 0) >= dp.val:
                            return
                        waited[key] = dp.val
                        eng.wait_ge(dp.sem, dp.val)
                    for o in self.ops[e]:
                        for dp in o.deps:
                            wait(dp)
                        if o.rot_dep is not None:
                            wait(o.rot_dep)
                        if o.fn is None:
                            continue
                        ins = o.fn(eng)
                        if o.is_dma:
                            ins.then_inc(o.sem, 16)
                        elif o.need_inc:
                            ins.then_inc(o.sem, 1)
                    if e == "sp":
                        for dp in final_wait_ops:
                            wait(dp)
                getattr(block, engobj[e])(body)
        return {e: len(self.ops[e]) for e in ENGS}


class Ctx:
    def __init__(self, nc):
        self.nc = nc
        self.S = Sched(nc)
        self.stack = None
        self.uid = 0

    @contextlib.contextmanager
    def phase(self):
        prev = self.stack
        with contextlib.ExitStack() as st:
            self.stack = st
            yield
            self.S.barrier()
        self.stack = prev

    def sb(self, name, shape, dtype):
        self.uid += 1
        return self.stack.enter_context(self.nc.sbuf_tensor(f"{name}_{self.uid}", list(shape), dtype))

    def ps(self, name, shape, dtype=F32):
        self.uid += 1
        return self.stack.enter_context(self.nc.psum_tensor(f"{name}_{self.uid}", list(shape), dtype))


def token_blocks():
    return [(0, 512, 0), (512, 512, 0), (1024, 512, 0), (1536, 512, 0), (2048, 256, 1)]


def phase_load(C, G):
    S = C.S
    hT = G["hT"]
    with C.phase():
        xt = [C.sb(f"ld_x{i}", [128, D], F32) for i in range(3)]
        tp = [C.ps(f"ld_tp{i}", [128, 8, 128], F32) for i in range(2)]
        for t in range(NT):
            src = G["x"][t * 128:(t + 1) * 128, :] if t < 16 else G["ctx"][(t - 16) * 128:(t - 15) * 128, :]
            xb, pb = xt[t % 3], tp[t % 2]
            kx, kp = f"ldx{t % 3}", f"ldp{t % 2}"
            S.dma("sp", xb[:], src, writes=[kx])
            for k in range(8):
                S.op("pe", lambda e, k=k, xb=xb, pb=pb: e.transpose(out=pb[:, k, :], in_=xb[:, k * 128:(k + 1) * 128],
                                                                 identity=G["ident_f"][:]),
                     reads=[kx, "ident_f"], writes=[kp])
            eng = "act" if t % 2 == 0 else "dve"
            if eng == "act":
                S.op("act", lambda e, pb=pb, t=t: e.copy(out=hT[:, :, t * 128:(t + 1) * 128], in_=pb[:]),
                     reads=[kp], writes=[("hT", t)])
            else:
                S.op("dve", lambda e, pb=pb, t=t: e.tensor_copy(out=hT[:, :, t * 128:(t + 1) * 128], in_=pb[:]),
                     reads=[kp], writes=[("hT", t)])


def phase_store(C, G):
    S = C.S
    hT = G["hT"]
    outs = []
    with C.phase():
        ot = [C.sb(f"st_o{i}", [128, D], F32) for i in range(3)]
        tp = [C.ps(f"st_tp{i}", [128, 8, 128], F32) for i in range(2)]
        for t in range(16):
            ob, pb = ot[t % 3], tp[t % 2]
            ko, kp = f"sto{t % 3}", f"stp{t % 2}"
            for k in range(8):
                S.op("pe", lambda e, k=k, pb=pb, t=t: e.transpose(out=pb[:, k, :], in_=hT[:, k, t * 128:(t + 1) * 128],
                                                                identity=G["ident_f"][:]),
                     reads=[("hT", t), "ident_f"], writes=[kp])
            if t % 2 == 0:
                S.op("act", lambda e, pb=pb, ob=ob: e.copy(out=ob[:].rearrange("p (k d) -> p k d", k=8), in_=pb[:]),
                     reads=[kp], writes=[ko])
            else:
                S.op("dve", lambda e, pb=pb, ob=ob: e.tensor_copy(out=ob[:].rearrange("p (k d) -> p k d", k=8), in_=pb[:]),
                     reads=[kp], writes=[ko])
            outs.append(S.dma("sp", G["out"][t * 128:(t + 1) * 128, :], ob[:], reads=[ko]))
        G["final_ops"] = outs


def phase_mods(C, G, mid=None):
    S = C.S
    nc = C.nc
    with C.phase():
        c2sb = C.sb("c2sb", [2, D], F32)
        bsb = C.sb("bsb", [2, 6 * D], F32)
        cs = C.sb("cs", [128, 8, 2], BF16)
        bcol = C.sb("bcol", [128, 48, 2], F32)
        cps = C.ps("cps", [128, 8, 2], F32)
        bps = C.ps("bps", [128, 48, 2], F32)
        S.dma("sp", c2sb[:], G["c2"], writes=["c2sb"])
        S.dma("sp", bsb[:], G["ada_b"], writes=["bsb"])
        for k in range(8):
            S.op("pe", lambda e, k=k: e.transpose(out=cps[:, k, :], in_=c2sb[0:2, k * 128:(k + 1) * 128], identity=G["ident_f"][0:2, 0:2]),
                 reads=["c2sb", "ident_f"], writes=["cps"])
        for j in range(48):
            S.op("pe", lambda e, j=j: e.transpose(out=bps[:, j, :], in_=bsb[0:2, j * 128:(j + 1) * 128], identity=G["ident_f"][0:2, 0:2]),
                 reads=["bsb", "ident_f"], writes=["bps"])
        S.op("act", lambda e: e.activation(out=cs[:], in_=cps[:], func=AF.Silu), reads=["cps"], writes=["cs"])
        S.op("act", lambda e: e.copy(out=bcol[:], in_=bps[:]), reads=["bps"], writes=["bcol"])
        wb = [C.sb(f"adaw{i}", [128, 8, 1536], BF16) for i in range(2)]
        mp = C.ps("modp", [128, 2, 48, 2], F32)
        pieces = [(l, q) for l in range(2) for q in range(4)]

        def issue(i):
            l, q = pieces[i]
            S.dma("pool", wb[i % 2][:], G["ada_w"][l, :, q * 1536:(q + 1) * 1536].rearrange("(k p) n -> p k n", p=128), writes=[f"adaw{i % 2}"])
        issue(0)
        issue(1)
        if mid is not None:
            mid()
        i = 0
        for l in range(2):
            for q in range(4):
                w, kw = wb[i % 2], f"adaw{i % 2}"
                if i >= 2:
                    issue(i)
                for jj in range(12):
                    j = q * 12 + jj
                    for k in range(8):
                        S.op("pe", lambda e, w=w, l=l, j=j, jj=jj, k=k: e.matmul(mp[:, l, j, :], lhsT=w[:, k, jj * 128:(jj + 1) * 128],
                                                                              rhs=cs[:, k, :], start=(k == 0), stop=(k == 7)),
                             reads=[kw, "cs"], writes=["modp"])
                i += 1
        for l in range(2):
            for s in range(2):
                S.op("dve", lambda e, l=l, s=s: e.tensor_tensor(out=G["modc"][:, l, :, s], in0=mp[:, l, :, s], in1=bcol[:, :, l], op=ALU.add),
                     reads=["modp", "bcol"], writes=["modc"])
        for l in range(2):
            for j0 in (8, 32):
                S.op("dve", lambda e, l=l, j0=j0: e.tensor_scalar(out=G["modc"][:, l, j0:j0 + 8, :], in0=G["modc"][:, l, j0:j0 + 8, :],
                                                                 scalar1=1.0, scalar2=None, op0=ALU.add),
                     reads=["modc"], writes=["modc"])


def phase_norm(C, G, l, which, router=False, ntok=T):
    S = C.S
    sh0 = 0 if which == 0 else 24
    sc0 = 8 if which == 0 else 32
    hT, aT, modc = G["hT"], G["aT"], G["modc"]
    with C.phase():
        sq = [C.sb(f"nsq{i}", [128, 512], F32) for i in range(4)]
        tmp = [C.sb(f"ntmp{i}", [128, 512], F32) for i in range(3)]
        rstd = [C.sb(f"nrstd{i}", [128, 512], F32) for i in range(2)]
        ssp = [C.ps(f"nss{i}", [128, 512], F32) for i in range(2)]
        if router:
            v32 = [C.sb(f"nv32{i}", [128, 512], F32) for i in range(8)]
            wr = C.sb("wr32", [128, 8, 36], F32)
            rb = C.sb("rbias", [128, 36], F32)
            lgp = [C.ps(f"lgp{i}", [128, 4, 64], F32) for i in range(2)]
            ctp = C.ps("combTp", [32, 128], F32)
            combT = C.sb("combT", [32, T], BF16)
            S.dma("sp", wr[:, :, 0:4], G["moe_w_grp"][l].rearrange("(k p) n -> p k n", p=128), writes=["wr32a"])
            S.dma("sp", wr[:, :, 4:36], G["moe_w_rt"][l].rearrange("(k p) n -> p k n", p=128), writes=["wr32b"])
            S.dma("sp", rb[:, 0:4], G["moe_b_grp"][l:l + 1, :].to_broadcast([128, 4]), writes=["rba"])
            S.dma("sp", rb[:, 4:36], G["moe_b_rt"][l:l + 1, :].to_broadcast([128, 32]), writes=["rbb"])
            rts = [C.sb(f"rt{i}", [128, 160], F32) for i in range(8)]
        cnt = 0
        tile_idx = 0
        for bi, (t0, n, s) in enumerate(token_blocks()):
            if t0 >= ntok:
                continue
            sp_, rs_ = ssp[bi % 2], rstd[bi % 2]
            ksp, krs = f"nss{bi % 2}", f"nrstd{bi % 2}"
            hkeys = [("hT", tt) for tt in range(t0 // 128, (t0 + n) // 128)]
            for k in range(8):
                b = sq[cnt % 4]
                kb = f"nsq{cnt % 4}"
                cnt += 1
                if k % 2 == 0:
                    S.op("pool", lambda e, b=b, k=k, t0=t0, n=n: e.tensor_tensor(out=b[:, :n], in0=hT[:, k, t0:t0 + n], in1=hT[:, k, t0:t0 + n], op=ALU.mult),
                         reads=hkeys, writes=[kb])
                else:
                    S.op("act", lambda e, b=b, k=k, t0=t0, n=n: e.activation(out=b[:, :n], in_=hT[:, k, t0:t0 + n], func=AF.Square),
                         reads=hkeys, writes=[kb])
                S.op("pe", lambda e, b=b, k=k, n=n, sp_=sp_: e.matmul(sp_[:, :n], lhsT=G["ones_f"][:], rhs=b[:, :n], start=(k == 0), stop=(k == 7)),
                     reads=[kb, "ones_f"], writes=[ksp])
            S.op("act", lambda e, sp_=sp_, rs_=rs_, n=n: e.activation(out=rs_[:, :n], in_=sp_[:, :n], func=AF.Sqrt, scale=1.0 / D, bias=EPS),
                 reads=[ksp], writes=[krs])
            S.op("dve", lambda e, rs_=rs_, n=n: e.reciprocal(out=rs_[:, :n], in_=rs_[:, :n]), reads=[krs], writes=[krs])
            akeys = [("aT", tt) for tt in range(t0 // 128, (t0 + n) // 128)]
            lg = lgp[bi % 2] if router else None
            klg = f"lgp{bi % 2}"
            for k in range(8):
                tb = tmp[k % 3]
                ktb = f"ntmp{k % 3}"
                S.op("dve", lambda e, tb=tb, k=k, t0=t0, n=n, rs_=rs_: e.tensor_tensor(out=tb[:, :n], in0=hT[:, k, t0:t0 + n], in1=rs_[:, :n], op=ALU.mult),
                     reads=hkeys + [krs], writes=[ktb])
                if not router:
                    S.op("act", lambda e, tb=tb, k=k, t0=t0, n=n, s=s: e.activation(
                        out=aT[:, k, t0:t0 + n], in_=tb[:, :n], func=AF.Identity,
                        scale=modc[:, l, sc0 + k, s:s + 1], bias=modc[:, l, sh0 + k, s:s + 1]),
                        reads=[ktb, "modc"], writes=akeys)
                else:
                    vb = v32[k]
                    kvb = f"nv32{k}"
                    S.op("act", lambda e, tb=tb, vb=vb, k=k, n=n, s=s: e.activation(
                        out=vb[:, :n], in_=tb[:, :n], func=AF.Identity,
                        scale=modc[:, l, sc0 + k, s:s + 1], bias=modc[:, l, sh0 + k, s:s + 1]),
                        reads=[ktb, "modc"], writes=[kvb])
                    S.op("pool", lambda e, vb=vb, k=k, t0=t0, n=n: e.tensor_copy(out=aT[:, k, t0:t0 + n], in_=vb[:, :n]),
                         reads=[kvb], writes=akeys)
            if router:
                for st in range(n // 128):
                    for k in range(8):
                        S.op("pe", lambda e, k=k, st=st, lg=lg: e.matmul(lg[:, st, 0:36], lhsT=v32[k][:, st * 128:(st + 1) * 128], rhs=wr[:, k, :],
                                                                     start=(k == 0), stop=(k == 7)),
                             reads=[f"nv32{k}", "wr32a", "wr32b"], writes=[klg])
                gens = []
                for st in range(n // 128):
                    tt = t0 // 128 + st
                    gens.append(route_tile(C, G, lg[:, st, 0:36], klg, rb, rts[tile_idx % 8], f"rt{tile_idx % 8}", ctp, combT, tt))
                    tile_idx += 1
                while gens:
                    for g_ in list(gens):
                        try:
                            next(g_)
                        except StopIteration:
                            gens.remove(g_)
        if router:
            S.dma("sp", G["combT_d"][:, :], combT[:], reads=["combT"], writes=["combT_d"])


def route_tile(C, G, lgp, klg, rb, rt, krt, ctp, combT, tt):
    S = C.S
    LG = rt[:, 0:36]
    mg, nmg, sumg, pmax = rt[:, 36:37], rt[:, 37:38], rt[:, 38:39], rt[:, 39:40]
    ohg = rt[:, 40:44]
    eg = rt[:, 44:48]
    les = rt[:, 48:56]
    m1, m2, dm, ed = rt[:, 56:57], rt[:, 57:58], rt[:, 58:59], rt[:, 59:60]
    mk1 = rt[:, 60:68]
    les2 = rt[:, 68:76]
    mk2 = rt[:, 76:84]
    w1, w2 = rt[:, 84:85], rt[:, 85:86]
    ce = rt[:, 88:96]
    comb = rt[:, 96:128]
    R, W = [krt], [krt]

    def dv(fn, extra_r=()):
        S.op("dve", fn, reads=R + list(extra_r), writes=W)

    dv(lambda e: e.tensor_tensor(out=LG, in0=lgp, in1=rb[:], op=ALU.add), extra_r=[klg, "rba", "rbb"])
    yield
    dv(lambda e: e.reduce_max(out=mg, in_=rt[:, 0:4], axis=AX.X))
    yield
    dv(lambda e: e.tensor_scalar(out=ohg, in0=rt[:, 0:4], scalar1=mg, scalar2=None, op0=ALU.is_equal))
    yield
    dv(lambda e: e.tensor_scalar(out=nmg, in0=mg, scalar1=-1.0, scalar2=None, op0=ALU.mult))
    yield
    S.op("act", lambda e: e.activation(out=eg, in_=rt[:, 0:4], func=AF.Exp, bias=nmg, scale=1.0, accum_out=sumg), reads=R, writes=W)
    yield
    dv(lambda e: e.reciprocal(out=pmax, in_=sumg))
    yield
    dv(lambda e: e.tensor_scalar(out=les, in0=rt[:, 4:12], scalar1=rt[:, 40:41], scalar2=None, op0=ALU.mult))
    yield
    for g in range(1, 4):
        dv(lambda e, g=g: e.scalar_tensor_tensor(out=les, in0=rt[:, 4 + 8 * g:12 + 8 * g], scalar=rt[:, 40 + g:41 + g], in1=les,
                                                 op0=ALU.mult, op1=ALU.add))
        yield
    dv(lambda e: e.reduce_max(out=m1, in_=les, axis=AX.X))
    yield
    dv(lambda e: e.tensor_scalar(out=mk1, in0=les, scalar1=m1, scalar2=None, op0=ALU.is_equal))
    yield
    dv(lambda e: e.scalar_tensor_tensor(out=les2, in0=mk1, scalar=-1e30, in1=les, op0=ALU.mult, op1=ALU.add))
    yield
    dv(lambda e: e.reduce_max(out=m2, in_=les2, axis=AX.X))
    yield
    dv(lambda e: e.tensor_scalar(out=mk2, in0=les2, scalar1=m2, scalar2=None, op0=ALU.is_equal))
    yield
    dv(lambda e: e.tensor_tensor(out=dm, in0=m2, in1=m1, op=ALU.subtract))
    yield
    S.op("act", lambda e: e.activation(out=ed, in_=dm, func=AF.Exp), reads=R, writes=W)
    yield
    dv(lambda e: e.tensor_scalar(out=w1, in0=ed, scalar1=1.0, scalar2=None, op0=ALU.add))
    yield
    dv(lambda e: e.reciprocal(out=w1, in_=w1))
    yield
    dv(lambda e: e.tensor_tensor(out=w2, in0=ed, in1=w1, op=ALU.mult))
    yield
    dv(lambda e: e.tensor_scalar(out=rt[:, 84:86], in0=rt[:, 84:86], scalar1=pmax, scalar2=None, op0=ALU.mult))
    yield
    dv(lambda e: e.tensor_scalar(out=ce, in0=mk1, scalar1=w1, scalar2=None, op0=ALU.mult))
    yield
    dv(lambda e: e.scalar_tensor_tensor(out=ce, in0=mk2, scalar=w2, in1=ce, op0=ALU.mult, op1=ALU.add))
    yield
    for g in range(4):
        dv(lambda e, g=g: e.tensor_scalar(out=rt[:, 96 + 8 * g:104 + 8 * g], in0=ce, scalar1=rt[:, 40 + g:41 + g], scalar2=None, op0=ALU.mult))
        yield
    S.op("pe", lambda e: e.transpose(out=ctp[:, :], in_=comb, identity=G["ident_f"][:]), reads=R + ["ident_f"], writes=["combTp"])
    S.op("act", lambda e: e.copy(out=combT[:, tt * 128:(tt + 1) * 128], in_=ctp[:, :]), reads=["combTp"], writes=["combT"])
    yield


def phase_moe(C, G, l, ntok):
    S = C.S
    hT, aT, modc = G["hT"], G["aT"], G["modc"]
    EG = 2
    NB = 2 * EG
    blocks = [(t0, n) for (t0, n, s_) in token_blocks() if t0 < ntok]
    with C.phase():
        wg = [C.sb(f"wg{i}", [128, 8, 256], BF16) for i in range(NB)]
        wu = [C.sb(f"wu{i}", [128, 8, 256], BF16) for i in range(NB)]
        wd = [C.sb(f"wd{i}", [128, 2, 1024], BF16) for i in range(NB)]
        cb = [C.sb(f"cb{i}", [128, T], BF16) for i in range(NB)]
        sg = [C.sb(f"sg{i}", [128, 512], F32) for i in range(2)]
        tg = [C.sb(f"tg{i}", [128, 512], F32) for i in range(2)]
        at = [[[C.sb(f"at{u}_{e}_{c}", [128, 512], BF16) for c in range(2)] for e in range(EG)] for u in range(2)]
        hp = [C.ps(f"hp{i}", [128, 2, 512], F32) for i in range(2)]
        yp = C.ps("yp", [128, 4, 512], F32)

        def load_group(g0):
            for e in range(g0, g0 + EG):
                w = e % NB
                gi, ei = e // 8, e % 8
                S.dma("pool", wg[w][:], G["moe_w_gate"][l, gi, ei].rearrange("(k p) f -> p k f", p=128), writes=[f"wg{w}"])
                S.dma("pool", wu[w][:], G["moe_w_up"][l, gi, ei].rearrange("(k p) f -> p k f", p=128), writes=[f"wu{w}"])
                S.dma("pool", wd[w][:], G["moe_w_down"][l, gi, ei].rearrange("(c p) d -> p c d", p=128), writes=[f"wd{w}"])
                S.dma("sp", cb[w][:, :], G["combT_d"][e:e + 1, :].to_broadcast([128, T]), writes=[f"cb{w}"])

        units = [(g0, bi) for g0 in range(0, NEXP, EG) for bi in range(len(blocks))]
        gcount = [0]

        def emit_g(u, el, c):
            g0, bi = units[u]
            t0, n = blocks[bi]
            e = g0 + el
            w = e % NB
            i = gcount[0]
            gcount[0] += 1
            h, kh = hp[i % 2], f"hp{i % 2}"
            for (wt, kw, q) in ((wg, f"wg{w}", 0), (wu, f"wu{w}", 1)):
                for k in range(8):
                    S.op("pe", lambda e_, h=h, wt=wt, w=w, c=c, k=k, t0=t0, n=n, q=q: e_.matmul(
                        h[:, q, :n], lhsT=wt[w][:, k, c * 128:(c + 1) * 128], rhs=aT[:, k, t0:t0 + n], start=(k == 0), stop=(k == 7)),
                        reads=[kw], writes=[kh])
            sgt, tgt = sg[i % 2], tg[i % 2]
            ksg, ktg = f"sg{i % 2}", f"tg{i % 2}"
            a_ = at[u % 2][el][c]
            ka = f"at{u % 2}_{el}_{c}"
            S.op("act", lambda e_, h=h, sgt=sgt, n=n: e_.activation(out=sgt[:, :n], in_=h[:, 0, :n], func=AF.Silu), reads=[kh], writes=[ksg])
            S.op("dve", lambda e_, h=h, tgt=tgt, w=w, t0=t0, n=n: e_.tensor_tensor(out=tgt[:, :n], in0=h[:, 1, :n], in1=cb[w][:, t0:t0 + n], op=ALU.mult),
                 reads=[kh, f"cb{w}"], writes=[ktg])
            S.op("pool", lambda e_, sgt=sgt, tgt=tgt, a_=a_, n=n: e_.tensor_tensor(out=a_[:, :n], in0=sgt[:, :n], in1=tgt[:, :n], op=ALU.mult),
                 reads=[ksg, ktg], writes=[ka])

        def emit_d(u, half):
            g0, bi = units[u]
            t0, n = blocks[bi]
            for jj in range(4):
                j = half * 4 + jj
                for el in range(EG):
                    w = (g0 + el) % NB
                    for c in range(2):
                        S.op("pe", lambda e_, jj=jj, j=j, el=el, w=w, c=c, n=n, u=u: e_.matmul(
                            yp[:, jj, :n], lhsT=wd[w][:, c, j * 128:(j + 1) * 128], rhs=at[u % 2][el][c][:, :n],
                            start=(el == 0 and c == 0), stop=(el == EG - 1 and c == 1)),
                            reads=[f"wd{w}", f"at{u % 2}_{el}_{c}"], writes=[("yp", jj)])
            s = 0 if t0 < SEQ else 1
            hkeys = [("hT", tt) for tt in range(t0 // 128, (t0 + n) // 128)]
            for jj in range(4):
                j = half * 4 + jj
                S.op("dve", lambda e_, j=j, jj=jj, t0=t0, n=n, s=s: e_.scalar_tensor_tensor(
                    out=hT[:, j, t0:t0 + n], in0=yp[:, jj, :n], scalar=modc[:, l, 40 + j, s:s + 1], in1=hT[:, j, t0:t0 + n],
                    op0=ALU.mult, op1=ALU.add),
                    reads=[("yp", jj), "modc"] + hkeys, writes=hkeys)

        load_group(0)
        for el in range(EG):
            for c in range(2):
                emit_g(0, el, c)
        for u in range(len(units)):
            g0, bi = units[u]
            if bi == 0 and g0 + EG < NEXP:
                load_group(g0 + EG)
            nxt = u + 1 < len(units)
            if nxt:
                emit_g(u + 1, 0, 0)
                emit_g(u + 1, 0, 1)
            emit_d(u, 0)
            if nxt:
                emit_g(u + 1, 1, 0)
                emit_g(u + 1, 1, 1)
            emit_d(u, 1)


def load_cols(C, G, name, srcs, nchunk, width=128):
    S = C.S
    R = sum(a.shape[0] for a in srcs)
    cols = C.sb(name + "_cols", [128, nchunk, R], F32)
    with C.phase():
        rows = C.sb(name + "_rows", [R, nchunk * width], F32)
        pp = C.ps(name + "_ps", [128, nchunk, R], F32)
        r0 = 0
        for i, a in enumerate(srcs):
            S.dma("sp", rows[r0:r0 + a.shape[0], :], a, writes=[(name, "rows", i)])
            r0 += a.shape[0]
        rk = [(name, "rows", i) for i in range(len(srcs))]
        for j in range(nchunk):
            S.op("pe", lambda e, j=j: e.transpose(out=pp[0:width, j, :], in_=rows[0:R, j * width:(j + 1) * width], identity=G["ident_f"][0:R, 0:R]),
                 reads=rk + ["ident_f"], writes=[(name, "ps")])
        S.op("act", lambda e: e.copy(out=cols[0:width], in_=pp[0:width]), reads=[(name, "ps")], writes=[name])
    return cols


def phase_hy_inproj(C, G):
    S = C.S
    aT = G["aT"]
    with C.phase():
        w = C.sb("why", [128, 8, 1536], BF16)
        S.dma("pool", w[:], G["e_w_in"][0, :, 0:1536].rearrange("(k p) n -> p k n", p=128), writes=["why"])
        cw = load_cols(C, G, "hycw", [G["hy_conv_w"][0], G["hy_conv_b"]], 12)
        raw = [C.sb(f"hyraw{i}", [128, T + 4], F32) for i in range(2)]
        tmp = [C.sb(f"hytmp{i}", [128, T], F32) for i in range(2)]
        ob = [C.sb(f"hyob{i}", [128, T], BF16) for i in range(2)]
        vt = [C.sb(f"hyvt{i}", [128, 4, 128], BF16) for i in range(2)]
        pp = [C.ps(f"hypp{i}", [128, 512], F32) for i in range(3)]
        tp = [C.ps(f"hytp{i}", [128, 4, 128], BF16) for i in range(2)]
        for i in range(2):
            for c0 in (0, SEQ + 1, SEQ + 2, T + 3):
                S.op("pool", lambda e, i=i, c0=c0: e.memset(raw[i][:, c0:c0 + 1], 0.0), writes=[f"hyraw{i}"])
        ip = 0
        for cc in range(12):
            rw, tm, o = raw[cc % 2], tmp[cc % 2], ob[cc % 2]
            krw, ktm, ko = f"hyraw{cc % 2}", f"hytmp{cc % 2}", f"hyob{cc % 2}"
            for (t0, n, s) in token_blocks():
                p = pp[ip % 3]
                kp = f"hypp{ip % 3}"
                ip += 1
                for k in range(8):
                    S.op("pe", lambda e, p=p, k=k, cc=cc, t0=t0, n=n: e.matmul(p[:, :n], lhsT=w[:, k, cc * 128:(cc + 1) * 128], rhs=aT[:, k, t0:t0 + n],
                                                                            start=(k == 0), stop=(k == 7)),
                         reads=["why", "aT_all"], writes=[kp])
                off = 1 + t0 if s == 0 else 3 + t0
                S.op("act", lambda e, p=p, rw=rw, off=off, n=n: e.copy(out=rw[:, off:off + n], in_=p[:, :n]), reads=[kp], writes=[krw])
            for (a0, L_, o0) in ((0, SEQ, 0), (SEQ + 2, CTX, SEQ)):
                S.op("act", lambda e, rw=rw, tm=tm, a0=a0, L_=L_, o0=o0, cc=cc: e.activation(
                    out=tm[:, o0:o0 + L_], in_=rw[:, a0 + 1:a0 + 1 + L_], func=AF.Identity, scale=cw[:, cc, 1:2], bias=cw[:, cc, 3:4]),
                    reads=[krw, "hycw"], writes=[ktm])
                S.op("dve", lambda e, rw=rw, tm=tm, a0=a0, L_=L_, o0=o0, cc=cc: e.scalar_tensor_tensor(
                    out=tm[:, o0:o0 + L_], in0=rw[:, a0:a0 + L_], scalar=cw[:, cc, 0:1], in1=tm[:, o0:o0 + L_], op0=ALU.mult, op1=ALU.add),
                    reads=[krw, ktm, "hycw"], writes=[ktm])
                S.op("dve", lambda e, rw=rw, tm=tm, o=o, a0=a0, L_=L_, o0=o0, cc=cc: e.scalar_tensor_tensor(
                    out=o[:, o0:o0 + L_], in0=rw[:, a0 + 2:a0 + 2 + L_], scalar=cw[:, cc, 2:3], in1=tm[:, o0:o0 + L_], op0=ALU.mult, op1=ALU.add),
                    reads=[krw, ktm, "hycw"], writes=[ko])
            S.dma("sp", G["hyc_d"][cc * 128:(cc + 1) * 128, :], o[:], reads=[ko], writes=[("hyc_d", cc)])
            if cc < 4:
                for tg in range(0, NT, 4):
                    nt = min(4, NT - tg)
                    t_ps, t_sb = tp[(tg // 4) % 2], vt[(tg // 4) % 2]
                    kps, ksb = f"hytp{(tg // 4) % 2}", f"hyvt{(tg // 4) % 2}"
                    for i in range(nt):
                        S.op("pe", lambda e, i=i, tg=tg, o=o, t_ps=t_ps: e.transpose(out=t_ps[:, i, :], in_=o[:, (tg + i) * 128:(tg + i + 1) * 128],
                                                                                identity=G["ident_b"][:]),
                             reads=[ko, "ident_b"], writes=[kps])
                    S.op("dve", lambda e, t_ps=t_ps, t_sb=t_sb, nt=nt: e.tensor_copy(out=t_sb[:, :nt, :], in_=t_ps[:, :nt, :]), reads=[kps], writes=[ksb])
                    S.dma("sp", G["vtok_d"][tg * 128:(tg + nt) * 128, cc * 128:(cc + 1) * 128].rearrange("(i p) c -> p i c", p=128),
                          t_sb[:, :nt, :], reads=[ksb], writes=[("vtok_d", cc, tg)])


def phase_hy_filter(C, G, L, zemb_name, out_name):
    S = C.S
    TWO_PI = 2.0 * math.pi
    with C.phase():
        zT = C.sb("zT", [33, L], F32)
        w1 = C.sb("fw1", [33, 64], F32)
        w2 = C.sb("fw2", [64, 64], F32)
        w3 = C.sb("fw3", [64, 2048], F32)
        S.dma("sp", zT[:], G[zemb_name], writes=["zT"])
        S.dma("sp", w1[:], G["hy_f_w1"][0], writes=["fw1"])
        S.dma("sp", w2[:], G["hy_f_w2"][0], writes=["fw2"])
        S.dma("sp", w3[:], G["hy_f_w3"][0], writes=["fw3"])
        pc = load_cols(C, G, "hyfp", [G["hy_f_b1"], G["hy_f_b2"], G["hy_f_freq"][0]], 1, width=64)
        dl = C.sb("delta_bc", [128, 512], F32)
        negt = C.sb("negt", [128, L // 128], F32)
        mask0 = C.sb("mask0", [128, 1], F32)
        S.dma("sp", dl[:], G["c_delta"][0:1, :].to_broadcast([128, 512]), writes=["delta_bc"])
        S.dma("sp", negt[:], G["c_negt_%d" % L], writes=["negt"])
        S.dma("sp", mask0[:], G["c_mask0"], writes=["mask0"])
        h1 = C.sb("fh1", [64, L], F32)
        h2 = C.sb("fh2", [64, L], F32)
        a = C.sb("fa", [64, 512], F32)
        ki = C.sb("fki", [64, 512], I32)
        kf = C.sb("fkf", [64, 512], F32)
        hp = C.ps("fhp", [64, 512], F32)
        nb = max(1, L // 512)
        bs = min(L, 512)
        for layer, (wt, kw, src, ksrc, dst, kdst, bcol, fcol) in enumerate((
                (w1, "fw1", zT, "zT", h1, "fh1", 0, 2), (w2, "fw2", h1, "fh1", h2, "fh2", 1, 3))):
            for b in range(nb):
                sl = slice(b * bs, (b + 1) * bs)
                S.op("pe", lambda e, wt=wt, src=src, sl=sl: e.matmul(hp[:, :bs], lhsT=wt[:], rhs=src[:, sl], start=True, stop=True),
                     reads=[kw, ksrc], writes=["fhp"])
                S.op("dve", lambda e, bcol=bcol, fcol=fcol: e.tensor_scalar(out=a[:, :bs], in0=hp[:, :bs], scalar1=pc[0:64, 0, bcol:bcol + 1],
                                                                           scalar2=pc[0:64, 0, fcol:fcol + 1], op0=ALU.add, op1=ALU.mult),
                     reads=["fhp", "hyfp"], writes=["fa"])
                S.op("dve", lambda e: e.tensor_scalar(out=ki[:, :bs], in0=a[:, :bs], scalar1=1.0 / TWO_PI, scalar2=None, op0=ALU.mult),
                     reads=["fa"], writes=["fki"])
                S.op("dve", lambda e: e.tensor_copy(out=kf[:, :bs], in_=ki[:, :bs]), reads=["fki"], writes=["fkf"])
                S.op("dve", lambda e: e.scalar_tensor_tensor(out=a[:, :bs], in0=kf[:, :bs], scalar=-TWO_PI, in1=a[:, :bs], op0=ALU.mult, op1=ALU.add),
                     reads=["fkf", "fa"], writes=["fa"])
                S.op("dve", lambda e: e.tensor_scalar(out=a[:, :bs], in0=a[:, :bs], scalar1=3.1415925, scalar2=-3.1415925, op0=ALU.min, op1=ALU.max),
                     reads=["fa"], writes=["fa"])
                S.op("act", lambda e, dst=dst, sl=sl: e.activation(out=dst[:, sl], in_=a[:, :bs], func=AF.Sin), reads=["fa"], writes=[kdst])
        p3 = [C.ps(f"fp3_{i}", [128, 512], F32) for i in range(4)]
        dec = [C.sb(f"fdec{i}", [128, 512], F32) for i in range(2)]
        bw = [C.sb(f"fbw{i}", [128, 512], F32) for i in range(2)]
        sm = [C.sb(f"fsm{i}", [128, 512], F32) for i in range(2)]
        df = [C.sb(f"fdf{i}", [128, 512], F32) for i in range(2)]
        fo = [C.sb(f"ffo{i}", [128, 2, 512], BF16) for i in range(2)]
        it = 0
        for lt in range(L // 128):
            dc, kdc = dec[lt % 2], f"fdec{lt % 2}"
            S.op("act", lambda e, dc=dc, lt=lt: e.activation(out=dc[:], in_=dl[:], func=AF.Exp, scale=negt[:, lt:lt + 1]),
                 reads=["delta_bc", "negt"], writes=[kdc])
            for n in range(2):
                for dr in range(2):
                    q = n * 2 + dr
                    S.op("pe", lambda e, q=q, lt=lt: e.matmul(p3[q][:], lhsT=h2[:, lt * 128:(lt + 1) * 128], rhs=w3[:, q * 512:(q + 1) * 512],
                                                           start=True, stop=True),
                         reads=["fh2", "fw3"], writes=[f"fp3_{q}"])
            for n in range(2):
                b_, s_, d_, o_ = bw[it % 2], sm[it % 2], df[it % 2], fo[it % 2]
                kb, ks, kd, kfo = f"fbw{it % 2}", f"fsm{it % 2}", f"fdf{it % 2}", f"ffo{it % 2}"
                it += 1
                pf, pb = p3[n * 2], p3[n * 2 + 1]
                if lt == 0:
                    S.op("act", lambda e, b_=b_, pb=pb: e.activation(out=b_[:], in_=pb[:], func=AF.Identity, scale=mask0[:, 0:1]),
                         reads=[f"fp3_{n * 2 + 1}", "mask0"], writes=[kb])
                else:
                    S.op("act", lambda e, b_=b_, pb=pb: e.copy(out=b_[:], in_=pb[:]), reads=[f"fp3_{n * 2 + 1}"], writes=[kb])
                S.op("dve", lambda e, s_=s_, pf=pf, b_=b_: e.tensor_tensor(out=s_[:], in0=pf[:], in1=b_[:], op=ALU.add),
                     reads=[f"fp3_{n * 2}", kb], writes=[ks])
                S.op("dve", lambda e, d_=d_, pf=pf, b_=b_: e.tensor_tensor(out=d_[:], in0=pf[:], in1=b_[:], op=ALU.subtract),
                     reads=[f"fp3_{n * 2}", kb], writes=[kd])
                S.op("pool", lambda e, o_=o_, s_=s_, dc=dc: e.tensor_tensor(out=o_[:, 0, :], in0=s_[:], in1=dc[:], op=ALU.mult),
                     reads=[ks, kdc], writes=[kfo])
                S.op("pool", lambda e, o_=o_, d_=d_, dc=dc: e.tensor_tensor(out=o_[:, 1, :], in0=d_[:], in1=dc[:], op=ALU.mult),
                     reads=[kd, kdc], writes=[kfo])
                S.dma("sp", G[out_name][n, :, lt * 128:(lt + 1) * 128, :].rearrange("s p c -> p s c"), o_[:], reads=[kfo],
                      writes=[(out_name, n, lt)])


def phase_hyena_conv(C, G, L, tok0, filt_name, Fname, Gname, vtok_src):
    S = C.S
    nsc = L // 128
    npair = L // 128
    tb = min(L, 512)
    ntb = L // tb
    with C.phase():
        ztok = C.sb("ztok", [128, nsc, 512], BF16)
        hb = load_cols(C, G, "hybias", [G["hy_bias"][0]], 4)
        S.dma("sp", ztok[:], G[vtok_src][tok0:tok0 + L, :].rearrange("(c p) n -> p c n", p=128), writes=["ztok"])
        for order in range(2):
            with C.phase():
                fs = C.sb("fs", [128, nsc, 512], BF16)
                fd = C.sb("fd", [128, nsc, 512], BF16)
                S.dma("sp", fs[:], G[filt_name][order, 0].rearrange("(c p) n -> p c n", p=128), writes=["fs"])
                S.dma("sp", fd[:], G[filt_name][order, 1].rearrange("(c p) n -> p c n", p=128), writes=["fd"])
                Y = C.sb("Y", [128, 2 * npair, 512], BF16)
                with C.phase():
                    Ft = [C.sb(f"Ft{i}", [128, 2, nsc, 128], BF16) for i in range(2)]
                    zp = [C.ps(f"zp{i}", [128, 4, 512], F32) for i in range(2)]
                    hs = [C.sb(f"hs{i}", [128, 2, 512], F32) for i in range(2)]
                    t1 = [C.sb(f"t1_{i}", [128, 2, 512], F32) for i in range(2)]
                    t2 = [C.sb(f"t2_{i}", [128, 2, 512], F32) for i in range(2)]
                    for j in range(npair):
                        F_, kF = Ft[j % 2], f"Ft{j % 2}"
                        z, kz = zp[j % 2], f"zp{j % 2}"
                        h_, kh = hs[j % 2], f"hs{j % 2}"
                        a_, ka = t1[j % 2], f"t1_{j % 2}"
                        b_, kb = t2[j % 2], f"t2_{j % 2}"
                        S.dma("sp", F_[:, 0], G[Fname][j], writes=[kF])
                        S.dma("sp", F_[:, 1], G[Fname][npair + j], writes=[kF])
                        for q, (ri, mv, kmv) in enumerate(((0, ztok, "ztok"), (1, ztok, "ztok"), (0, fs, "fs"), (1, fd, "fd"))):
                            for c in range(nsc):
                                S.op("pe", lambda e, z=z, q=q, ri=ri, c=c, mv=mv, F_=F_: e.matmul(z[:, q, :], lhsT=F_[:, ri, c, :], rhs=mv[:, c, :],
                                                                                             start=(c == 0), stop=(c == nsc - 1)),
                                     reads=[kF, kmv], writes=[kz])
                        S.op("act", lambda e, z=z, h_=h_: e.copy(out=h_[:], in_=z[:, 2:4, :]), reads=[kz], writes=[kh])
                        S.op("dve", lambda e, z=z, h_=h_, a_=a_: e.tensor_tensor(out=a_[:], in0=z[:, 0:2, :], in1=h_[:], op=ALU.mult),
                             reads=[kz, kh], writes=[ka])
                        S.op("dve", lambda e, z=z, h_=h_, b_=b_: e.tensor_tensor(out=b_[:, 0, :], in0=z[:, 0, :], in1=h_[:, 1, :], op=ALU.mult),
                             reads=[kz, kh], writes=[kb])
                        S.op("dve", lambda e, z=z, h_=h_, b_=b_: e.tensor_tensor(out=b_[:, 1, :], in0=z[:, 1, :], in1=h_[:, 0, :], op=ALU.mult),
                             reads=[kz, kh], writes=[kb])
                        S.op("pool", lambda e, a_=a_, j=j, Y=Y: e.tensor_tensor(out=Y[:, j, :], in0=a_[:, 0, :], in1=a_[:, 1, :], op=ALU.subtract),
                             reads=[ka], writes=["Y"])
                        S.op("pool", lambda e, b_=b_, j=j, Y=Y: e.tensor_tensor(out=Y[:, npair + j, :], in0=b_[:, 0, :], in1=b_[:, 1, :], op=ALU.add),
                             reads=[kb], writes=["Y"])
                with C.phase():
                    KG = 4
                    Gt = [C.sb(f"Gt{i}", [128, KG, tb], BF16) for i in range(3)]
                    op_ = [C.ps(f"op{i}", [128, tb], F32) for i in range(4)]
                    zprev = [C.sb(f"zprev{i}", [128, tb], BF16) for i in range(4)]
                    gate = [C.sb(f"gate{i}", [128, tb], BF16) for i in range(4)]
                    tmpf = [C.sb(f"tmpf{i}", [128, tb], F32) for i in range(2)]
                    zn = [C.sb(f"zn{i}", [128, tb], BF16) for i in range(2)]
                    ttp = [C.ps(f"ttp{i}", [128, 4, 128], BF16) for i in range(2)]
                    ig = 0
                    ie = 0
                    for b in range(ntb):
                        for kg in range(2 * npair // KG):
                            g_, kg_ = Gt[ig % 3], f"Gt{ig % 3}"
                            ig += 1
                            S.dma("sp", g_[:], G[Gname][b, kg], writes=[kg_])
                            for kk in range(KG):
                                kc = kg * KG + kk
                                for cc in range(4):
                                    S.op("pe", lambda e, cc=cc, kc=kc, kk=kk, g_=g_, Y=Y, op_=op_: e.matmul(op_[cc][:], lhsT=Y[:, kc, cc * 128:(cc + 1) * 128], rhs=g_[:, kk, :],
                                                                                         start=(kc == 0), stop=(kc == 2 * npair - 1)),
                                         reads=["Y", kg_], writes=[f"op{cc}"])
                        t_lo = tok0 + b * tb
                        for cc in range(4):
                            zp_, kzp = zprev[ie % 4], f"zprev{ie % 4}"
                            gt_, kgt = gate[ie % 4], f"gate{ie % 4}"
                            tf_, ktf = tmpf[ie % 2], f"tmpf{ie % 2}"
                            zn_, kzn = zn[ie % 2], f"zn{ie % 2}"
                            tp_, ktp = ttp[ie % 2], f"ttp{ie % 2}"
                            ie += 1
                            if order == 0:
                                S.dma("sp", zp_[:], G["hyc_d"][cc * 128:(cc + 1) * 128, t_lo:t_lo + tb], writes=[kzp])
                            else:
                                S.dma("sp", zp_[:], G["z1_d"][cc * 128:(cc + 1) * 128, t_lo:t_lo + tb], reads=[("z1_d", cc, t_lo)], writes=[kzp])
                            grow = (1 + order) * 512 + cc * 128
                            S.dma("sp", gt_[:], G["hyc_d"][grow:grow + 128, t_lo:t_lo + tb], writes=[kgt])
                            S.op("dve", lambda e, tf_=tf_, zp_=zp_, cc=cc, order=order, op_=op_: e.scalar_tensor_tensor(
                                out=tf_[:], in0=zp_[:], scalar=hb[:, cc, order:order + 1], in1=op_[cc][:], op0=ALU.mult, op1=ALU.add),
                                reads=[kzp, f"op{cc}", "hybias"], writes=[ktf])
                            S.op("pool", lambda e, zn_=zn_, tf_=tf_, gt_=gt_: e.tensor_tensor(out=zn_[:], in0=tf_[:], in1=gt_[:], op=ALU.mult),
                                 reads=[ktf, kgt], writes=[kzn])
                            if order == 0:
                                S.dma("pool", G["z1_d"][cc * 128:(cc + 1) * 128, t_lo:t_lo + tb], zn_[:], reads=[kzn], writes=[("z1_d", cc, t_lo)])
                                for i in range(tb // 128):
                                    S.op("pe", lambda e, i=i, zn_=zn_, tp_=tp_: e.transpose(out=tp_[:, i, :], in_=zn_[:, i * 128:(i + 1) * 128],
                                                                                       identity=G["ident_b"][:]),
                                         reads=[kzn, "ident_b"], writes=[ktp])
                                c0 = b * (tb // 128)
                                S.op("act", lambda e, tp_=tp_, cc=cc, c0=c0: e.copy(out=ztok[:, c0:c0 + tb // 128, cc * 128:(cc + 1) * 128],
                                                                                in_=tp_[:, 0:tb // 128, :]),
                                     reads=[ktp], writes=["ztok"])
                            else:
                                S.dma("pool", G["mix_d"][cc * 128:(cc + 1) * 128, t_lo:t_lo + tb], zn_[:], reads=[kzn], writes=[("mix_d", cc, t_lo)])


def qk_prep(C, G, pq, kpq, gain, kgain, cs, sn, tt, rope, dstT, kdst, nh, W, tag):
    S = C.S
    n = nh * 64
    sq, ss, xn, xg, t1, t2, qn, tp = W["sq"], W["ss"], W["xn"], W["xg"], W["t1"], W["t2"], W["qn"], W["tp"]
    k = lambda nm: (W.get("tpkey", (tag, "tp")) if nm == "tp" else (tag, nm))
    S.op("act", lambda e: e.activation(out=sq[:, :n], in_=pq, func=AF.Square), reads=[kpq], writes=[k("sq")])
    yield
    S.op("dve", lambda e: e.reduce_sum(out=ss[:, :nh], in_=sq[:, :n].rearrange("p (h d) -> p h d", d=64), axis=AX.X), reads=[k("sq")], writes=[k("ss")])
    yield
    S.op("act", lambda e: e.activation(out=ss[:, :nh], in_=ss[:, :nh], func=AF.Sqrt, scale=1.0 / 64, bias=EPS), reads=[k("ss")], writes=[k("ss")])
    yield
    S.op("dve", lambda e: e.reciprocal(out=ss[:, :nh], in_=ss[:, :nh]), reads=[k("ss")], writes=[k("ss")])
    yield
    S.op("dve", lambda e: e.tensor_tensor(out=xn[:, :n].rearrange("p (h d) -> p h d", d=64), in0=pq.rearrange("p (h d) -> p h d", d=64),
                                          in1=ss[:, :nh, None].to_broadcast([128, nh, 64]), op=ALU.mult), reads=[kpq, k("ss")], writes=[k("xn")])
    yield
    if rope:
        S.op("pool", lambda e: e.tensor_tensor(out=xg[:, :n].rearrange("p (h d) -> p h d", d=64), in0=xn[:, :n].rearrange("p (h d) -> p h d", d=64),
                                               in1=gain[:, None, :].to_broadcast([128, nh, 64]), op=ALU.mult), reads=[k("xn"), kgain], writes=[k("xg")])
        yield
        xv = xg[:, :n].rearrange("p (h a f) -> p h a f", a=2, f=32)
        x1, x2 = xv[:, :, :, 0:16], xv[:, :, :, 16:32]
        cb = cs[:, tt, None, :].rearrange("p o (a f) -> p o a f", a=2).to_broadcast([128, nh, 2, 16])
        sb_ = sn[:, tt, None, :].rearrange("p o (a f) -> p o a f", a=2).to_broadcast([128, nh, 2, 16])
        h2 = n // 2
        t1v = t1[:, :h2].rearrange("p (h a f) -> p h a f", a=2, f=16)
        t2v = t2[:, :h2].rearrange("p (h a f) -> p h a f", a=2, f=16)
        qv = qn[:, :n].rearrange("p (h a f) -> p h a f", a=2, f=32)
        S.op("dve", lambda e: e.tensor_tensor(out=t1v, in0=x1, in1=cb, op=ALU.mult), reads=[k("xg"), "rope"], writes=[k("t1")])
        yield
        S.op("pool", lambda e: e.tensor_tensor(out=t2v, in0=x2, in1=sb_, op=ALU.mult), reads=[k("xg"), "rope"], writes=[k("t2")])
        yield
        S.op("dve", lambda e: e.tensor_tensor(out=qv[:, :, :, 0:16], in0=t1v, in1=t2v, op=ALU.subtract), reads=[k("t1"), k("t2")], writes=[k("qn")])
        yield
        S.op("pool", lambda e: e.tensor_tensor(out=t1v, in0=x2, in1=cb, op=ALU.mult), reads=[k("xg"), "rope", k("qn")], writes=[k("t1")])
        yield
        S.op("dve", lambda e: e.tensor_tensor(out=t2v, in0=x1, in1=sb_, op=ALU.mult), reads=[k("xg"), "rope", k("qn")], writes=[k("t2")])
        yield
        S.op("pool", lambda e: e.tensor_tensor(out=qv[:, :, :, 16:32], in0=t1v, in1=t2v, op=ALU.add), reads=[k("t1"), k("t2")], writes=[k("qn")])
        yield
    else:
        S.op("pool", lambda e: e.tensor_tensor(out=qn[:, :n].rearrange("p (h d) -> p h d", d=64), in0=xn[:, :n].rearrange("p (h d) -> p h d", d=64),
                                               in1=gain[:, None, :].to_broadcast([128, nh, 64]), op=ALU.mult), reads=[k("xn"), kgain], writes=[k("qn")])
        yield
    nj = n // 128
    for j in range(nj):
        S.op("pe", lambda e, j=j: e.transpose(out=tp[:, j, :], in_=qn[:, j * 128:(j + 1) * 128], identity=G["ident_b"][:]),
             reads=[k("qn"), "ident_b"], writes=[k("tp")])
    S.op("act", lambda e: e.copy(out=dstT[:, 0:nj, tt * 128:(tt + 1) * 128], in_=tp[:, 0:nj, :]), reads=[k("tp")], writes=[kdst])
    yield


def run_rr(gens):
    gens = list(gens)
    while gens:
        for g_ in list(gens):
            try:
                next(g_)
            except StopIteration:
                gens.remove(g_)

def qk_work(C, tag, tp=None):
    return {"sq": C.sb(tag + "sq", [128, 512], F32), "ss": C.sb(tag + "ss", [128, 8], F32), "xn": C.sb(tag + "xn", [128, 512], F32),
            "xg": C.sb(tag + "xg", [128, 512], F32), "t1": C.sb(tag + "t1", [128, 256], F32), "t2": C.sb(tag + "t2", [128, 256], F32),
            "qn": C.sb(tag + "qn", [128, 512], BF16), "tp": tp if tp is not None else C.ps(tag + "tp", [128, 4, 128], BF16)}


def phase_diff_attn(C, G):
    S = C.S
    aT = G["aT"]
    LAM_INIT = 0.8 - 0.6 * math.exp(0.0)
    with C.phase():
        qT = C.sb("qT", [128, 4, T], BF16)
        kT = C.sb("kT", [128, 4, T], BF16)
        vx = C.sb("vx", [128, NT, 4, 129], BF16)
        gq = C.sb("gq", [128, 64], F32)
        gk = C.sb("gk", [128, 64], F32)
        cs = C.sb("ropec", [128, 16, 32], F32)
        sn = C.sb("ropes", [128, 16, 32], F32)
        sub = C.sb("subln", [128, 128], F32)
        lamb = C.sb("lamb", [128, 4, 64], F32)
        lw = C.sb("lamw", [128, 8], F32)
        S.dma("sp", gq[:], G["da_q_norm"][0:1, :].to_broadcast([128, 64]), writes=["gq"])
        S.dma("sp", gk[:], G["da_k_norm"][0:1, :].to_broadcast([128, 64]), writes=["gk"])
        S.dma("sp", sub[:], G["da_subln"][0:1, :].to_broadcast([128, 128]), writes=["subln"])
        S.dma("sp", lamb[:].rearrange("p a b -> p (a b)"), G["da_lam"].rearrange("o a b -> o (a b)").to_broadcast([128, 256]), writes=["lamb"])
        S.dma("sp", cs[:], G["c_cos"].rearrange("(t p) f -> p t f", p=128), writes=["rope"])
        S.dma("sp", sn[:], G["c_sin"].rearrange("(t p) f -> p t f", p=128), writes=["rope"])
        S.op("dve", lambda e: e.tensor_scalar(out=gq[:], in0=gq[:], scalar1=0.125, scalar2=None, op0=ALU.mult), reads=["gq"], writes=["gq"])
        S.op("dve", lambda e: e.tensor_scalar(out=sub[:], in0=sub[:], scalar1=1.0 - LAM_INIT, scalar2=None, op0=ALU.mult), reads=["subln"], writes=["subln"])
        S.op("dve", lambda e: e.tensor_tensor(out=lamb[:, 0, :], in0=lamb[:, 0, :], in1=lamb[:, 1, :], op=ALU.mult), reads=["lamb"], writes=["lamb"])
        S.op("dve", lambda e: e.tensor_tensor(out=lamb[:, 2, :], in0=lamb[:, 2, :], in1=lamb[:, 3, :], op=ALU.mult), reads=["lamb"], writes=["lamb"])
        S.op("dve", lambda e: e.reduce_sum(out=lw[:, 0:1], in_=lamb[:, 0, :], axis=AX.X), reads=["lamb"], writes=["lamw"])
        S.op("dve", lambda e: e.reduce_sum(out=lw[:, 1:2], in_=lamb[:, 2, :], axis=AX.X), reads=["lamb"], writes=["lamw"])
        S.op("act", lambda e: e.activation(out=lw[:, 2:4], in_=lw[:, 0:2], func=AF.Exp), reads=["lamw"], writes=["lamw"])
        S.op("dve", lambda e: e.tensor_tensor(out=lw[:, 4:5], in0=lw[:, 3:4], in1=lw[:, 2:3], op=ALU.subtract), reads=["lamw"], writes=["lamw"])
        S.op("dve", lambda e: e.tensor_scalar(out=lw[:, 5:6], in0=lw[:, 4:5], scalar1=-LAM_INIT, scalar2=None, op0=ALU.add), reads=["lamw"], writes=["lamw"])
        S.op("pool", lambda e: e.memset(vx[:, :, :, 128:129], 1.0), writes=["vx1"])
        with C.phase():
            w = C.sb("wqkv", [128, 8, 1536], BF16)
            S.dma("pool", w[:], G["e_w_in"][0, :, 1536:3072].rearrange("(k p) n -> p k n", p=128), writes=["wqkv"])
            pq = [C.ps(f"pq{i}", [128, 512], F32) for i in range(2)]
            pk = [C.ps(f"pk{i}", [128, 512], F32) for i in range(2)]
            pv = C.ps("pv", [128, 512], F32)
            tps_ = [C.ps(f"qktp{i}", [128, 4, 128], BF16) for i in range(2)]
            Wq = [qk_work(C, f"wq{i}", tps_[i]) for i in range(2)]
            Wk = [qk_work(C, f"wk{i}", tps_[i]) for i in range(2)]
            for i in range(2):
                Wq[i]["tpkey"] = Wk[i]["tpkey"] = f"qktp{i}"
            for t2 in range(0, NT, 2):
                gens = []
                for tt in (t2, t2 + 1):
                    q_, k_ = pq[tt % 2], pk[tt % 2]
                    for (dst, kd, c0) in ((q_, f"pq{tt % 2}", 0), (k_, f"pk{tt % 2}", 512), (pv, "pv", 1024)):
                        for k in range(8):
                            S.op("pe", lambda e, dst=dst, k=k, c0=c0, tt=tt: e.matmul(dst[:], lhsT=aT[:, k, tt * 128:(tt + 1) * 128], rhs=w[:, k, c0:c0 + 512],
                                                                                 start=(k == 0), stop=(k == 7)),
                                 reads=["wqkv"], writes=[kd])
                    S.op("act", lambda e, tt=tt: e.copy(out=vx[:, tt, :, 0:128], in_=pv[:].rearrange("p (h d) -> p h d", d=128)), reads=["pv"], writes=["vx"])
                    rope = tt < 16
                    gens.append(qk_prep(C, G, q_[:], f"pq{tt % 2}", gq, "gq", cs, sn, tt, rope, qT, "qT", 8, Wq[tt % 2], f"wq{tt % 2}"))
                    gens.append(qk_prep(C, G, k_[:], f"pk{tt % 2}", gk, "gk", cs, sn, tt, rope, kT, "kT", 8, Wk[tt % 2], f"wk{tt % 2}"))
                run_rr(gens)
        if G.get("dbg_qk"):
            for nm, tl in (("qT", qT), ("kT", kT)):
                dd = C.nc.dram_tensor("dbgq_" + nm, [128, 4 * T], BF16, kind="ExternalOutput").ap()
                G.setdefault("final_ops2", []).append(S.dma("sp", dd, tl[:].rearrange("p a b -> p (a b)"), reads=[nm]))
        subc = load_cols(C, G, "subc", [G["da_subln"]], 1)
        S.op("dve", lambda e: e.tensor_scalar(out=subc[:, 0, :], in0=subc[:, 0, :], scalar1=1.0 - LAM_INIT, scalar2=None, op0=ALU.mult),
             reads=["subc"], writes=["subc"])
        with C.phase():
            ones_b = C.sb("ones_b", [128, 128], BF16)
            S.op("pool", lambda e: e.memset(ones_b[:], 1.0), writes=["ones_b"])
            sp = [C.ps(f"sp{i}", [128, 512], F32) for i in range(2)]
            oTp = [C.ps(f"oTp{m}", [128, 512], F32) for m in range(2)]
            dnp = [C.ps(f"dnp{m}", [128, 512], F32) for m in range(2)]
            ssq = C.ps("ssq", [128, 512], F32)
            pt = [C.sb(f"pt{i}", [128, 512], BF16) for i in range(3)]
            rcp = [C.sb(f"rcp{m}", [128, 512], F32) for m in range(2)]
            osb = [[C.sb(f"osb{b}_{m}", [128, 512], F32) for m in range(2)] for b in range(2)]
            sqb = [C.sb(f"sqb{b}", [128, 512], BF16) for b in range(2)]
            rs = [C.sb(f"rs{b}", [128, 512], F32) for b in range(2)]
            oT = [C.sb(f"oT{i}", [128, 512], BF16) for i in range(2)]
            its = []
            for h in range(4):
                for (q0, nq, kts) in ((0, 512, range(NT)), (512, 512, range(NT)), (1024, 512, range(NT)), (1536, 512, range(NT)),
                                      (2048, 256, (16, 17))):
                    kts = list(kts)
                    for m in range(2):
                        for kt in kts:
                            its.append(dict(h=h, q0=q0, nq=nq, m=m, kt=kt, first=(kt == kts[0]), last=(kt == kts[-1])))

            def emit_s(i):
                d_ = its[i]
                s_, ks_ = sp[i % 2], f"sp{i % 2}"
                p_, kp_ = pt[i % 3], f"pt{i % 3}"
                pb = slice(d_["m"] * 64, (d_["m"] + 1) * 64)
                h, kt, q0, nq = d_["h"], d_["kt"], d_["q0"], d_["nq"]
                S.op("pe", lambda e, s_=s_, pb=pb, h=h, kt=kt, q0=q0, nq=nq: e.matmul(
                    s_[:, :nq], lhsT=kT[pb, h, kt * 128:(kt + 1) * 128], rhs=qT[pb, h, q0:q0 + nq], start=True, stop=True),
                    reads=["qT", "kT"], writes=[ks_])
                S.op("act", lambda e, s_=s_, p_=p_, nq=nq: e.activation(out=p_[:, :nq], in_=s_[:, :nq], func=AF.Exp), reads=[ks_], writes=[kp_])

            ib = 0
            emit_s(0)
            for i in range(len(its)):
                if i + 1 < len(its):
                    emit_s(i + 1)
                d_ = its[i]
                h, kt, q0, nq, m = d_["h"], d_["kt"], d_["q0"], d_["nq"], d_["m"]
                p_, kp_ = pt[i % 3], f"pt{i % 3}"
                S.op("pe", lambda e, p_=p_, kt=kt, h=h, m=m, nq=nq, f_=d_["first"], l_=d_["last"]: e.matmul(
                    oTp[m][:, :nq], lhsT=vx[:, kt, h, 0:128], rhs=p_[:, :nq], start=f_, stop=l_),
                    reads=[kp_, "vx"], writes=[f"oTp{m}"])
                S.op("pe", lambda e, p_=p_, m=m, nq=nq, f_=d_["first"], l_=d_["last"]: e.matmul(
                    dnp[m][:, :nq], lhsT=ones_b[:], rhs=p_[:, :nq], start=f_, stop=l_),
                    reads=[kp_, "ones_b"], writes=[f"dnp{m}"])
                if not d_["last"]:
                    continue
                bsel = ib % 2
                o_m, ko_m = osb[bsel][m], f"osb{bsel}_{m}"
                S.op("dve", lambda e, m=m, nq=nq: e.reciprocal(out=rcp[m][:, :nq], in_=dnp[m][:, :nq]), reads=[f"dnp{m}"], writes=[f"rcp{m}"])
                S.op("dve", lambda e, m=m, nq=nq, o_m=o_m: e.tensor_tensor(out=o_m[:, :nq], in0=oTp[m][:, :nq], in1=rcp[m][:, :nq], op=ALU.mult),
                     reads=[f"oTp{m}", f"rcp{m}"], writes=[ko_m])
                if m == 0:
                    continue
                o0, o1 = osb[bsel][0], osb[bsel][1]
                k0, k1 = f"osb{bsel}_0", f"osb{bsel}_1"
                sq_, ksq = sqb[bsel], f"sqb{bsel}"
                r_, kr = rs[bsel], f"rs{bsel}"
                o_T, ko_T = oT[bsel], f"oT{bsel}"
                ib += 1
                S.op("dve", lambda e, o0=o0, o1=o1, nq=nq: e.scalar_tensor_tensor(out=o0[:, :nq], in0=o1[:, :nq], scalar=lw[:, 5:6], in1=o0[:, :nq],
                                                                                op0=ALU.mult, op1=ALU.add),
                     reads=[k0, k1, "lamw"], writes=[k0])
                S.op("pool", lambda e, o0=o0, sq_=sq_, nq=nq: e.tensor_tensor(out=sq_[:, :nq], in0=o0[:, :nq], in1=o0[:, :nq], op=ALU.mult), reads=[k0], writes=[ksq])
                S.op("pe", lambda e, sq_=sq_, nq=nq: e.matmul(ssq[:, :nq], lhsT=ones_b[:], rhs=sq_[:, :nq], start=True, stop=True),
                     reads=[ksq, "ones_b"], writes=["ssq"])
                S.op("act", lambda e, r_=r_, nq=nq: e.activation(out=r_[:, :nq], in_=ssq[:, :nq], func=AF.Sqrt, scale=1.0 / 128, bias=EPS), reads=["ssq"], writes=[kr])
                S.op("dve", lambda e, r_=r_, nq=nq: e.reciprocal(out=r_[:, :nq], in_=r_[:, :nq]), reads=[kr], writes=[kr])
                S.op("dve", lambda e, o0=o0, r_=r_, o_T=o_T, nq=nq: e.scalar_tensor_tensor(out=o_T[:, :nq], in0=o0[:, :nq], scalar=subc[:, 0, :], in1=r_[:, :nq],
                                                                                       op0=ALU.mult, op1=ALU.mult),
                     reads=[k0, kr, "subc"], writes=[ko_T])
                S.dma("sp", G["mix_d"][512 + h * 128:512 + (h + 1) * 128, q0:q0 + nq], o_T[:, :nq], reads=[ko_T], writes=[("mix_d", "a", h, q0)])


def phase_outproj(C, G, l, w_name, ntok):
    S = C.S
    hT, aT, modc = G["hT"], G["aT"], G["modc"]
    with C.phase():
        w = C.sb("wout", [128, 8, D], BF16)
        S.dma("pool", w[:], G[w_name][0].rearrange("(k p) n -> p k n", p=128), writes=["wout"])
        for k in range(8):
            S.dma("sp", aT[:, k, 0:ntok], G["mix_d"][k * 128:(k + 1) * 128, 0:ntok], writes=[("aTm", k)])
        akeys = [("aTm", k) for k in range(8)]
        pp = [C.ps(f"opp{i}", [128, 512], F32) for i in range(4)]
        ip = 0
        for (t0, n, s) in token_blocks():
            if t0 >= ntok:
                continue
            hkeys = [("hT", tt) for tt in range(t0 // 128, (t0 + n) // 128)]
            for j in range(8):
                p, kp = pp[ip % 4], f"opp{ip % 4}"
                ip += 1
                for k in range(8):
                    S.op("pe", lambda e, p=p, j=j, k=k, t0=t0, n=n: e.matmul(p[:, :n], lhsT=w[:, k, j * 128:(j + 1) * 128], rhs=aT[:, k, t0:t0 + n],
                                                                         start=(k == 0), stop=(k == 7)),
                         reads=["wout"] + akeys, writes=[kp])
                S.op("dve", lambda e, p=p, j=j, t0=t0, n=n, s=s: e.scalar_tensor_tensor(
                    out=hT[:, j, t0:t0 + n], in0=p[:, :n], scalar=modc[:, l, 16 + j, s:s + 1], in1=hT[:, j, t0:t0 + n], op0=ALU.mult, op1=ALU.add),
                    reads=[kp, "modc"] + hkeys, writes=hkeys)


def spill_h(C, G, to_dram):
    S = C.S
    with C.phase():
        for k in range(8):
            if to_dram:
                S.dma("sp", G["hT_d"][:, k * T:(k + 1) * T], G["hT"][:, k, :])
            else:
                S.dma("sp", G["hT"][:, k, :], G["hT_d"][:, k * T:(k + 1) * T])


def build_program(stages=("load", "mods", "l0mix", "l0moe", "l1mix", "l1moe", "store"), debug=False, dbg_moe=None, EG=2, odd_parts="rg", l0parts="iafc"):
    nc = bass.Bass("TRN2", target_bir_lowering=False)
    C = Ctx(nc)
    G = {"dbg_moe": dbg_moe, "EG": EG, "dbg_qk": bool(debug), "odd_parts": odd_parts, "l0parts": l0parts}
    dbgk = {"kind": "ExternalOutput"} if debug else {}

    def din(name, shape, dtype=F32):
        G[name] = nc.dram_tensor(name, list(shape), dtype, kind="ExternalInput").ap()

    def dscr(name, shape, dtype):
        G[name] = nc.dram_tensor(name, list(shape), dtype, **dbgk).ap()

    din("x", [SEQ, D]); din("ctx", [CTX, D]); din("c2", [2, D])
    din("ada_w", [2, D, 6 * D]); din("ada_b", [2, 6 * D])
    din("e_w_in", [1, D, 3072]); din("e_w_out", [1, D, D])
    din("hy_conv_w", [1, 3, 1536]); din("hy_conv_b", [1, 1536])
    din("hy_f_w1", [1, 33, 64]); din("hy_f_b1", [1, 64]); din("hy_f_w2", [1, 64, 64]); din("hy_f_b2", [1, 64])
    din("hy_f_w3", [1, 64, 2048]); din("hy_f_freq", [1, 2, 64]); din("hy_bias", [1, 2, 512])
    din("da_q_norm", [1, 64]); din("da_k_norm", [1, 64]); din("da_lam", [1, 4, 64]); din("da_subln", [1, 128])
    din("o_w_in", [1, D, 2816]); din("o_w_out", [1, D, D])
    din("ret_decay", [1, 2, 4]); din("ret_gn", [1, 512]); din("gq_q_norm", [1, 64]); din("gq_k_norm", [1, 64]); din("gq_sink", [1, 8])
    din("moe_w_grp", [2, D, 4]); din("moe_b_grp", [2, 4]); din("moe_w_rt", [2, D, 32]); din("moe_b_rt", [2, 32])
    din("moe_w_gate", [2, 4, 8, D, DEXP]); din("moe_w_up", [2, 4, 8, D, DEXP]); din("moe_w_down", [2, 4, 8, DEXP, D])
    for name, (shape, dtype) in CONST_SPECS.items():
        din(name, shape, dtype)
    G["out"] = nc.dram_tensor("out", [SEQ, D], F32, kind="ExternalOutput").ap()
    dscr("combT_d", [NEXP, T], BF16)
    dscr("hyc_d", [1536, T], BF16)
    dscr("vtok_d", [T, 512], BF16)
    dscr("filt_d", [2, 2, SEQ, 512], BF16)
    dscr("filtc_d", [2, 2, CTX, 512], BF16)
    dscr("z1_d", [512, T], BF16)
    dscr("mix_d", [D, T], BF16)
    dscr("hT_d", [128, 8 * T], F32)

    S = C.S
    with contextlib.ExitStack() as st0:
        C.stack = st0
        G["modc"] = C.sb("modc", [128, 2, 48, 2], F32)
        G["ident_f"] = C.sb("ident_f", [128, 128], F32)
        G["ident_b"] = C.sb("ident_b", [128, 128], BF16)
        G["ones_f"] = C.sb("ones_f", [128, 128], F32)
        S.dma("sp", G["ident_f"][:], G["c_ident_f"], writes=["ident_f"])
        S.dma("sp", G["ident_b"][:], G["c_ident_b"], writes=["ident_b"])
        S.op("pool", lambda e: e.memset(G["ones_f"][:], 1.0), writes=["ones_f"])

        def open_scopes():
            stA = contextlib.ExitStack()
            C.stack = stA
            G["aT"] = C.sb("aT", [128, 8, T], BF16)
            stH = contextlib.ExitStack()
            C.stack = stH
            G["hT"] = C.sb("hT", [128, 8, T], F32)
            return stA, stH

        l0mix = "l0mix" in stages
        l1mix = "l1mix" in stages
        stA, stH = open_scopes()
        if "mods" in stages:
            phase_mods(C, G, mid=(lambda: phase_load(C, G)) if "load" in stages else None)
        elif "load" in stages:
            phase_load(C, G)
        if l0mix:
            phase_norm(C, G, 0, 0)
        spill_h(C, G, True)
        stH.close()
        C.stack = stA
        lp = G.get("l0parts", "iafc")
        if l0mix:
            if "i" in lp:
                phase_hy_inproj(C, G)
            if "a" in lp:
                phase_diff_attn(C, G)
            if "f" in lp:
                phase_hy_filter(C, G, SEQ, "c_zemb_2048", "filt_d")
                phase_hy_filter(C, G, CTX, "c_zemb_256", "filtc_d")
        S.barrier()
        stA.close()
        if l0mix and "c" in lp:
            C.stack = st0
            phase_hyena_conv(C, G, SEQ, 0, "filt_d", "c_F2048", "c_G2048", "vtok_d")
            phase_hyena_conv(C, G, CTX, SEQ, "filtc_d", "c_F256", "c_G256", "vtok_d")
        stA, stH = open_scopes()
        spill_h(C, G, False)
        if "dbgnorm" in stages:
            phase_norm(C, G, 0, 0)
        if l0mix:
            phase_outproj(C, G, 0, "e_w_out", T)
        if "l0moe" in stages:
            phase_norm(C, G, 0, 1, router=True)
            phase_moe(C, G, 0, T)
        fin = []
        dbg_done = False

        def dbg_dump():
            if debug:
                for name in debug:
                    t = G[name]
                    shp = list(t.shape)
                    dd = nc.dram_tensor("dbg_" + name, [shp[0], int(np.prod(shp[1:]))], t.dtype, kind="ExternalOutput").ap()
                    src = t[:]
                    if len(shp) == 3:
                        src = src.rearrange("p a b -> p (a b)")
                    elif len(shp) == 4:
                        src = src.rearrange("p a b c -> p (a b c)")
                    fin.append(S.dma("sp", dd, src))
                S.barrier()

        if l1mix:
            phase_norm(C, G, 1, 0)
            spill_h(C, G, True)
            stH.close()
            C.stack = stA
            phase_odd_mixer(C, G)
            S.barrier()
            stA.close()
            stA, stH = open_scopes()
            spill_h(C, G, False)
            phase_outproj(C, G, 1, "o_w_out", SEQ)
        if "l1moe" in stages:
            phase_norm(C, G, 1, 1, router=True, ntok=SEQ)
            phase_moe(C, G, 1, SEQ)
        if "store" in stages:
            phase_store(C, G)
        dbg_dump()
        fin = list(G.get("final_ops", ())) + list(G.get("final_ops2", ())) + fin
        S.barrier()
        stH.close()
        stA.close()
        counts = S.emit(final_wait_ops=fin)
    return nc, counts


def _dft_consts(L):
    N = 2 * L
    k = np.arange(L, dtype=np.float64)
    s = np.arange(L, dtype=np.float64)
    th = 2.0 * np.pi * (k + 0.5) / N
    ang = np.outer(s, th)
    F = np.concatenate([np.cos(ang), -np.sin(ang)], axis=1)
    nsc = L // 128
    Ft = F.reshape(nsc, 128, 2 * nsc, 128).transpose(2, 1, 0, 3)
    Gm = F.T / L
    tb = min(L, 512)
    ntb = L // tb
    KG = 4
    ng = 2 * nsc // KG
    Gt = Gm.reshape(ng, KG, 128, ntb, tb).transpose(3, 0, 2, 1, 4)
    return np.ascontiguousarray(Ft).astype(ml_dtypes.bfloat16), np.ascontiguousarray(Gt).astype(ml_dtypes.bfloat16)


def _zemb(L):
    t = np.linspace(0.0, 1.0, L)[:, None]
    w = (2.0 * np.pi / L) * np.arange(L)[:, None]
    fb = np.linspace(1e-4, 15.0, 16)[None, :]
    z = np.concatenate([t, np.cos(fb * w), -np.sin(fb * w)], axis=-1)
    return np.ascontiguousarray(z.T).astype(np.float32)


def _negt(L):
    t = np.linspace(0.0, 1.0, L)
    return np.ascontiguousarray(-t.reshape(L // 128, 128).T).astype(np.float32)


CONST_SPECS = {
    "c_ident_f": ([128, 128], F32), "c_ident_b": ([128, 128], BF16),
    "c_cos": ([SEQ, 32], F32), "c_sin": ([SEQ, 32], F32),
    "c_delta": ([1, 512], F32), "c_negt_2048": ([128, 16], F32), "c_negt_256": ([128, 2], F32), "c_mask0": ([128, 1], F32),
    "c_zemb_2048": ([33, SEQ], F32), "c_zemb_256": ([33, CTX], F32),
    "c_F2048": ([32, 128, 16, 128], BF16), "c_G2048": ([4, 8, 128, 4, 512], BF16),
    "c_F256": ([4, 128, 2, 128], BF16), "c_G256": ([1, 1, 128, 4, 256], BF16),
    "c_cos_rt": ([SEQ, 64], F32), "c_sin_rt": ([SEQ, 64], F32), "c_ret": ([5, 128, 128], F32), "c_retcol": ([128, 2], F32),
    "c_gmask": ([2, 128, 128], BF16),
}
_CONSTS = None


def host_constants():
    global _CONSTS
    if _CONSTS is not None:
        return _CONSTS
    c = {"c_ident_f": np.eye(128, dtype=np.float32), "c_ident_b": np.eye(128).astype(ml_dtypes.bfloat16)}
    tok = np.arange(SEQ)
    inv = 10000.0 ** (-np.arange(16, dtype=np.float64) / 16)
    ang = np.concatenate([(tok // 64)[:, None] * inv[None, :], (tok % 64)[:, None] * inv[None, :]], axis=1)
    c["c_cos"] = np.cos(ang).astype(np.float32)
    c["c_sin"] = np.sin(ang).astype(np.float32)
    mx, mn = math.log(1e-2) / 0.3, math.log(1e-2) / 1.5
    c["c_delta"] = np.abs(np.linspace(mn, mx, 512)).astype(np.float32)[None, :]
    c["c_negt_2048"] = _negt(SEQ)
    c["c_negt_256"] = _negt(CTX)
    m0 = np.ones((128, 1), np.float32); m0[0, 0] = 0.0
    c["c_mask0"] = m0
    c["c_zemb_2048"] = _zemb(SEQ)
    c["c_zemb_256"] = _zemb(CTX)
    c["c_F2048"], c["c_G2048"] = _dft_consts(SEQ)
    c["c_F256"], c["c_G256"] = _dft_consts(CTX)
    inv_rt = 1.0 / (10000.0 ** np.linspace(0.0, 1.0, 64))
    ang_rt = np.arange(SEQ, dtype=np.float64)[:, None] * inv_rt[None, :]
    c["c_cos_rt"] = np.cos(ang_rt).astype(np.float32)
    c["c_sin_rt"] = np.sin(ang_rt).astype(np.float32)
    j = np.arange(128)[:, None]; i = np.arange(128)[None, :]
    relf = np.maximum(i - j, 0); maskf = (i >= j)
    relb = np.maximum(j - i, 0); maskb = (j >= i)
    iota1 = np.broadcast_to(np.arange(1, 129)[None, :], (128, 128))
    c["c_ret"] = np.stack([relf, maskf, relb, maskb, iota1]).astype(np.float32)
    c["c_retcol"] = np.stack([127 - np.arange(128), np.arange(128)], axis=1).astype(np.float32)
    c["c_gmask"] = np.stack([(i <= j), (j <= i)]).astype(ml_dtypes.bfloat16)
    _CONSTS = c
    return c


WEIGHT_KEYS = ["ada_w", "ada_b", "e_w_in", "e_w_out", "hy_conv_w", "hy_conv_b", "hy_f_w1", "hy_f_b1", "hy_f_w2", "hy_f_b2", "hy_f_w3",
               "hy_f_freq", "hy_bias", "da_q_norm", "da_k_norm", "da_lam", "da_subln", "o_w_in", "o_w_out", "ret_decay", "ret_gn",
               "gq_q_norm", "gq_k_norm", "gq_sink", "moe_w_grp", "moe_b_grp", "moe_w_rt", "moe_b_rt", "moe_w_gate", "moe_w_up", "moe_w_down"]


def make_in_maps(inputs, ncores=8):
    consts = host_constants()
    maps = []
    for b in range(ncores):
        m = {"x": np.ascontiguousarray(inputs["x"][b]), "ctx": np.ascontiguousarray(inputs["ctx"][b]),
             "c2": np.ascontiguousarray(np.stack([inputs["c"][b], inputs["c_ctx"]], axis=0))}
        for k in WEIGHT_KEYS:
            m[k] = np.ascontiguousarray(inputs[k])
        m.update(consts)
        maps.append(m)
    return maps


def kernel(**inputs):
    inputs = {k: np.asarray(v) for k, v in inputs.items()}
    nc, _ = build_program()
    in_maps = make_in_maps(inputs, 8)
    res = run_bass_kernel_spmd(nc, in_maps, core_ids=list(range(8)))
    return np.stack([np.asarray(r["out"]) for r in res.results], axis=0).astype(np.float32)


def phase_odd_mixer(C, G):
    parts = G.get("odd_parts", "rg")
    if "r" in parts:
        phase_retention(C, G)
    if "g" in parts:
        phase_gqa(C, G)


def phase_retention(C, G):
    S = C.S
    aT = G["aT"]
    RS = 128 ** -0.5
    with C.phase():
        qT = C.sb("rqT", [128, 4, SEQ], BF16)
        kT = C.sb("rkT", [128, 4, T], BF16)
        ktok = C.sb("rktok", [128, NT, 4, 128], BF16)
        vv = C.sb("rv", [128, NT, 512], BF16)
        sg = C.sb("rsg", [128, 16, 512], BF16)
        with C.phase():
            w = C.sb("wret", [128, 8, 2048], BF16)
            S.dma("pool", w[:], G["o_w_in"][0, :, 0:2048].rearrange("(k p) n -> p k n", p=128), writes=["wret"])
            cs = C.sb("rtc", [128, 16, 64], F32)
            sn = C.sb("rts", [128, 16, 64], F32)
            S.dma("sp", cs[:], G["c_cos_rt"].rearrange("(t p) f -> p t f", p=128), writes=["rtrope"])
            S.dma("sp", sn[:], G["c_sin_rt"].rearrange("(t p) f -> p t f", p=128), writes=["rtrope"])
            pp = [C.ps(f"rpp{i}", [128, 512], F32) for i in range(4)]
            tp = [C.ps(f"rtp{i}", [128, 4, 128], BF16) for i in range(2)]
            xs = [[C.sb(f"rxs{p}{i}", [128, 512], F32) for i in range(2)] for p in range(2)]
            t1 = [[C.sb(f"rt1{p}{i}", [128, 256], F32) for i in range(2)] for p in range(2)]
            t2 = [[C.sb(f"rt2{p}{i}", [128, 256], F32) for i in range(2)] for p in range(2)]
            qn = [C.sb(f"rqn{p}", [128, 512], BF16) for p in range(2)]

            def chain(tt, which):
                lat = tt < 16
                p = tt % 2
                x_, kx = xs[p][which], f"rxs{p}{which}"
                a_, ka = t1[p][which], f"rt1{p}{which}"
                b_, kb = t2[p][which], f"rt2{p}{which}"
                dst = qn[p][:] if which == 0 else ktok[:, tt].rearrange("p h d -> p (h d)")
                kdst = f"rqn{p}" if which == 0 else ("rktok", tt)
                if lat:
                    S.op("act", lambda e: e.activation(out=x_[:], in_=pp[which][:], func=AF.Identity, scale=(1.0 if which == 0 else RS)),
                         reads=[f"rpp{which}"], writes=[kx])
                    yield
                    xv = x_[:].rearrange("p (h a f) -> p h a f", a=2, f=64)
                    x1, x2 = xv[:, :, 0, :], xv[:, :, 1, :]
                    cb = cs[:, tt, None, :].to_broadcast([128, 4, 64])
                    sb_ = sn[:, tt, None, :].to_broadcast([128, 4, 64])
                    av = a_[:].rearrange("p (h f) -> p h f", f=64)
                    bv = b_[:].rearrange("p (h f) -> p h f", f=64)
                    dv_ = dst.rearrange("p (h a f) -> p h a f", a=2, f=64)
                    S.op("dve", lambda e: e.tensor_tensor(out=av, in0=x1, in1=cb, op=ALU.mult), reads=[kx, "rtrope"], writes=[ka])
                    yield
                    S.op("pool", lambda e: e.tensor_tensor(out=bv, in0=x2, in1=sb_, op=ALU.mult), reads=[kx, "rtrope"], writes=[kb])
                    yield
                    S.op("dve", lambda e: e.tensor_tensor(out=dv_[:, :, 0, :], in0=av, in1=bv, op=ALU.subtract), reads=[ka, kb], writes=[kdst])
                    yield
                    S.op("pool", lambda e: e.tensor_tensor(out=av, in0=x2, in1=cb, op=ALU.mult), reads=[kx, "rtrope"], writes=[ka])
                    yield
                    S.op("dve", lambda e: e.tensor_tensor(out=bv, in0=x1, in1=sb_, op=ALU.mult), reads=[kx, "rtrope"], writes=[kb])
                    yield
                    S.op("pool", lambda e: e.tensor_tensor(out=dv_[:, :, 1, :], in0=av, in1=bv, op=ALU.add), reads=[ka, kb], writes=[kdst])
                    yield
                else:
                    S.op("act", lambda e: e.activation(out=dst, in_=pp[1][:], func=AF.Identity, scale=RS), reads=["rpp1"], writes=[kdst])
                    yield
                tp_, ktp = tp[which], f"rtp{which}"
                for j in range(4):
                    S.op("pe", lambda e, j=j: e.transpose(out=tp_[:, j, :], in_=dst[:, j * 128:(j + 1) * 128], identity=G["ident_b"][:]),
                         reads=[kdst, "ident_b"], writes=[ktp])
                dT, kdT = (qT, "rqT") if which == 0 else (kT, "rkT")
                S.op("act", lambda e: e.copy(out=dT[:, :, tt * 128:(tt + 1) * 128], in_=tp_[:]), reads=[ktp], writes=[kdT])
                yield

            for t2_ in range(0, NT, 2):
                gens = []
                for tt in (t2_, t2_ + 1):
                    lat = tt < 16
                    cols = ((0, 0), (1, 512), (2, 1024), (3, 1536)) if lat else ((1, 512), (2, 1024))
                    for (pi, c0) in cols:
                        for k in range(8):
                            S.op("pe", lambda e, pi=pi, c0=c0, k=k, tt=tt: e.matmul(pp[pi][:], lhsT=aT[:, k, tt * 128:(tt + 1) * 128], rhs=w[:, k, c0:c0 + 512],
                                                                               start=(k == 0), stop=(k == 7)),
                                 reads=["wret"], writes=[f"rpp{pi}"])
                    S.op("act", lambda e, tt=tt: e.copy(out=vv[:, tt, :], in_=pp[2][:]), reads=["rpp2"], writes=["rv"])
                    if lat:
                        S.op("act", lambda e, tt=tt: e.activation(out=sg[:, tt, :], in_=pp[3][:], func=AF.Silu), reads=["rpp3"], writes=["rsg"])
                    for which in ((0, 1) if lat else (1,)):
                        g_ = chain(tt, which)
                        next(g_)
                        gens.append(g_)
                run_rr(gens)
        with C.phase():
            lg = C.sb("rlg", [128, 8], F32)
            S.dma("sp", lg[:], G["ret_decay"].rearrange("o a b -> o (a b)").to_broadcast([128, 8]), writes=["rlg"])
            S.op("act", lambda e: e.activation(out=lg[:], in_=lg[:], func=AF.Exp, scale=-1.0), reads=["rlg"], writes=["rlg"])
            S.op("act", lambda e: e.activation(out=lg[:], in_=lg[:], func=AF.Ln, bias=1.0), reads=["rlg"], writes=["rlg"])
            S.op("dve", lambda e: e.tensor_scalar(out=lg[:], in0=lg[:], scalar1=-1.0, scalar2=None, op0=ALU.mult), reads=["rlg"], writes=["rlg"])
            cst = C.sb("rcst", [128, 5, 128], F32)
            S.dma("sp", cst[:], G["c_ret"].rearrange("a p f -> p a f"), writes=["rcst"])
            ccol = C.sb("rccol", [128, 2], F32)
            S.dma("sp", ccol[:], G["c_retcol"], writes=["rccol"])
            DT = C.sb("rDT", [128, 8, 128], F32)
            XI = C.sb("rXI", [128, 8, 128], F32)
            zc = C.sb("rzc", [128, 8], F32)
            g128 = C.sb("rg128", [128, 8], F32)
            xib = C.sb("rxib", [128, 128], F32)
            S.op("dve", lambda e: e.tensor_scalar(out=xib[:], in0=cst[:, 4, :], scalar1=-1.0, scalar2=129.0, op0=ALU.mult, op1=ALU.add), reads=["rcst"], writes=["rxib"])
            for dr in range(2):
                for h in range(4):
                    c = dr * 4 + h
                    S.op("act", lambda e, c=c, dr=dr: e.activation(out=DT[:, c, :], in_=cst[:, 2 * dr, :], func=AF.Exp, scale=lg[:, c:c + 1]),
                         reads=["rlg", "rcst"], writes=["rDT"])
                    S.op("dve", lambda e, c=c, dr=dr: e.tensor_tensor(out=DT[:, c, :], in0=DT[:, c, :], in1=cst[:, 2 * dr + 1, :], op=ALU.mult),
                         reads=["rDT", "rcst"], writes=["rDT"])
                    src = cst[:, 4, :] if dr == 0 else xib[:]
                    S.op("act", lambda e, c=c, src=src: e.activation(out=XI[:, c, :], in_=src, func=AF.Exp, scale=lg[:, c:c + 1]),
                         reads=["rlg", "rcst", "rxib"], writes=["rXI"])
                    S.op("act", lambda e, c=c, dr=dr: e.activation(out=zc[:, c:c + 1], in_=ccol[:, dr:dr + 1], func=AF.Exp, scale=lg[:, c:c + 1]),
                         reads=["rlg", "rccol"], writes=["rzc"])
            S.op("act", lambda e: e.activation(out=g128[:], in_=lg[:], func=AF.Exp, scale=128.0), reads=["rlg"], writes=["rg128"])
            oall = C.sb("roall", [128, 16, 512], F32)
            Sf = [C.sb(f"rSf{c}", [128, 128], F32) for c in range(8)]
            Sb = [[C.sb(f"rSb{c}_{i}", [128, 128], BF16) for i in range(2)] for c in range(8)]
            ap_ = [C.ps(f"rap{i}", [128, 128], F32) for i in range(3)]
            op_ = [C.ps(f"rop{i}", [128, 128], F32) for i in range(2)]
            up_ = [C.ps(f"rup{i}", [128, 128], F32) for i in range(2)]
            At = [C.sb(f"rAt{i}", [128, 128], BF16) for i in range(8)]
            qx = [C.sb(f"rqx{i}", [128, 128], BF16) for i in range(8)]
            kz = [C.sb(f"rkz{i}", [128, 128], BF16) for i in range(8)]
            for c in range(8):
                S.op("pool", lambda e, c=c: e.memset(Sf[c][:], 0.0), writes=[f"rSf{c}"])
                S.op("pool", lambda e, c=c: e.memset(Sb[c][0][:], 0.0), writes=[f"rSb{c}_0"])
            orders = {0: [16, 17] + list(range(16)), 1: [17, 16] + list(range(15, -1, -1))}
            ia = 0
            io = 0
            for step in range(18):
                chains = [(dr, h) for dr in range(2) for h in range(4)]
                for (dr, h) in chains:
                    ch = orders[dr][step]
                    c = dr * 4 + h
                    if ch < 16:
                        a3 = ia % 3
                        ia += 1
                        S.op("pe", lambda e, a3=a3, h=h, ch=ch: e.matmul(ap_[a3][:], lhsT=kT[:, h, ch * 128:(ch + 1) * 128], rhs=qT[:, h, ch * 128:(ch + 1) * 128],
                                                                   start=True, stop=True), reads=["rkT", "rqT"], writes=[f"rap{a3}"])
                        S.op("dve", lambda e, a3=a3, c=c: e.tensor_tensor(out=At[c][:], in0=ap_[a3][:], in1=DT[:, c, :], op=ALU.mult),
                             reads=[f"rap{a3}", "rDT"], writes=[f"rAt{c}"])
                        S.op("pool", lambda e, c=c, h=h, ch=ch: e.tensor_tensor(out=qx[c][:], in0=qT[:, h, ch * 128:(ch + 1) * 128], in1=XI[:, c, :], op=ALU.mult),
                             reads=["rqT", "rXI"], writes=[f"rqx{c}"])
                    if step < 17:
                        S.op("pool", lambda e, ch=ch, h=h, c=c: e.tensor_scalar(out=kz[c][:], in0=ktok[:, ch, h, :], scalar1=zc[:, c:c + 1], scalar2=None, op0=ALU.mult),
                             reads=[("rktok", ch), "rzc"], writes=[f"rkz{c}"])
                for (dr, h) in chains:
                    ch = orders[dr][step]
                    c = dr * 4 + h
                    cur, nxt = Sb[c][step % 2], Sb[c][(step + 1) % 2]
                    kcur, knxt = f"rSb{c}_{step % 2}", f"rSb{c}_{(step + 1) % 2}"
                    i2 = io % 2
                    io += 1
                    if ch < 16:
                        S.op("pe", lambda e, i2=i2, c=c, h=h, ch=ch: e.matmul(op_[i2][:], lhsT=At[c][:], rhs=vv[:, ch, h * 128:(h + 1) * 128], start=True, stop=False),
                             reads=[f"rAt{c}", "rv"], writes=[f"rop{i2}"])
                        S.op("pe", lambda e, i2=i2, c=c, cur=cur: e.matmul(op_[i2][:], lhsT=qx[c][:], rhs=cur[:], start=False, stop=True),
                             reads=[f"rqx{c}", kcur], writes=[f"rop{i2}"])
                        first = (ch + 2 < 17 - ch) if dr == 0 else (17 - ch < ch + 2)
                        if first:
                            S.op("act", lambda e, i2=i2, h=h, ch=ch: e.copy(out=oall[:, ch, h * 128:(h + 1) * 128], in_=op_[i2][:]),
                                 reads=[f"rop{i2}"], writes=[("roall", ch, h)])
                        else:
                            S.op("dve", lambda e, i2=i2, h=h, ch=ch: e.tensor_tensor(out=oall[:, ch, h * 128:(h + 1) * 128], in0=op_[i2][:],
                                                                                   in1=oall[:, ch, h * 128:(h + 1) * 128], op=ALU.add),
                                 reads=[f"rop{i2}", ("roall", ch, h)], writes=[("roall", ch, h)])
                    if step < 17:
                        S.op("pe", lambda e, i2=i2, c=c, ch=ch, h=h: e.matmul(up_[i2][:], lhsT=kz[c][:], rhs=vv[:, ch, h * 128:(h + 1) * 128], start=True, stop=True),
                             reads=[f"rkz{c}", "rv"], writes=[f"rup{i2}"])
                        S.op("dve", lambda e, i2=i2, c=c: e.scalar_tensor_tensor(out=Sf[c][:], in0=Sf[c][:], scalar=g128[:, c:c + 1], in1=up_[i2][:], op0=ALU.mult, op1=ALU.add),
                             reads=[f"rup{i2}", f"rSf{c}", "rg128"], writes=[f"rSf{c}"])
                        S.op("act", lambda e, c=c, nxt=nxt: e.copy(out=nxt[:], in_=Sf[c][:]), reads=[f"rSf{c}"], writes=[knxt])
            gn = C.sb("rgn", [128, 512], F32)
            S.dma("sp", gn[:], G["ret_gn"][0:1, :].to_broadcast([128, 512]), writes=["rgn"])
            st2 = [C.sb(f"rst{i}", [128, 8], F32) for i in range(2)]
            xc2 = [C.sb(f"rxc{i}", [128, 512], F32) for i in range(2)]
            sq2 = [C.sb(f"rsq{i}", [128, 512], F32) for i in range(2)]
            yb = [C.sb(f"ryb{i}", [128, 512], BF16) for i in range(2)]
            yT = [C.sb(f"ryT{i}", [128, 4, 128], BF16) for i in range(2)]
            tpo = C.ps("rtpo", [128, 4, 128], BF16)

            def gn_chain(ch):
                p = ch % 2
                st, xc, sq = st2[p], xc2[p], sq2[p]
                kst, kxc, ksq = f"rst{p}", f"rxc{p}", f"rsq{p}"
                ok = [("roall", ch, h) for h in range(4)]
                o3 = oall[:, ch, :].rearrange("p (h d) -> p h d", d=128)
                y_, ky = yb[p], f"ryb{p}"
                t_, kt_ = yT[p], f"ryT{p}"
                S.op("dve", lambda e: e.reduce_sum(out=st[:, 0:4], in_=o3, axis=AX.X), reads=ok, writes=[kst])
                yield
                S.op("dve", lambda e: e.tensor_scalar(out=st[:, 0:4], in0=st[:, 0:4], scalar1=-1.0 / 128, scalar2=None, op0=ALU.mult), reads=[kst], writes=[kst])
                yield
                S.op("dve", lambda e: e.tensor_tensor(out=xc[:].rearrange("p (h d) -> p h d", d=128), in0=o3,
                                                      in1=st[:, 0:4, None].to_broadcast([128, 4, 128]), op=ALU.add), reads=ok + [kst], writes=[kxc])
                yield
                S.op("act", lambda e: e.activation(out=sq[:], in_=xc[:], func=AF.Square), reads=[kxc], writes=[ksq])
                yield
                S.op("dve", lambda e: e.reduce_sum(out=st[:, 4:8], in_=sq[:].rearrange("p (h d) -> p h d", d=128), axis=AX.X), reads=[ksq], writes=[kst])
                yield
                S.op("act", lambda e: e.activation(out=st[:, 4:8], in_=st[:, 4:8], func=AF.Sqrt, scale=1.0 / 128, bias=EPS), reads=[kst], writes=[kst])
                yield
                S.op("dve", lambda e: e.reciprocal(out=st[:, 4:8], in_=st[:, 4:8]), reads=[kst], writes=[kst])
                yield
                S.op("dve", lambda e: e.tensor_tensor(out=xc[:].rearrange("p (h d) -> p h d", d=128), in0=xc[:].rearrange("p (h d) -> p h d", d=128),
                                                      in1=st[:, 4:8, None].to_broadcast([128, 4, 128]), op=ALU.mult), reads=[kxc, kst], writes=[kxc])
                yield
                S.op("pool", lambda e: e.tensor_tensor(out=xc[:], in0=xc[:], in1=gn[:], op=ALU.mult), reads=[kxc, "rgn"], writes=[kxc])
                yield
                S.op("pool", lambda e: e.tensor_tensor(out=y_[:], in0=xc[:], in1=sg[:, ch, :], op=ALU.mult), reads=[kxc, "rsg"], writes=[ky])
                yield
                for j in range(4):
                    S.op("pe", lambda e, j=j: e.transpose(out=tpo[:, j, :], in_=y_[:, j * 128:(j + 1) * 128], identity=G["ident_b"][:]),
                         reads=[ky, "ident_b"], writes=["rtpo"])
                S.op("act", lambda e: e.copy(out=t_[:], in_=tpo[:]), reads=["rtpo"], writes=[kt_])
                S.dma("sp", G["mix_d"][0:512, ch * 128:(ch + 1) * 128].rearrange("(j p) t -> p j t", p=128), t_[:], reads=[kt_], writes=[("mix_d", "r", ch)])
                yield

            for c2 in range(0, 16, 2):
                run_rr([gn_chain(c2), gn_chain(c2 + 1)])


def phase_gqa(C, G):
    S = C.S
    aT = G["aT"]
    with C.phase():
        qT = C.sb("gqT", [128, 4, SEQ], BF16)
        kT = C.sb("gkT", [128, 2, T], BF16)
        vx = C.sb("gvx", [128, NT, 2, 65], BF16)
        gq = C.sb("ggq", [128, 64], F32)
        gk = C.sb("ggk", [128, 64], F32)
        cs = C.sb("gropec", [128, 16, 32], F32)
        sn = C.sb("gropes", [128, 16, 32], F32)
        sk = C.sb("gsink", [128, 8], F32)
        msk = C.sb("gmask", [128, 2, 128], BF16)
        S.dma("sp", gq[:], G["gq_q_norm"][0:1, :].to_broadcast([128, 64]), writes=["ggq"])
        S.dma("sp", gk[:], G["gq_k_norm"][0:1, :].to_broadcast([128, 64]), writes=["ggk"])
        S.dma("sp", sk[:], G["gq_sink"][0:1, :].to_broadcast([128, 8]), writes=["gsink"])
        S.dma("sp", cs[:], G["c_cos"].rearrange("(t p) f -> p t f", p=128), writes=["rope"])
        S.dma("sp", sn[:], G["c_sin"].rearrange("(t p) f -> p t f", p=128), writes=["rope"])
        S.dma("sp", msk[:], G["c_gmask"].rearrange("a p f -> p a f"), writes=["gmask"])
        S.op("dve", lambda e: e.tensor_scalar(out=gq[:], in0=gq[:], scalar1=0.125, scalar2=None, op0=ALU.mult), reads=["ggq"], writes=["ggq"])
        S.op("act", lambda e: e.activation(out=sk[:], in_=sk[:], func=AF.Exp), reads=["gsink"], writes=["gsink"])
        S.op("pool", lambda e: e.memset(vx[:, :, :, 64:65], 1.0), writes=["gvx1"])
        with C.phase():
            w = C.sb("wgqa", [128, 8, 896], BF16)
            base = 2048
            S.dma("pool", w[:, :, 0:512], G["o_w_in"][0, :, base:base + 512].rearrange("(k p) n -> p k n", p=128), writes=["wgqa0"])
            for i, kv in enumerate((0, 0, 1, 1)):
                S.dma("pool", w[:, :, 512 + i * 64:512 + (i + 1) * 64],
                      G["o_w_in"][0, :, base + 512 + kv * 64:base + 512 + (kv + 1) * 64].rearrange("(k p) n -> p k n", p=128), writes=[f"wgqa1{i}"])
            S.dma("pool", w[:, :, 768:896], G["o_w_in"][0, :, base + 640:base + 768].rearrange("(k p) n -> p k n", p=128), writes=["wgqa2"])
            wk_ = ["wgqa0", "wgqa10", "wgqa11", "wgqa12", "wgqa13", "wgqa2"]
            pq = [C.ps(f"gpq{i}", [128, 512], F32) for i in range(2)]
            pk = [C.ps(f"gpk{i}", [128, 512], F32) for i in range(2)]
            tps_ = [C.ps(f"gqktp{i}", [128, 4, 128], BF16) for i in range(2)]
            Wq = [qk_work(C, f"gwq{i}", tps_[i]) for i in range(2)]
            Wk = [qk_work(C, f"gwk{i}", tps_[i]) for i in range(2)]
            for i in range(2):
                Wq[i]["tpkey"] = Wk[i]["tpkey"] = f"gqktp{i}"
            for t2 in range(0, NT, 2):
                gens = []
                for tt in (t2, t2 + 1):
                    lat = tt < 16
                    q_, k_ = pq[tt % 2], pk[tt % 2]
                    if lat:
                        for k in range(8):
                            S.op("pe", lambda e, q_=q_, k=k, tt=tt: e.matmul(q_[:], lhsT=aT[:, k, tt * 128:(tt + 1) * 128], rhs=w[:, k, 0:512], start=(k == 0), stop=(k == 7)),
                                 reads=wk_, writes=[f"gpq{tt % 2}"])
                    for k in range(8):
                        S.op("pe", lambda e, k_=k_, k=k, tt=tt: e.matmul(k_[:, 0:384], lhsT=aT[:, k, tt * 128:(tt + 1) * 128], rhs=w[:, k, 512:896], start=(k == 0), stop=(k == 7)),
                             reads=wk_, writes=[f"gpk{tt % 2}"])
                    S.op("act", lambda e, k_=k_, tt=tt: e.copy(out=vx[:, tt, :, 0:64], in_=k_[:, 256:384].rearrange("p (h d) -> p h d", d=64)), reads=[f"gpk{tt % 2}"], writes=["gvx"])
                    if lat:
                        gens.append(qk_prep(C, G, q_[:], f"gpq{tt % 2}", gq, "ggq", cs, sn, tt, True, qT, "gqT", 8, Wq[tt % 2], f"gwq{tt % 2}"))
                    gens.append(qk_prep(C, G, k_[:, 0:256], f"gpk{tt % 2}", gk, "ggk", cs, sn, tt, lat, kT, "gkT", 4, Wk[tt % 2], f"gwk{tt % 2}"))
                run_rr(gens)
        with C.phase():
            sp = [C.ps(f"gsp{i}", [128, 512], F32) for i in range(4)]
            oacc = [C.ps(f"goacc{i}", [128, 65], F32) for i in range(4)]
            pt = [C.sb(f"gpt{i}", [128, 512], BF16) for i in range(3)]
            den = C.sb("gden", [128, 8], F32)
            on = [C.sb(f"gon{i}", [128, 512], F32) for i in range(2)]
            oT = [C.sb(f"goT{i}", [128, 4, 128], BF16) for i in range(2)]
            its = []
            for qt in range(16):
                for kv in range(2):
                    tiles = [(16, None), (17, None)]
                    if qt > 0:
                        tiles.append((qt - 1, 0))
                    tiles.append((qt, None))
                    if qt < 15:
                        tiles.append((qt + 1, 1))
                    for ti, (kt, mk) in enumerate(tiles):
                        its.append(dict(qt=qt, kv=kv, kt=kt, mk=mk, first=(ti == 0), last=(ti == len(tiles) - 1)))

            def emit_s(i):
                d_ = its[i]
                qt, kv, kt, mk = d_["qt"], d_["kv"], d_["kt"], d_["mk"]
                p_, kp_ = pt[i % 3], f"gpt{i % 3}"
                for half in range(2):
                    sb_i = (i % 2) * 2 + half
                    pb = slice(half * 64, (half + 1) * 64)
                    S.op("pe", lambda e, pb=pb, sb_i=sb_i, kv=kv, kt=kt, qt=qt: e.matmul(
                        sp[sb_i][:, 0:256], lhsT=kT[pb, kv, kt * 128:(kt + 1) * 128],
                        rhs=qT[pb, kv * 2:kv * 2 + 2, qt * 128:(qt + 1) * 128], start=True, stop=True),
                        reads=["gqT", "gkT"], writes=[f"gsp{sb_i}"])
                    S.op("act", lambda e, p_=p_, half=half, sb_i=sb_i: e.activation(out=p_[:, half * 256:(half + 1) * 256], in_=sp[sb_i][:, 0:256], func=AF.Exp),
                         reads=[f"gsp{sb_i}"], writes=[kp_])
                if mk is not None:
                    S.op("pool", lambda e, p_=p_, mk=mk: e.tensor_tensor(out=p_[:].rearrange("p (a b) -> p a b", b=128), in0=p_[:].rearrange("p (a b) -> p a b", b=128),
                                                                         in1=msk[:, mk, None, :].to_broadcast([128, 4, 128]), op=ALU.mult),
                         reads=[kp_, "gmask"], writes=[kp_])

            emit_s(0)
            for i in range(len(its)):
                if i + 1 < len(its):
                    emit_s(i + 1)
                d_ = its[i]
                qt, kv, kt = d_["qt"], d_["kv"], d_["kt"]
                p_, kp_ = pt[i % 3], f"gpt{i % 3}"
                o_n, kon = on[qt % 2], f"gon{qt % 2}"
                for sl in range(4):
                    S.op("pe", lambda e, sl=sl, p_=p_, kt=kt, kv=kv, first=d_["first"], last=d_["last"]: e.matmul(
                        oacc[sl][:], lhsT=p_[:, sl * 128:(sl + 1) * 128], rhs=vx[:, kt, kv, :], start=first, stop=last),
                        reads=[kp_, "gvx", "gvx1"], writes=[f"goacc{sl}"])
                if not d_["last"]:
                    continue
                for sl in range(4):
                    half, i2 = sl // 2, sl % 2
                    hd = kv * 4 + 2 * i2 + half
                    S.op("dve", lambda e, sl=sl, hd=hd: e.tensor_tensor(out=den[:, hd:hd + 1], in0=oacc[sl][:, 64:65], in1=sk[:, hd:hd + 1], op=ALU.add),
                         reads=[f"goacc{sl}", "gsink"], writes=["gden"])
                    S.op("dve", lambda e, hd=hd: e.reciprocal(out=den[:, hd:hd + 1], in_=den[:, hd:hd + 1]), reads=["gden"], writes=["gden"])
                    S.op("dve", lambda e, sl=sl, hd=hd, o_n=o_n: e.tensor_scalar(out=o_n[:, hd * 64:(hd + 1) * 64], in0=oacc[sl][:, 0:64], scalar1=den[:, hd:hd + 1],
                                                                               scalar2=None, op0=ALU.mult),
                         reads=[f"goacc{sl}", "gden"], writes=[kon])
                if kv == 0:
                    continue
                t_, kt_ = oT[qt % 2], f"goT{qt % 2}"
                tb_i = ((i + 1) % 2) * 2 + 1 if i + 1 < len(its) else 1
                tb_i = (i % 2) * 2
                tps = sp[tb_i]
                for j in range(4):
                    S.op("pe", lambda e, j=j, o_n=o_n, tps=tps: e.transpose(out=tps[:, j * 128:(j + 1) * 128], in_=o_n[:, j * 128:(j + 1) * 128], identity=G["ident_f"][:]),
                         reads=[kon, "ident_f"], writes=[f"gsp{tb_i}"])
                S.op("act", lambda e, t_=t_, tps=tps: e.copy(out=t_[:], in_=tps[:].rearrange("p (a b) -> p a b", b=128)), reads=[f"gsp{tb_i}"], writes=[kt_])
                S.dma("sp", G["mix_d"][512:1024, qt * 128:(qt + 1) * 128].rearrange("(j p) t -> p j t", p=128), t_[:], reads=[kt_], writes=[("mix_d", "g", qt)])
```

```python
import contextlib
import math
import numpy as np
import ml_dtypes
import concourse.bass as bass
import concourse.mybir as mybir
from concourse.bass_utils import run_bass_kernel_spmd

F32 = mybir.dt.float32
BF16 = mybir.dt.bfloat16
I32 = mybir.dt.int32
AF = mybir.ActivationFunctionType
ALU = mybir.AluOpType
AX = mybir.AxisListType

D = 1024
SEQ = 2048
CTX = 256
T = SEQ + CTX
NT = T // 128
EPS = 1e-6
NEXP = 32
DEXP = 256

ENGS = ("pe", "act", "dve", "pool", "sp")
NDMA_SEM = 8


class _Op:
    __slots__ = ("eng", "fn", "deps", "need_inc", "sem", "val", "is_dma", "rot_dep")

    def __init__(self, eng, fn, is_dma):
        self.eng = eng
        self.fn = fn
        self.deps = []
        self.need_inc = False
        self.sem = None
        self.val = 0
        self.is_dma = is_dma
        self.rot_dep = None


class Sched:
    def __init__(self, nc):
        self.nc = nc
        self.ops = {e: [] for e in ENGS}
        self.last_writer = {}
        self.readers = {}
        self.dma_hist = {e: [] for e in ENGS}

    def _track(self, op, reads, writes):
        deps = []
        for k in reads:
            w = self.last_writer.get(k)
            if w is not None:
                deps.append(w)
        for k in writes:
            w = self.last_writer.get(k)
            if w is not None:
                deps.append(w)
            deps.extend(self.readers.get(k, ()))
        seen = set()
        for dp in deps:
            if dp is op or id(dp) in seen:
                continue
            if dp.eng == "pe" and op.eng == "pe" and not dp.is_dma and not op.is_dma:
                continue
            seen.add(id(dp))
            op.deps.append(dp)
            dp.need_inc = True
        for k in writes:
            self.last_writer[k] = op
            self.readers[k] = []
        for k in reads:
            self.readers.setdefault(k, []).append(op)

    def op(self, eng, fn, reads=(), writes=()):
        o = _Op(eng, fn, False)
        self._track(o, reads, writes)
        self.ops[eng].append(o)
        return o

    def dma(self, eng, out, in_, reads=(), writes=(), **kw):
        def fn(e, out=out, in_=in_, kw=kw):
            return e.dma_start(out=out, in_=in_, **kw)
        o = _Op(eng, fn, True)
        o.need_inc = True
        self._track(o, reads, writes)
        hist = self.dma_hist[eng]
        if len(hist) >= NDMA_SEM:
            o.rot_dep = hist[-NDMA_SEM]
        hist.append(o)
        self.ops[eng].append(o)
        return o

    def barrier(self):
        lasts = []
        for e in ENGS:
            for o in reversed(self.ops[e]):
                if not o.is_dma and o.fn is not None:
                    lasts.append(o)
                    break
            lasts.extend(self.dma_hist[e][-NDMA_SEM:])
        for e in ENGS:
            o = _Op(e, None, False)
            for dp in lasts:
                o.deps.append(dp)
                dp.need_inc = True
            self.ops[e].append(o)
        self.last_writer = {}
        self.readers = {}

    def emit(self, final_wait_ops=()):
        nc = self.nc
        sems = {e: nc.alloc_semaphore(f"s_{e}") for e in ENGS}
        dsems = {e: [nc.alloc_semaphore(f"d_{e}{i}") for i in range(NDMA_SEM)] for e in ENGS
                 if self.dma_hist[e]}
        for e in ENGS:
            cnt = 0
            dcnt = [0] * NDMA_SEM
            di = 0
            for o in self.ops[e]:
                if o.is_dma:
                    j = di % NDMA_SEM
                    dcnt[j] += 16
                    o.sem = dsems[e][j]
                    o.val = dcnt[j]
                    di += 1
                elif o.need_inc and o.fn is not None:
                    cnt += 1
                    o.sem = sems[e]
                    o.val = cnt
        engobj = {"pe": "tensor", "act": "scalar", "dve": "vector", "pool": "gpsimd", "sp": "sync"}
        final_wait_ops = list(final_wait_ops)
        with nc.Block() as block:
            for e in ENGS:
                def body(eng, e=e):
                    waited = {}

                    def wait(dp):
                        if dp.sem is None:
                            return
                        key = id(dp.sem)
                        if waited.get(key, 0) >= dp.val:
                            return
                        waited[key] = dp.val
                        eng.wait_ge(dp.sem, dp.val)
                    for o in self.ops[e]:
                        for dp in o.deps:
                            wait(dp)
                        if o.rot_dep is not None:
                            wait(o.rot_dep)
                        if o.fn is None:
                            continue
                        ins = o.fn(eng)
                        if o.is_dma:
                            ins.then_inc(o.sem, 16)
                        elif o.need_inc:
                            ins.then_inc(o.sem, 1)
                    if e == "sp":
                        for dp in final_wait_ops:
                            wait(dp)
                getattr(block, engobj[e])(body)
        return {e: len(self.ops[e]) for e in ENGS}


class Ctx:
    def __init__(self, nc):
        self.nc = nc
        self.S = Sched(nc)
        self.stack = None
        self.uid = 0

    @contextlib.contextmanager
    def phase(self):
        prev = self.stack
        with contextlib.ExitStack() as st:
            self.stack = st
            yield
            self.S.barrier()
        self.stack = prev

    def sb(self, name, shape, dtype):
        self.uid += 1
        return self.stack.enter_context(self.nc.sbuf_tensor(f"{name}_{self.uid}", list(shape), dtype))

    def ps(self, name, shape, dtype=F32):
        self.uid += 1
        return self.stack.enter_context(self.nc.psum_tensor(f"{name}_{self.uid}", list(shape), dtype))


def token_blocks():
    return [(0, 512, 0), (512, 512, 0), (1024, 512, 0), (1536, 512, 0), (2048, 256, 1)]


def phase_load(C, G):
    S = C.S
    hT = G["hT"]
    with C.phase():
        xt = [C.sb(f"ld_x{i}", [128, D], F32) for i in range(3)]
        tp = [C.ps(f"ld_tp{i}", [128, 8, 128], F32) for i in range(2)]
        for t in range(NT):
            src = G["x"][t * 128:(t + 1) * 128, :] if t < 16 else G["ctx"][(t - 16) * 128:(t - 15) * 128, :]
            xb, pb = xt[t % 3], tp[t % 2]
            kx, kp = f"ldx{t % 3}", f"ldp{t % 2}"
            S.dma("sp", xb[:], src, writes=[kx])
            for k in range(8):
                S.op("pe", lambda e, k=k, xb=xb, pb=pb: e.transpose(out=pb[:, k, :], in_=xb[:, k * 128:(k + 1) * 128],
                                                                 identity=G["ident_f"][:]),
                     reads=[kx, "ident_f"], writes=[kp])
            eng = "act" if t % 2 == 0 else "dve"
            if eng == "act":
                S.op("act", lambda e, pb=pb, t=t: e.copy(out=hT[:, :, t * 128:(t + 1) * 128], in_=pb[:]),
                     reads=[kp], writes=[("hT", t)])
            else:
                S.op("dve", lambda e, pb=pb, t=t: e.tensor_copy(out=hT[:, :, t * 128:(t + 1) * 128], in_=pb[:]),
                     reads=[kp], writes=[("hT", t)])


def phase_store(C, G):
    S = C.S
    hT = G["hT"]
    outs = []
    with C.phase():
        ot = [C.sb(f"st_o{i}", [128, D], F32) for i in range(3)]
        tp = [C.ps(f"st_tp{i}", [128, 8, 128], F32) for i in range(2)]
        for t in range(16):
            ob, pb = ot[t % 3], tp[t % 2]
            ko, kp = f"sto{t % 3}", f"stp{t % 2}"
            for k in range(8):
                S.op("pe", lambda e, k=k, pb=pb, t=t: e.transpose(out=pb[:, k, :], in_=hT[:, k, t * 128:(t + 1) * 128],
                                                                identity=G["ident_f"][:]),
                     reads=[("hT", t), "ident_f"], writes=[kp])
            if t % 2 == 0:
                S.op("act", lambda e, pb=pb, ob=ob: e.copy(out=ob[:].rearrange("p (k d) -> p k d", k=8), in_=pb[:]),
                     reads=[kp], writes=[ko])
            else:
                S.op("dve", lambda e, pb=pb, ob=ob: e.tensor_copy(out=ob[:].rearrange("p (k d) -> p k d", k=8), in_=pb[:]),
                     reads=[kp], writes=[ko])
            outs.append(S.dma("sp", G["out"][t * 128:(t + 1) * 128, :], ob[:], reads=[ko]))
        G["final_ops"] = outs


def phase_mods(C, G, mid=None):
    S = C.S
    nc = C.nc
    with C.phase():
        c2sb = C.sb("c2sb", [2, D], F32)
        bsb = C.sb("bsb", [2, 6 * D], F32)
        cs = C.sb("cs", [128, 8, 2], BF16)
        bcol = C.sb("bcol", [128, 48, 2], F32)
        cps = C.ps("cps", [128, 8, 2], F32)
        bps = C.ps("bps", [128, 48, 2], F32)
        S.dma("sp", c2sb[:], G["c2"], writes=["c2sb"])
        S.dma("sp", bsb[:], G["ada_b"], writes=["bsb"])
        for k in range(8):
            S.op("pe", lambda e, k=k: e.transpose(out=cps[:, k, :], in_=c2sb[0:2, k * 128:(k + 1) * 128], identity=G["ident_f"][0:2, 0:2]),
                 reads=["c2sb", "ident_f"], writes=["cps"])
        for j in range(48):
            S.op("pe", lambda e, j=j: e.transpose(out=bps[:, j, :], in_=bsb[0:2, j * 128:(j + 1) * 128], identity=G["ident_f"][0:2, 0:2]),
                 reads=["bsb", "ident_f"], writes=["bps"])
        S.op("act", lambda e: e.activation(out=cs[:], in_=cps[:], func=AF.Silu), reads=["cps"], writes=["cs"])
        S.op("act", lambda e: e.copy(out=bcol[:], in_=bps[:]), reads=["bps"], writes=["bcol"])
        wb = [C.sb(f"adaw{i}", [128, 8, 1536], BF16) for i in range(2)]
        mp = C.ps("modp", [128, 2, 48, 2], F32)
        pieces = [(l, q) for l in range(2) for q in range(4)]

        def issue(i):
            l, q = pieces[i]
            S.dma("pool", wb[i % 2][:], G["ada_w"][l, :, q * 1536:(q + 1) * 1536].rearrange("(k p) n -> p k n", p=128), writes=[f"adaw{i % 2}"])
        issue(0)
        issue(1)
        if mid is not None:
            mid()
        i = 0
        for l in range(2):
            for q in range(4):
                w, kw = wb[i % 2], f"adaw{i % 2}"
                if i >= 2:
                    issue(i)
                for jj in range(12):
                    j = q * 12 + jj
                    for k in range(8):
                        S.op("pe", lambda e, w=w, l=l, j=j, jj=jj, k=k: e.matmul(mp[:, l, j, :], lhsT=w[:, k, jj * 128:(jj + 1) * 128],
                                                                              rhs=cs[:, k, :], start=(k == 0), stop=(k == 7)),
                             reads=[kw, "cs"], writes=["modp"])
                i += 1
        for l in range(2):
            for s in range(2):
                S.op("dve", lambda e, l=l, s=s: e.tensor_tensor(out=G["modc"][:, l, :, s], in0=mp[:, l, :, s], in1=bcol[:, :, l], op=ALU.add),
                     reads=["modp", "bcol"], writes=["modc"])
        for l in range(2):
            for j0 in (8, 32):
                S.op("dve", lambda e, l=l, j0=j0: e.tensor_scalar(out=G["modc"][:, l, j0:j0 + 8, :], in0=G["modc"][:, l, j0:j0 + 8, :],
                                                                 scalar1=1.0, scalar2=None, op0=ALU.add),
                     reads=["modc"], writes=["modc"])


def phase_norm(C, G, l, which, router=False, ntok=T):
    S = C.S
    sh0 = 0 if which == 0 else 24
    sc0 = 8 if which == 0 else 32
    hT, aT, modc = G["hT"], G["aT"], G["modc"]
    with C.phase():
        sq = [C.sb(f"nsq{i}", [128, 512], F32) for i in range(3)]
        tmp = [C.sb(f"ntmp{i}", [128, 512], F32) for i in range(3)]
        rstd = [C.sb(f"nrstd{i}", [128, 512], F32) for i in range(2)]
        ssp = [C.ps(f"nss{i}", [128, 512], F32) for i in range(2)]
        if router:
            v32 = [C.sb(f"nv32{i}", [128, 512], F32) for i in range(8)]
            wr = C.sb("wr32", [128, 8, 36], F32)
            rb = C.sb("rbias", [128, 36], F32)
            lgp = [C.ps(f"lgp{i}", [128, 4, 64], F32) for i in range(2)]
            ctp = C.ps("combTp", [32, 128], F32)
            combT = C.sb("combT", [32, T], BF16)
            S.dma("sp", wr[:, :, 0:4], G["moe_w_grp"][l].rearrange("(k p) n -> p k n", p=128), writes=["wr32a"])
            S.dma("sp", wr[:, :, 4:36], G["moe_w_rt"][l].rearrange("(k p) n -> p k n", p=128), writes=["wr32b"])
            S.dma("sp", rb[:, 0:4], G["moe_b_grp"][l:l + 1, :].to_broadcast([128, 4]), writes=["rba"])
            S.dma("sp", rb[:, 4:36], G["moe_b_rt"][l:l + 1, :].to_broadcast([128, 32]), writes=["rbb"])
            rts = [C.sb(f"rt{i}", [128, 160], F32) for i in range(8)]
        cnt = 0
        tile_idx = 0
        for bi, (t0, n, s) in enumerate(token_blocks()):
            if t0 >= ntok:
                continue
            sp_, rs_ = ssp[bi % 2], rstd[bi % 2]
            ksp, krs = f"nss{bi % 2}", f"nrstd{bi % 2}"
            hkeys = [("hT", tt) for tt in range(t0 // 128, (t0 + n) // 128)]
            for k in range(8):
                b = sq[cnt % 3]
                kb = f"nsq{cnt % 3}"
                cnt += 1
                S.op("act", lambda e, b=b, k=k, t0=t0, n=n: e.activation(out=b[:, :n], in_=hT[:, k, t0:t0 + n], func=AF.Square),
                     reads=hkeys, writes=[kb])
                S.op("pe", lambda e, b=b, k=k, n=n, sp_=sp_: e.matmul(sp_[:, :n], lhsT=G["ones_f"][:], rhs=b[:, :n], start=(k == 0), stop=(k == 7)),
                     reads=[kb, "ones_f"], writes=[ksp])
            S.op("act", lambda e, sp_=sp_, rs_=rs_, n=n: e.activation(out=rs_[:, :n], in_=sp_[:, :n], func=AF.Sqrt, scale=1.0 / D, bias=EPS),
                 reads=[ksp], writes=[krs])
            S.op("dve", lambda e, rs_=rs_, n=n: e.reciprocal(out=rs_[:, :n], in_=rs_[:, :n]), reads=[krs], writes=[krs])
            akeys = [("aT", tt) for tt in range(t0 // 128, (t0 + n) // 128)]
            lg = lgp[bi % 2] if router else None
            klg = f"lgp{bi % 2}"
            for k in range(8):
                tb = tmp[k % 3]
                ktb = f"ntmp{k % 3}"
                S.op("dve", lambda e, tb=tb, k=k, t0=t0, n=n, rs_=rs_: e.tensor_tensor(out=tb[:, :n], in0=hT[:, k, t0:t0 + n], in1=rs_[:, :n], op=ALU.mult),
                     reads=hkeys + [krs], writes=[ktb])
                if not router:
                    S.op("act", lambda e, tb=tb, k=k, t0=t0, n=n, s=s: e.activation(
                        out=aT[:, k, t0:t0 + n], in_=tb[:, :n], func=AF.Identity,
                        scale=modc[:, l, sc0 + k, s:s + 1], bias=modc[:, l, sh0 + k, s:s + 1]),
                        reads=[ktb, "modc"], writes=akeys)
                else:
                    vb = v32[k]
                    kvb = f"nv32{k}"
                    S.op("act", lambda e, tb=tb, vb=vb, k=k, n=n, s=s: e.activation(
                        out=vb[:, :n], in_=tb[:, :n], func=AF.Identity,
                        scale=modc[:, l, sc0 + k, s:s + 1], bias=modc[:, l, sh0 + k, s:s + 1]),
                        reads=[ktb, "modc"], writes=[kvb])
                    S.op("pool", lambda e, vb=vb, k=k, t0=t0, n=n: e.tensor_copy(out=aT[:, k, t0:t0 + n], in_=vb[:, :n]),
                         reads=[kvb], writes=akeys)
            if router:
                for st in range(n // 128):
                    for k in range(8):
                        S.op("pe", lambda e, k=k, st=st, lg=lg: e.matmul(lg[:, st, 0:36], lhsT=v32[k][:, st * 128:(st + 1) * 128], rhs=wr[:, k, :],
                                                                     start=(k == 0), stop=(k == 7)),
                             reads=[f"nv32{k}", "wr32a", "wr32b"], writes=[klg])
                gens = []
                for st in range(n // 128):
                    tt = t0 // 128 + st
                    gens.append(route_tile(C, G, lg[:, st, 0:36], klg, rb, rts[tile_idx % 8], f"rt{tile_idx % 8}", ctp, combT, tt))
                    tile_idx += 1
                while gens:
                    for g_ in list(gens):
                        try:
                            next(g_)
                        except StopIteration:
                            gens.remove(g_)
        if router:
            S.dma("sp", G["combT_d"][:, :], combT[:], reads=["combT"], writes=["combT_d"])


def route_tile(C, G, lgp, klg, rb, rt, krt, ctp, combT, tt):
    S = C.S
    LG = rt[:, 0:36]
    mg, nmg, sumg, pmax = rt[:, 36:37], rt[:, 37:38], rt[:, 38:39], rt[:, 39:40]
    ohg = rt[:, 40:44]
    eg = rt[:, 44:48]
    les = rt[:, 48:56]
    m1, m2, dm, ed = rt[:, 56:57], rt[:, 57:58], rt[:, 58:59], rt[:, 59:60]
    mk1 = rt[:, 60:68]
    les2 = rt[:, 68:76]
    mk2 = rt[:, 76:84]
    w1, w2 = rt[:, 84:85], rt[:, 85:86]
    ce = rt[:, 88:96]
    comb = rt[:, 96:128]
    R, W = [krt], [krt]

    def dv(fn, extra_r=()):
        S.op("dve", fn, reads=R + list(extra_r), writes=W)

    dv(lambda e: e.tensor_tensor(out=LG, in0=lgp, in1=rb[:], op=ALU.add), extra_r=[klg, "rba", "rbb"])
    yield
    dv(lambda e: e.reduce_max(out=mg, in_=rt[:, 0:4], axis=AX.X))
    yield
    dv(lambda e: e.tensor_scalar(out=ohg, in0=rt[:, 0:4], scalar1=mg, scalar2=None, op0=ALU.is_equal))
    yield
    dv(lambda e: e.tensor_scalar(out=nmg, in0=mg, scalar1=-1.0, scalar2=None, op0=ALU.mult))
    yield
    S.op("act", lambda e: e.activation(out=eg, in_=rt[:, 0:4], func=AF.Exp, bias=nmg, scale=1.0, accum_out=sumg), reads=R, writes=W)
    yield
    dv(lambda e: e.reciprocal(out=pmax, in_=sumg))
    yield
    dv(lambda e: e.tensor_scalar(out=les, in0=rt[:, 4:12], scalar1=rt[:, 40:41], scalar2=None, op0=ALU.mult))
    yield
    for g in range(1, 4):
        dv(lambda e, g=g: e.scalar_tensor_tensor(out=les, in0=rt[:, 4 + 8 * g:12 + 8 * g], scalar=rt[:, 40 + g:41 + g], in1=les,
                                                 op0=ALU.mult, op1=ALU.add))
        yield
    dv(lambda e: e.reduce_max(out=m1, in_=les, axis=AX.X))
    yield
    dv(lambda e: e.tensor_scalar(out=mk1, in0=les, scalar1=m1, scalar2=None, op0=ALU.is_equal))
    yield
    dv(lambda e: e.scalar_tensor_tensor(out=les2, in0=mk1, scalar=-1e30, in1=les, op0=ALU.mult, op1=ALU.add))
    yield
    dv(lambda e: e.reduce_max(out=m2, in_=les2, axis=AX.X))
    yield
    dv(lambda e: e.tensor_scalar(out=mk2, in0=les2, scalar1=m2, scalar2=None, op0=ALU.is_equal))
    yield
    dv(lambda e: e.tensor_tensor(out=dm, in0=m2, in1=m1, op=ALU.subtract))
    yield
    S.op("act", lambda e: e.activation(out=ed, in_=dm, func=AF.Exp), reads=R, writes=W)
    yield
    dv(lambda e: e.tensor_scalar(out=w1, in0=ed, scalar1=1.0, scalar2=None, op0=ALU.add))
    yield
    dv(lambda e: e.reciprocal(out=w1, in_=w1))
    yield
    dv(lambda e: e.tensor_tensor(out=w2, in0=ed, in1=w1, op=ALU.mult))
    yield
    dv(lambda e: e.tensor_scalar(out=rt[:, 84:86], in0=rt[:, 84:86], scalar1=pmax, scalar2=None, op0=ALU.mult))
    yield
    dv(lambda e: e.tensor_scalar(out=ce, in0=mk1, scalar1=w1, scalar2=None, op0=ALU.mult))
    yield
    dv(lambda e: e.scalar_tensor_tensor(out=ce, in0=mk2, scalar=w2, in1=ce, op0=ALU.mult, op1=ALU.add))
    yield
    for g in range(4):
        dv(lambda e, g=g: e.tensor_scalar(out=rt[:, 96 + 8 * g:104 + 8 * g], in0=ce, scalar1=rt[:, 40 + g:41 + g], scalar2=None, op0=ALU.mult))
        yield
    S.op("pe", lambda e: e.transpose(out=ctp[:, :], in_=comb, identity=G["ident_f"][:]), reads=R + ["ident_f"], writes=["combTp"])
    S.op("act", lambda e: e.copy(out=combT[:, tt * 128:(tt + 1) * 128], in_=ctp[:, :]), reads=["combTp"], writes=["combT"])
    yield


def phase_moe(C, G, l, ntok):
    S = C.S
    hT, aT, modc = G["hT"], G["aT"], G["modc"]
    EG = 2
    NB = 2 * EG
    blocks = [(t0, n) for (t0, n, s_) in token_blocks() if t0 < ntok]
    with C.phase():
        wg = [C.sb(f"wg{i}", [128, 8, 256], BF16) for i in range(NB)]
        wu = [C.sb(f"wu{i}", [128, 8, 256], BF16) for i in range(NB)]
        wd = [C.sb(f"wd{i}", [128, 2, 1024], BF16) for i in range(NB)]
        cb = [C.sb(f"cb{i}", [128, T], BF16) for i in range(NB)]
        sg = [C.sb(f"sg{i}", [128, 512], F32) for i in range(2)]
        tg = [C.sb(f"tg{i}", [128, 512], F32) for i in range(2)]
        at = [[[C.sb(f"at{u}_{e}_{c}", [128, 512], BF16) for c in range(2)] for e in range(EG)] for u in range(2)]
        hp = [C.ps(f"hp{i}", [128, 2, 512], F32) for i in range(2)]
        yp = C.ps("yp", [128, 4, 512], F32)

        def load_group(g0):
            for e in range(g0, g0 + EG):
                w = e % NB
                gi, ei = e // 8, e % 8
                S.dma("pool", wg[w][:], G["moe_w_gate"][l, gi, ei].rearrange("(k p) f -> p k f", p=128), writes=[f"wg{w}"])
                S.dma("pool", wu[w][:], G["moe_w_up"][l, gi, ei].rearrange("(k p) f -> p k f", p=128), writes=[f"wu{w}"])
                S.dma("pool", wd[w][:], G["moe_w_down"][l, gi, ei].rearrange("(c p) d -> p c d", p=128), writes=[f"wd{w}"])
                S.dma("sp", cb[w][:, :], G["combT_d"][e:e + 1, :].to_broadcast([128, T]), writes=[f"cb{w}"])

        units = [(g0, bi) for g0 in range(0, NEXP, EG) for bi in range(len(blocks))]
        gcount = [0]

        def emit_g(u, el, c):
            g0, bi = units[u]
            t0, n = blocks[bi]
            e = g0 + el
            w = e % NB
            i = gcount[0]
            gcount[0] += 1
            h, kh = hp[i % 2], f"hp{i % 2}"
            for (wt, kw, q) in ((wg, f"wg{w}", 0), (wu, f"wu{w}", 1)):
                for k in range(8):
                    S.op("pe", lambda e_, h=h, wt=wt, w=w, c=c, k=k, t0=t0, n=n, q=q: e_.matmul(
                        h[:, q, :n], lhsT=wt[w][:, k, c * 128:(c + 1) * 128], rhs=aT[:, k, t0:t0 + n], start=(k == 0), stop=(k == 7)),
                        reads=[kw], writes=[kh])
            sgt, tgt = sg[i % 2], tg[i % 2]
            ksg, ktg = f"sg{i % 2}", f"tg{i % 2}"
            a_ = at[u % 2][el][c]
            ka = f"at{u % 2}_{el}_{c}"
            S.op("act", lambda e_, h=h, sgt=sgt, n=n: e_.activation(out=sgt[:, :n], in_=h[:, 0, :n], func=AF.Silu), reads=[kh], writes=[ksg])
            S.op("dve", lambda e_, h=h, tgt=tgt, w=w, t0=t0, n=n: e_.tensor_tensor(out=tgt[:, :n], in0=h[:, 1, :n], in1=cb[w][:, t0:t0 + n], op=ALU.mult),
                 reads=[kh, f"cb{w}"], writes=[ktg])
            S.op("pool", lambda e_, sgt=sgt, tgt=tgt, a_=a_, n=n: e_.tensor_tensor(out=a_[:, :n], in0=sgt[:, :n], in1=tgt[:, :n], op=ALU.mult),
                 reads=[ksg, ktg], writes=[ka])

        def emit_d(u, half):
            g0, bi = units[u]
            t0, n = blocks[bi]
            for jj in range(4):
                j = half * 4 + jj
                for el in range(EG):
                    w = (g0 + el) % NB
                    for c in range(2):
                        S.op("pe", lambda e_, jj=jj, j=j, el=el, w=w, c=c, n=n, u=u: e_.matmul(
                            yp[:, jj, :n], lhsT=wd[w][:, c, j * 128:(j + 1) * 128], rhs=at[u % 2][el][c][:, :n],
                            start=(el == 0 and c == 0), stop=(el == EG - 1 and c == 1)),
                            reads=[f"wd{w}", f"at{u % 2}_{el}_{c}"], writes=[("yp", jj)])
            s = 0 if t0 < SEQ else 1
            hkeys = [("hT", tt) for tt in range(t0 // 128, (t0 + n) // 128)]
            for jj in range(4):
                j = half * 4 + jj
                S.op("dve", lambda e_, j=j, jj=jj, t0=t0, n=n, s=s: e_.scalar_tensor_tensor(
                    out=hT[:, j, t0:t0 + n], in0=yp[:, jj, :n], scalar=modc[:, l, 40 + j, s:s + 1], in1=hT[:, j, t0:t0 + n],
                    op0=ALU.mult, op1=ALU.add),
                    reads=[("yp", jj), "modc"] + hkeys, writes=hkeys)

        load_group(0)
        for el in range(EG):
            for c in range(2):
                emit_g(0, el, c)
        for u in range(len(units)):
            g0, bi = units[u]
            if bi == 0 and g0 + EG < NEXP:
                load_group(g0 + EG)
            nxt = u + 1 < len(units)
            if nxt:
                emit_g(u + 1, 0, 0)
                emit_g(u + 1, 0, 1)
            emit_d(u, 0)
            if nxt:
                emit_g(u + 1, 1, 0)
                emit_g(u + 1, 1, 1)
            emit_d(u, 1)


def load_cols(C, G, name, srcs, nchunk, width=128):
    S = C.S
    R = sum(a.shape[0] for a in srcs)
    cols = C.sb(name + "_cols", [128, nchunk, R], F32)
    with C.phase():
        rows = C.sb(name + "_rows", [R, nchunk * width], F32)
        pp = C.ps(name + "_ps", [128, nchunk, R], F32)
        r0 = 0
        for i, a in enumerate(srcs):
            S.dma("sp", rows[r0:r0 + a.shape[0], :], a, writes=[(name, "rows", i)])
            r0 += a.shape[0]
        rk = [(name, "rows", i) for i in range(len(srcs))]
        for j in range(nchunk):
            S.op("pe", lambda e, j=j: e.transpose(out=pp[0:width, j, :], in_=rows[0:R, j * width:(j + 1) * width], identity=G["ident_f"][0:R, 0:R]),
                 reads=rk + ["ident_f"], writes=[(name, "ps")])
        S.op("act", lambda e: e.copy(out=cols[0:width], in_=pp[0:width]), reads=[(name, "ps")], writes=[name])
    return cols


def phase_hy_inproj(C, G):
    S = C.S
    aT = G["aT"]
    with C.phase():
        w = C.sb("why", [128, 8, 1536], BF16)
        S.dma("pool", w[:], G["e_w_in"][0, :, 0:1536].rearrange("(k p) n -> p k n", p=128), writes=["why"])
        cw = load_cols(C, G, "hycw", [G["hy_conv_w"][0], G["hy_conv_b"]], 12)
        raw = [C.sb(f"hyraw{i}", [128, T + 4], F32) for i in range(2)]
        tmp = [C.sb(f"hytmp{i}", [128, T], F32) for i in range(2)]
        ob = [C.sb(f"hyob{i}", [128, T], BF16) for i in range(2)]
        vt = [C.sb(f"hyvt{i}", [128, 4, 128], BF16) for i in range(2)]
        pp = [C.ps(f"hypp{i}", [128, 512], F32) for i in range(3)]
        tp = [C.ps(f"hytp{i}", [128, 4, 128], BF16) for i in range(2)]
        for i in range(2):
            for c0 in (0, SEQ + 1, SEQ + 2, T + 3):
                S.op("pool", lambda e, i=i, c0=c0: e.memset(raw[i][:, c0:c0 + 1], 0.0), writes=[f"hyraw{i}"])
        ip = 0
        for cc in range(12):
            rw, tm, o = raw[cc % 2], tmp[cc % 2], ob[cc % 2]
            krw, ktm, ko = f"hyraw{cc % 2}", f"hytmp{cc % 2}", f"hyob{cc % 2}"
            for (t0, n, s) in token_blocks():
                p = pp[ip % 3]
                kp = f"hypp{ip % 3}"
                ip += 1
                for k in range(8):
                    S.op("pe", lambda e, p=p, k=k, cc=cc, t0=t0, n=n: e.matmul(p[:, :n], lhsT=w[:, k, cc * 128:(cc + 1) * 128], rhs=aT[:, k, t0:t0 + n],
                                                                            start=(k == 0), stop=(k == 7)),
                         reads=["why", "aT_all"], writes=[kp])
                off = 1 + t0 if s == 0 else 3 + t0
                S.op("act", lambda e, p=p, rw=rw, off=off, n=n: e.copy(out=rw[:, off:off + n], in_=p[:, :n]), reads=[kp], writes=[krw])
            for (a0, L_, o0) in ((0, SEQ, 0), (SEQ + 2, CTX, SEQ)):
                S.op("act", lambda e, rw=rw, tm=tm, a0=a0, L_=L_, o0=o0, cc=cc: e.activation(
                    out=tm[:, o0:o0 + L_], in_=rw[:, a0 + 1:a0 + 1 + L_], func=AF.Identity, scale=cw[:, cc, 1:2], bias=cw[:, cc, 3:4]),
                    reads=[krw, "hycw"], writes=[ktm])
                S.op("dve", lambda e, rw=rw, tm=tm, a0=a0, L_=L_, o0=o0, cc=cc: e.scalar_tensor_tensor(
                    out=tm[:, o0:o0 + L_], in0=rw[:, a0:a0 + L_], scalar=cw[:, cc, 0:1], in1=tm[:, o0:o0 + L_], op0=ALU.mult, op1=ALU.add),
                    reads=[krw, ktm, "hycw"], writes=[ktm])
                S.op("dve", lambda e, rw=rw, tm=tm, o=o, a0=a0, L_=L_, o0=o0, cc=cc: e.scalar_tensor_tensor(
                    out=o[:, o0:o0 + L_], in0=rw[:, a0 + 2:a0 + 2 + L_], scalar=cw[:, cc, 2:3], in1=tm[:, o0:o0 + L_], op0=ALU.mult, op1=ALU.add),
                    reads=[krw, ktm, "hycw"], writes=[ko])
            S.dma("sp", G["hyc_d"][cc * 128:(cc + 1) * 128, :], o[:], reads=[ko], writes=[("hyc_d", cc)])
            if cc < 4:
                for tg in range(0, NT, 4):
                    nt = min(4, NT - tg)
                    t_ps, t_sb = tp[(tg // 4) % 2], vt[(tg // 4) % 2]
                    kps, ksb = f"hytp{(tg // 4) % 2}", f"hyvt{(tg // 4) % 2}"
                    for i in range(nt):
                        S.op("pe", lambda e, i=i, tg=tg, o=o, t_ps=t_ps: e.transpose(out=t_ps[:, i, :], in_=o[:, (tg + i) * 128:(tg + i + 1) * 128],
                                                                                identity=G["ident_b"][:]),
                             reads=[ko, "ident_b"], writes=[kps])
                    S.op("dve", lambda e, t_ps=t_ps, t_sb=t_sb, nt=nt: e.tensor_copy(out=t_sb[:, :nt, :], in_=t_ps[:, :nt, :]), reads=[kps], writes=[ksb])
                    S.dma("sp", G["vtok_d"][tg * 128:(tg + nt) * 128, cc * 128:(cc + 1) * 128].rearrange("(i p) c -> p i c", p=128),
                          t_sb[:, :nt, :], reads=[ksb], writes=[("vtok_d", cc, tg)])


def phase_hy_filter(C, G, L, zemb_name, out_name):
    S = C.S
    TWO_PI = 2.0 * math.pi
    with C.phase():
        zT = C.sb("zT", [33, L], F32)
        w1 = C.sb("fw1", [33, 64], F32)
        w2 = C.sb("fw2", [64, 64], F32)
        w3 = C.sb("fw3", [64, 2048], F32)
        S.dma("sp", zT[:], G[zemb_name], writes=["zT"])
        S.dma("sp", w1[:], G["hy_f_w1"][0], writes=["fw1"])
        S.dma("sp", w2[:], G["hy_f_w2"][0], writes=["fw2"])
        S.dma("sp", w3[:], G["hy_f_w3"][0], writes=["fw3"])
        pc = load_cols(C, G, "hyfp", [G["hy_f_b1"], G["hy_f_b2"], G["hy_f_freq"][0]], 1, width=64)
        dl = C.sb("delta_bc", [128, 512], F32)
        negt = C.sb("negt", [128, L // 128], F32)
        mask0 = C.sb("mask0", [128, 1], F32)
        S.dma("sp", dl[:], G["c_delta"][0:1, :].to_broadcast([128, 512]), writes=["delta_bc"])
        S.dma("sp", negt[:], G["c_negt_%d" % L], writes=["negt"])
        S.dma("sp", mask0[:], G["c_mask0"], writes=["mask0"])
        h1 = C.sb("fh1", [64, L], F32)
        h2 = C.sb("fh2", [64, L], F32)
        a = C.sb("fa", [64, 512], F32)
        ki = C.sb("fki", [64, 512], I32)
        kf = C.sb("fkf", [64, 512], F32)
        hp = C.ps("fhp", [64, 512], F32)
        nb = max(1, L // 512)
        bs = min(L, 512)
        for layer, (wt, kw, src, ksrc, dst, kdst, bcol, fcol) in enumerate((
                (w1, "fw1", zT, "zT", h1, "fh1", 0, 2), (w2, "fw2", h1, "fh1", h2, "fh2", 1, 3))):
            for b in range(nb):
                sl = slice(b * bs, (b + 1) * bs)
                S.op("pe", lambda e, wt=wt, src=src, sl=sl: e.matmul(hp[:, :bs], lhsT=wt[:], rhs=src[:, sl], start=True, stop=True),
                     reads=[kw, ksrc], writes=["fhp"])
                S.op("dve", lambda e, bcol=bcol, fcol=fcol: e.tensor_scalar(out=a[:, :bs], in0=hp[:, :bs], scalar1=pc[0:64, 0, bcol:bcol + 1],
                                                                           scalar2=pc[0:64, 0, fcol:fcol + 1], op0=ALU.add, op1=ALU.mult),
                     reads=["fhp", "hyfp"], writes=["fa"])
                S.op("dve", lambda e: e.tensor_scalar(out=ki[:, :bs], in0=a[:, :bs], scalar1=1.0 / TWO_PI, scalar2=None, op0=ALU.mult),
                     reads=["fa"], writes=["fki"])
                S.op("dve", lambda e: e.tensor_copy(out=kf[:, :bs], in_=ki[:, :bs]), reads=["fki"], writes=["fkf"])
                S.op("dve", lambda e: e.scalar_tensor_tensor(out=a[:, :bs], in0=kf[:, :bs], scalar=-TWO_PI, in1=a[:, :bs], op0=ALU.mult, op1=ALU.add),
                     reads=["fkf", "fa"], writes=["fa"])
                S.op("dve", lambda e: e.tensor_scalar(out=a[:, :bs], in0=a[:, :bs], scalar1=3.1415925, scalar2=-3.1415925, op0=ALU.min, op1=ALU.max),
                     reads=["fa"], writes=["fa"])
                S.op("act", lambda e, dst=dst, sl=sl: e.activation(out=dst[:, sl], in_=a[:, :bs], func=AF.Sin), reads=["fa"], writes=[kdst])
        p3 = [C.ps(f"fp3_{i}", [128, 512], F32) for i in range(4)]
        dec = [C.sb(f"fdec{i}", [128, 512], F32) for i in range(2)]
        bw = [C.sb(f"fbw{i}", [128, 512], F32) for i in range(2)]
        sm = [C.sb(f"fsm{i}", [128, 512], F32) for i in range(2)]
        df = [C.sb(f"fdf{i}", [128, 512], F32) for i in range(2)]
        fo = [C.sb(f"ffo{i}", [128, 2, 512], BF16) for i in range(2)]
        it = 0
        for lt in range(L // 128):
            dc, kdc = dec[lt % 2], f"fdec{lt % 2}"
            S.op("act", lambda e, dc=dc, lt=lt: e.activation(out=dc[:], in_=dl[:], func=AF.Exp, scale=negt[:, lt:lt + 1]),
                 reads=["delta_bc", "negt"], writes=[kdc])
            for n in range(2):
                for dr in range(2):
                    q = n * 2 + dr
                    S.op("pe", lambda e, q=q, lt=lt: e.matmul(p3[q][:], lhsT=h2[:, lt * 128:(lt + 1) * 128], rhs=w3[:, q * 512:(q + 1) * 512],
                                                           start=True, stop=True),
                         reads=["fh2", "fw3"], writes=[f"fp3_{q}"])
            for n in range(2):
                b_, s_, d_, o_ = bw[it % 2], sm[it % 2], df[it % 2], fo[it % 2]
                kb, ks, kd, kfo = f"fbw{it % 2}", f"fsm{it % 2}", f"fdf{it % 2}", f"ffo{it % 2}"
                it += 1
                pf, pb = p3[n * 2], p3[n * 2 + 1]
                if lt == 0:
                    S.op("act", lambda e, b_=b_, pb=pb: e.activation(out=b_[:], in_=pb[:], func=AF.Identity, scale=mask0[:, 0:1]),
                         reads=[f"fp3_{n * 2 + 1}", "mask0"], writes=[kb])
                else:
                    S.op("act", lambda e, b_=b_, pb=pb: e.copy(out=b_[:], in_=pb[:]), reads=[f"fp3_{n * 2 + 1}"], writes=[kb])
                S.op("dve", lambda e, s_=s_, pf=pf, b_=b_: e.tensor_tensor(out=s_[:], in0=pf[:], in1=b_[:], op=ALU.add),
                     reads=[f"fp3_{n * 2}", kb], writes=[ks])
                S.op("dve", lambda e, d_=d_, pf=pf, b_=b_: e.tensor_tensor(out=d_[:], in0=pf[:], in1=b_[:], op=ALU.subtract),
                     reads=[f"fp3_{n * 2}", kb], writes=[kd])
                S.op("pool", lambda e, o_=o_, s_=s_, dc=dc: e.tensor_tensor(out=o_[:, 0, :], in0=s_[:], in1=dc[:], op=ALU.mult),
                     reads=[ks, kdc], writes=[kfo])
                S.op("pool", lambda e, o_=o_, d_=d_, dc=dc: e.tensor_tensor(out=o_[:, 1, :], in0=d_[:], in1=dc[:], op=ALU.mult),
                     reads=[kd, kdc], writes=[kfo])
                S.dma("sp", G[out_name][n, :, lt * 128:(lt + 1) * 128, :].rearrange("s p c -> p s c"), o_[:], reads=[kfo],
                      writes=[(out_name, n, lt)])


def phase_hyena_conv(C, G, L, tok0, filt_name, Fname, Gname, vtok_src):
    S = C.S
    nsc = L // 128
    npair = L // 128
    tb = min(L, 512)
    ntb = L // tb
    with C.phase():
        ztok = C.sb("ztok", [128, nsc, 512], BF16)
        hb = load_cols(C, G, "hybias", [G["hy_bias"][0]], 4)
        S.dma("sp", ztok[:], G[vtok_src][tok0:tok0 + L, :].rearrange("(c p) n -> p c n", p=128), writes=["ztok"])
        for order in range(2):
            with C.phase():
                fs = C.sb("fs", [128, nsc, 512], BF16)
                fd = C.sb("fd", [128, nsc, 512], BF16)
                S.dma("sp", fs[:], G[filt_name][order, 0].rearrange("(c p) n -> p c n", p=128), writes=["fs"])
                S.dma("sp", fd[:], G[filt_name][order, 1].rearrange("(c p) n -> p c n", p=128), writes=["fd"])
                Y = C.sb("Y", [128, 2 * npair, 512], BF16)
                with C.phase():
                    Ft = [C.sb(f"Ft{i}", [128, 2, nsc, 128], BF16) for i in range(2)]
                    zp = [C.ps(f"zp{i}", [128, 4, 512], F32) for i in range(2)]
                    hs = [C.sb(f"hs{i}", [128, 2, 512], F32) for i in range(2)]
                    t1 = [C.sb(f"t1_{i}", [128, 2, 512], F32) for i in range(2)]
                    t2 = [C.sb(f"t2_{i}", [128, 2, 512], F32) for i in range(2)]
                    for j in range(npair):
                        F_, kF = Ft[j % 2], f"Ft{j % 2}"
                        z, kz = zp[j % 2], f"zp{j % 2}"
                        h_, kh = hs[j % 2], f"hs{j % 2}"
                        a_, ka = t1[j % 2], f"t1_{j % 2}"
                        b_, kb = t2[j % 2], f"t2_{j % 2}"
                        S.dma("sp", F_[:, 0], G[Fname][j], writes=[kF])
                        S.dma("sp", F_[:, 1], G[Fname][npair + j], writes=[kF])
                        for q, (ri, mv, kmv) in enumerate(((0, ztok, "ztok"), (1, ztok, "ztok"), (0, fs, "fs"), (1, fd, "fd"))):
                            for c in range(nsc):
                                S.op("pe", lambda e, z=z, q=q, ri=ri, c=c, mv=mv, F_=F_: e.matmul(z[:, q, :], lhsT=F_[:, ri, c, :], rhs=mv[:, c, :],
                                                                                             start=(c == 0), stop=(c == nsc - 1)),
                                     reads=[kF, kmv], writes=[kz])
                        S.op("act", lambda e, z=z, h_=h_: e.copy(out=h_[:], in_=z[:, 2:4, :]), reads=[kz], writes=[kh])
                        S.op("dve", lambda e, z=z, h_=h_, a_=a_: e.tensor_tensor(out=a_[:], in0=z[:, 0:2, :], in1=h_[:], op=ALU.mult),
                             reads=[kz, kh], writes=[ka])
                        S.op("dve", lambda e, z=z, h_=h_, b_=b_: e.tensor_tensor(out=b_[:, 0, :], in0=z[:, 0, :], in1=h_[:, 1, :], op=ALU.mult),
                             reads=[kz, kh], writes=[kb])
                        S.op("dve", lambda e, z=z, h_=h_, b_=b_: e.tensor_tensor(out=b_[:, 1, :], in0=z[:, 1, :], in1=h_[:, 0, :], op=ALU.mult),
                             reads=[kz, kh], writes=[kb])
                        S.op("pool", lambda e, a_=a_, j=j, Y=Y: e.tensor_tensor(out=Y[:, j, :], in0=a_[:, 0, :], in1=a_[:, 1, :], op=ALU.subtract),
                             reads=[ka], writes=["Y"])
                        S.op("pool", lambda e, b_=b_, j=j, Y=Y: e.tensor_tensor(out=Y[:, npair + j, :], in0=b_[:, 0, :], in1=b_[:, 1, :], op=ALU.add),
                             reads=[kb], writes=["Y"])
                with C.phase():
                    KG = 4
                    Gt = [C.sb(f"Gt{i}", [128, KG, tb], BF16) for i in range(3)]
                    op_ = [C.ps(f"op{i}", [128, tb], F32) for i in range(4)]
                    zprev = [C.sb(f"zprev{i}", [128, tb], BF16) for i in range(4)]
                    gate = [C.sb(f"gate{i}", [128, tb], BF16) for i in range(4)]
                    tmpf = [C.sb(f"tmpf{i}", [128, tb], F32) for i in range(2)]
                    zn = [C.sb(f"zn{i}", [128, tb], BF16) for i in range(2)]
                    ttp = [C.ps(f"ttp{i}", [128, 4, 128], BF16) for i in range(2)]
                    ig = 0
                    ie = 0
                    for b in range(ntb):
                        for kg in range(2 * npair // KG):
                            g_, kg_ = Gt[ig % 3], f"Gt{ig % 3}"
                            ig += 1
                            S.dma("sp", g_[:], G[Gname][b, kg], writes=[kg_])
                            for kk in range(KG):
                                kc = kg * KG + kk
                                for cc in range(4):
                                    S.op("pe", lambda e, cc=cc, kc=kc, kk=kk, g_=g_, Y=Y, op_=op_: e.matmul(op_[cc][:], lhsT=Y[:, kc, cc * 128:(cc + 1) * 128], rhs=g_[:, kk, :],
                                                                                         start=(kc == 0), stop=(kc == 2 * npair - 1)),
                                         reads=["Y", kg_], writes=[f"op{cc}"])
                        t_lo = tok0 + b * tb
                        for cc in range(4):
                            zp_, kzp = zprev[ie % 4], f"zprev{ie % 4}"
                            gt_, kgt = gate[ie % 4], f"gate{ie % 4}"
                            tf_, ktf = tmpf[ie % 2], f"tmpf{ie % 2}"
                            zn_, kzn = zn[ie % 2], f"zn{ie % 2}"
                            tp_, ktp = ttp[ie % 2], f"ttp{ie % 2}"
                            ie += 1
                            if order == 0:
                                S.dma("sp", zp_[:], G["hyc_d"][cc * 128:(cc + 1) * 128, t_lo:t_lo + tb], writes=[kzp])
                            else:
                                S.dma("sp", zp_[:], G["z1_d"][cc * 128:(cc + 1) * 128, t_lo:t_lo + tb], reads=[("z1_d", cc, t_lo)], writes=[kzp])
                            grow = (1 + order) * 512 + cc * 128
                            S.dma("sp", gt_[:], G["hyc_d"][grow:grow + 128, t_lo:t_lo + tb], writes=[kgt])
                            S.op("dve", lambda e, tf_=tf_, zp_=zp_, cc=cc, order=order, op_=op_: e.scalar_tensor_tensor(
                                out=tf_[:], in0=zp_[:], scalar=hb[:, cc, order:order + 1], in1=op_[cc][:], op0=ALU.mult, op1=ALU.add),
                                reads=[kzp, f"op{cc}", "hybias"], writes=[ktf])
                            S.op("pool", lambda e, zn_=zn_, tf_=tf_, gt_=gt_: e.tensor_tensor(out=zn_[:], in0=tf_[:], in1=gt_[:], op=ALU.mult),
                                 reads=[ktf, kgt], writes=[kzn])
                            if order == 0:
                                S.dma("pool", G["z1_d"][cc * 128:(cc + 1) * 128, t_lo:t_lo + tb], zn_[:], reads=[kzn], writes=[("z1_d", cc, t_lo)])
                                for i in range(tb // 128):
                                    S.op("pe", lambda e, i=i, zn_=zn_, tp_=tp_: e.transpose(out=tp_[:, i, :], in_=zn_[:, i * 128:(i + 1) * 128],
                                                                                       identity=G["ident_b"][:]),
                                         reads=[kzn, "ident_b"], writes=[ktp])
                                c0 = b * (tb // 128)
                                S.op("act", lambda e, tp_=tp_, cc=cc, c0=c0: e.copy(out=ztok[:, c0:c0 + tb // 128, cc * 128:(cc + 1) * 128],
                                                                                in_=tp_[:, 0:tb // 128, :]),
                                     reads=[ktp], writes=["ztok"])
                            else:
                                S.dma("pool", G["mix_d"][cc * 128:(cc + 1) * 128, t_lo:t_lo + tb], zn_[:], reads=[kzn], writes=[("mix_d", cc, t_lo)])


def qk_prep(C, G, pq, kpq, gain, kgain, cs, sn, tt, rope, dstT, kdst, nh, W, tag):
    S = C.S
    n = nh * 64
    sq, ss, xn, xg, t1, t2, qn, tp = W["sq"], W["ss"], W["xn"], W["xg"], W["t1"], W["t2"], W["qn"], W["tp"]
    k = lambda nm: (W.get("tpkey", (tag, "tp")) if nm == "tp" else (tag, nm))
    S.op("act", lambda e: e.activation(out=sq[:, :n], in_=pq, func=AF.Square), reads=[kpq], writes=[k("sq")])
    yield
    S.op("dve", lambda e: e.reduce_sum(out=ss[:, :nh], in_=sq[:, :n].rearrange("p (h d) -> p h d", d=64), axis=AX.X), reads=[k("sq")], writes=[k("ss")])
    yield
    S.op("act", lambda e: e.activation(out=ss[:, :nh], in_=ss[:, :nh], func=AF.Sqrt, scale=1.0 / 64, bias=EPS), reads=[k("ss")], writes=[k("ss")])
    yield
    S.op("dve", lambda e: e.reciprocal(out=ss[:, :nh], in_=ss[:, :nh]), reads=[k("ss")], writes=[k("ss")])
    yield
    S.op("dve", lambda e: e.tensor_tensor(out=xn[:, :n].rearrange("p (h d) -> p h d", d=64), in0=pq.rearrange("p (h d) -> p h d", d=64),
                                          in1=ss[:, :nh, None].to_broadcast([128, nh, 64]), op=ALU.mult), reads=[kpq, k("ss")], writes=[k("xn")])
    yield
    if rope:
        S.op("pool", lambda e: e.tensor_tensor(out=xg[:, :n].rearrange("p (h d) -> p h d", d=64), in0=xn[:, :n].rearrange("p (h d) -> p h d", d=64),
                                               in1=gain[:, None, :].to_broadcast([128, nh, 64]), op=ALU.mult), reads=[k("xn"), kgain], writes=[k("xg")])
        yield
        xv = xg[:, :n].rearrange("p (h a f) -> p h a f", a=2, f=32)
        x1, x2 = xv[:, :, :, 0:16], xv[:, :, :, 16:32]
        cb = cs[:, tt, None, :].rearrange("p o (a f) -> p o a f", a=2).to_broadcast([128, nh, 2, 16])
        sb_ = sn[:, tt, None, :].rearrange("p o (a f) -> p o a f", a=2).to_broadcast([128, nh, 2, 16])
        h2 = n // 2
        t1v = t1[:, :h2].rearrange("p (h a f) -> p h a f", a=2, f=16)
        t2v = t2[:, :h2].rearrange("p (h a f) -> p h a f", a=2, f=16)
        qv = qn[:, :n].rearrange("p (h a f) -> p h a f", a=2, f=32)
        S.op("dve", lambda e: e.tensor_tensor(out=t1v, in0=x1, in1=cb, op=ALU.mult), reads=[k("xg"), "rope"], writes=[k("t1")])
        yield
        S.op("pool", lambda e: e.tensor_tensor(out=t2v, in0=x2, in1=sb_, op=ALU.mult), reads=[k("xg"), "rope"], writes=[k("t2")])
        yield
        S.op("dve", lambda e: e.tensor_tensor(out=qv[:, :, :, 0:16], in0=t1v, in1=t2v, op=ALU.subtract), reads=[k("t1"), k("t2")], writes=[k("qn")])
        yield
        S.op("pool", lambda e: e.tensor_tensor(out=t1v, in0=x2, in1=cb, op=ALU.mult), reads=[k("xg"), "rope", k("qn")], writes=[k("t1")])
        yield
        S.op("dve", lambda e: e.tensor_tensor(out=t2v, in0=x1, in1=sb_, op=ALU.mult), reads=[k("xg"), "rope", k("qn")], writes=[k("t2")])
        yield
        S.op("pool", lambda e: e.tensor_tensor(out=qv[:, :, :, 16:32], in0=t1v, in1=t2v, op=ALU.add), reads=[k("t1"), k("t2")], writes=[k("qn")])
        yield
    else:
        S.op("pool", lambda e: e.tensor_tensor(out=qn[:, :n].rearrange("p (h d) -> p h d", d=64), in0=xn[:, :n].rearrange("p (h d) -> p h d", d=64),
                                               in1=gain[:, None, :].to_broadcast([128, nh, 64]), op=ALU.mult), reads=[k("xn"), kgain], writes=[k("qn")])
        yield
    nj = n // 128
    for j in range(nj):
        S.op("pe", lambda e, j=j: e.transpose(out=tp[:, j, :], in_=qn[:, j * 128:(j + 1) * 128], identity=G["ident_b"][:]),
             reads=[k("qn"), "ident_b"], writes=[k("tp")])
    S.op("act", lambda e: e.copy(out=dstT[:, 0:nj, tt * 128:(tt + 1) * 128], in_=tp[:, 0:nj, :]), reads=[k("tp")], writes=[kdst])
    yield


def run_rr(gens):
    gens = list(gens)
    while gens:
        for g_ in list(gens):
            try:
                next(g_)
            except StopIteration:
                gens.remove(g_)

def qk_work(C, tag, tp=None):
    return {"sq": C.sb(tag + "sq", [128, 512], F32), "ss": C.sb(tag + "ss", [128, 8], F32), "xn": C.sb(tag + "xn", [128, 512], F32),
            "xg": C.sb(tag + "xg", [128, 512], F32), "t1": C.sb(tag + "t1", [128, 256], F32), "t2": C.sb(tag + "t2", [128, 256], F32),
            "qn": C.sb(tag + "qn", [128, 512], BF16), "tp": tp if tp is not None else C.ps(tag + "tp", [128, 4, 128], BF16)}


def phase_diff_attn(C, G):
    S = C.S
    aT = G["aT"]
    LAM_INIT = 0.8 - 0.6 * math.exp(0.0)
    with C.phase():
        qT = C.sb("qT", [128, 4, T], BF16)
        kT = C.sb("kT", [128, 4, T], BF16)
        vx = C.sb("vx", [128, NT, 4, 129], BF16)
        gq = C.sb("gq", [128, 64], F32)
        gk = C.sb("gk", [128, 64], F32)
        cs = C.sb("ropec", [128, 16, 32], F32)
        sn = C.sb("ropes", [128, 16, 32], F32)
        sub = C.sb("subln", [128, 128], F32)
        lamb = C.sb("lamb", [128, 4, 64], F32)
        lw = C.sb("lamw", [128, 8], F32)
        S.dma("sp", gq[:], G["da_q_norm"][0:1, :].to_broadcast([128, 64]), writes=["gq"])
        S.dma("sp", gk[:], G["da_k_norm"][0:1, :].to_broadcast([128, 64]), writes=["gk"])
        S.dma("sp", sub[:], G["da_subln"][0:1, :].to_broadcast([128, 128]), writes=["subln"])
        S.dma("sp", lamb[:].rearrange("p a b -> p (a b)"), G["da_lam"].rearrange("o a b -> o (a b)").to_broadcast([128, 256]), writes=["lamb"])
        S.dma("sp", cs[:], G["c_cos"].rearrange("(t p) f -> p t f", p=128), writes=["rope"])
        S.dma("sp", sn[:], G["c_sin"].rearrange("(t p) f -> p t f", p=128), writes=["rope"])
        S.op("dve", lambda e: e.tensor_scalar(out=gq[:], in0=gq[:], scalar1=0.125, scalar2=None, op0=ALU.mult), reads=["gq"], writes=["gq"])
        S.op("dve", lambda e: e.tensor_scalar(out=sub[:], in0=sub[:], scalar1=1.0 - LAM_INIT, scalar2=None, op0=ALU.mult), reads=["subln"], writes=["subln"])
        S.op("dve", lambda e: e.tensor_tensor(out=lamb[:, 0, :], in0=lamb[:, 0, :], in1=lamb[:, 1, :], op=ALU.mult), reads=["lamb"], writes=["lamb"])
        S.op("dve", lambda e: e.tensor_tensor(out=lamb[:, 2, :], in0=lamb[:, 2, :], in1=lamb[:, 3, :], op=ALU.mult), reads=["lamb"], writes=["lamb"])
        S.op("dve", lambda e: e.reduce_sum(out=lw[:, 0:1], in_=lamb[:, 0, :], axis=AX.X), reads=["lamb"], writes=["lamw"])
        S.op("dve", lambda e: e.reduce_sum(out=lw[:, 1:2], in_=lamb[:, 2, :], axis=AX.X), reads=["lamb"], writes=["lamw"])
        S.op("act", lambda e: e.activation(out=lw[:, 2:4], in_=lw[:, 0:2], func=AF.Exp), reads=["lamw"], writes=["lamw"])
        S.op("dve", lambda e: e.tensor_tensor(out=lw[:, 4:5], in0=lw[:, 3:4], in1=lw[:, 2:3], op=ALU.subtract), reads=["lamw"], writes=["lamw"])
        S.op("dve", lambda e: e.tensor_scalar(out=lw[:, 5:6], in0=lw[:, 4:5], scalar1=-LAM_INIT, scalar2=None, op0=ALU.add), reads=["lamw"], writes=["lamw"])
        S.op("pool", lambda e: e.memset(vx[:, :, :, 128:129], 1.0), writes=["vx1"])
        with C.phase():
            w = C.sb("wqkv", [128, 8, 1536], BF16)
            S.dma("pool", w[:], G["e_w_in"][0, :, 1536:3072].rearrange("(k p) n -> p k n", p=128), writes=["wqkv"])
            pq = [C.ps(f"pq{i}", [128, 512], F32) for i in range(2)]
            pk = [C.ps(f"pk{i}", [128, 512], F32) for i in range(2)]
            pv = C.ps("pv", [128, 512], F32)
            tps_ = [C.ps(f"qktp{i}", [128, 4, 128], BF16) for i in range(2)]
            Wq = [qk_work(C, f"wq{i}", tps_[i]) for i in range(2)]
            Wk = [qk_work(C, f"wk{i}", tps_[i]) for i in range(2)]
            for i in range(2):
                Wq[i]["tpkey"] = Wk[i]["tpkey"] = f"qktp{i}"
            for t2 in range(0, NT, 2):
                gens = []
                for tt in (t2, t2 + 1):
                    q_, k_ = pq[tt % 2], pk[tt % 2]
                    for (dst, kd, c0) in ((q_, f"pq{tt % 2}", 0), (k_, f"pk{tt % 2}", 512), (pv, "pv", 1024)):
                        for k in range(8):
                            S.op("pe", lambda e, dst=dst, k=k, c0=c0, tt=tt: e.matmul(dst[:], lhsT=aT[:, k, tt * 128:(tt + 1) * 128], rhs=w[:, k, c0:c0 + 512],
                                                                                 start=(k == 0), stop=(k == 7)),
                                 reads=["wqkv"], writes=[kd])
                    S.op("act", lambda e, tt=tt: e.copy(out=vx[:, tt, :, 0:128], in_=pv[:].rearrange("p (h d) -> p h d", d=128)), reads=["pv"], writes=["vx"])
                    rope = tt < 16
                    gens.append(qk_prep(C, G, q_[:], f"pq{tt % 2}", gq, "gq", cs, sn, tt, rope, qT, "qT", 8, Wq[tt % 2], f"wq{tt % 2}"))
                    gens.append(qk_prep(C, G, k_[:], f"pk{tt % 2}", gk, "gk", cs, sn, tt, rope, kT, "kT", 8, Wk[tt % 2], f"wk{tt % 2}"))
                run_rr(gens)
        if G.get("dbg_qk"):
            for nm, tl in (("qT", qT), ("kT", kT)):
                dd = C.nc.dram_tensor("dbgq_" + nm, [128, 4 * T], BF16, kind="ExternalOutput").ap()
                G.setdefault("final_ops2", []).append(S.dma("sp", dd, tl[:].rearrange("p a b -> p (a b)"), reads=[nm]))
        with C.phase():
            sp = [C.ps(f"sp{i}", [128, 512], F32) for i in range(2)]
            oacc = [[C.ps(f"oacc{s_}_{b}", [128, 2, 129], F32) for b in range(2)] for s_ in range(2)]
            tpo = C.ps("tpo", [128, 4, 128], BF16)
            pt = [C.sb(f"pt{i}", [128, 512], BF16) for i in range(3)]
            osb2 = [[C.sb(f"osb{b}_{m}", [128, 4, 128], F32) for m in range(2)] for b in range(2)]
            rc2 = [C.sb(f"rc{b}", [128, 8], F32) for b in range(2)]
            dsc = C.sb("dsc", [128, 128], F32)
            on2 = [C.sb(f"on{b}", [128, 4, 128], BF16) for b in range(2)]
            pending = []
            oT = [C.sb(f"oT{i}", [128, 512], BF16) for i in range(2)]
            its = []
            for h in range(4):
                for (q0, nq, kts) in ((0, 512, range(NT)), (512, 512, range(NT)), (1024, 512, range(NT)), (1536, 512, range(NT)),
                                      (2048, 256, (16, 17))):
                    kts = list(kts)
                    for m in range(2):
                        for kt in kts:
                            its.append(dict(h=h, q0=q0, nq=nq, m=m, kt=kt, first=(kt == kts[0]), last=(kt == kts[-1])))

            def emit_s(i):
                d_ = its[i]
                s_, ks_ = sp[i % 2], f"sp{i % 2}"
                p_, kp_ = pt[i % 3], f"pt{i % 3}"
                pb = slice(d_["m"] * 64, (d_["m"] + 1) * 64)
                h, kt, q0, nq = d_["h"], d_["kt"], d_["q0"], d_["nq"]
                S.op("pe", lambda e, s_=s_, pb=pb, h=h, kt=kt, q0=q0, nq=nq: e.matmul(
                    s_[:, :nq], lhsT=kT[pb, h, kt * 128:(kt + 1) * 128], rhs=qT[pb, h, q0:q0 + nq], start=True, stop=True),
                    reads=["qT", "kT"], writes=[ks_])
                S.op("act", lambda e, s_=s_, p_=p_, nq=nq: e.activation(out=p_[:, :nq], in_=s_[:, :nq], func=AF.Exp), reads=[ks_], writes=[kp_])

            ib = 0
            rnd = [-1]
            emit_s(0)
            for i in range(len(its)):
                if i + 1 < len(its):
                    emit_s(i + 1)
                d_ = its[i]
                h, kt, q0, nq, m = d_["h"], d_["kt"], d_["q0"], d_["nq"], d_["m"]
                nqs = nq // 128
                p_, kp_ = pt[i % 3], f"pt{i % 3}"
                if d_["first"]:
                    rnd[0] += 1
                ss_ = rnd[0] % 2
                for qs in range(nqs):
                    S.op("pe", lambda e, qs=qs, p_=p_, kt=kt, h=h, ss_=ss_, f_=(d_["first"] and qs % 2 == 0), l_=d_["last"]: e.matmul(
                        oacc[ss_][qs // 2][:, qs % 2, :], lhsT=p_[:, qs * 128:(qs + 1) * 128], rhs=vx[:, kt, h, :], start=f_, stop=l_, skip_group_check=True),
                        reads=[kp_, "vx", "vx1"], writes=[f"oacc{ss_}_{qs // 2}"])
                if pending and pending[0][0] <= i:
                    pending.pop(0)[1]()
                if not d_["last"]:
                    continue
                bsel = ib % 2
                osb, rc, on = osb2[bsel], rc2[bsel], on2[bsel]
                ko = [f"osb{bsel}_0", f"osb{bsel}_1"]
                krc, kon = f"rc{bsel}", f"on{bsel}"
                for b in range(nqs // 2):
                    acc, kacc = oacc[ss_][b], f"oacc{ss_}_{b}"
                    c0 = m * 4 + 2 * b
                    S.op("dve", lambda e, acc=acc, rc=rc, c0=c0: e.reciprocal(out=rc[:, c0:c0 + 2], in_=acc[:, :, 128]),
                         reads=[kacc], writes=[krc])
                    S.op("dve", lambda e, acc=acc, rc=rc, c0=c0, osb=osb, m=m, b=b: e.tensor_tensor(
                        out=osb[m][:, 2 * b:2 * b + 2, :], in0=acc[:, :, 0:128], in1=rc[:, c0:c0 + 2, None].to_broadcast([128, 2, 128]), op=ALU.mult),
                        reads=[kacc, krc], writes=[ko[m]])
                if m == 0:
                    continue
                o_T, ko_T = oT[ib % 2], f"oT{ib % 2}"
                ib += 1
                for qs in range(nqs):
                    S.op("dve", lambda e, qs=qs, osb=osb: e.scalar_tensor_tensor(out=osb[0][:, qs, :], in0=osb[1][:, qs, :], scalar=lw[:, 5:6], in1=osb[0][:, qs, :],
                                                                                op0=ALU.mult, op1=ALU.add),
                         reads=ko + ["lamw"], writes=[ko[0]])
                    S.op("act", lambda e, qs=qs, osb=osb, rc=rc: e.activation(out=dsc[:], in_=osb[0][:, qs, :], func=AF.Square, accum_out=rc[:, qs:qs + 1]),
                         reads=[ko[0], krc], writes=["dsc", krc])
                    S.op("act", lambda e, qs=qs, rc=rc: e.activation(out=rc[:, qs:qs + 1], in_=rc[:, qs:qs + 1], func=AF.Sqrt, scale=1.0 / 128, bias=EPS),
                         reads=[krc], writes=[krc])
                    S.op("dve", lambda e, qs=qs, rc=rc: e.reciprocal(out=rc[:, qs:qs + 1], in_=rc[:, qs:qs + 1]), reads=[krc], writes=[krc])
                    S.op("dve", lambda e, qs=qs, rc=rc, osb=osb, on=on: e.scalar_tensor_tensor(out=on[:, qs, :], in0=osb[0][:, qs, :], scalar=rc[:, qs:qs + 1], in1=sub[:],
                                                                                            op0=ALU.mult, op1=ALU.mult),
                         reads=[ko[0], krc, "subln"], writes=[kon])

                def fin_(nqs=nqs, on=on, kon=kon, o_T=o_T, ko_T=ko_T, h=h, q0=q0, nq=nq):
                    for qs in range(nqs):
                        S.op("pe", lambda e, qs=qs, on=on: e.transpose(out=tpo[:, qs, :], in_=on[:, qs, :], identity=G["ident_b"][:]), reads=[kon, "ident_b"], writes=["tpo"])
                    S.op("act", lambda e, o_T=o_T, nqs=nqs: e.copy(out=o_T[:, :nqs * 128].rearrange("p (a b) -> p a b", b=128), in_=tpo[:, 0:nqs, :]),
                         reads=["tpo"], writes=[ko_T])
                    S.dma("sp", G["mix_d"][512 + h * 128:512 + (h + 1) * 128, q0:q0 + nq], o_T[:, :nq], reads=[ko_T], writes=[("mix_d", "a", h, q0)])
                pending.append((i + 6, fin_))
            for _, f_ in pending:
                f_()


def phase_outproj(C, G, l, w_name, ntok):
    S = C.S
    hT, aT, modc = G["hT"], G["aT"], G["modc"]
    with C.phase():
        w = C.sb("wout", [128, 8, D], BF16)
        S.dma("pool", w[:], G[w_name][0].rearrange("(k p) n -> p k n", p=128), writes=["wout"])
        for k in range(8):
            S.dma("sp", aT[:, k, 0:ntok], G["mix_d"][k * 128:(k + 1) * 128, 0:ntok], writes=[("aTm", k)])
        akeys = [("aTm", k) for k in range(8)]
        pp = [C.ps(f"opp{i}", [128, 512], F32) for i in range(4)]
        ip = 0
        for (t0, n, s) in token_blocks():
            if t0 >= ntok:
                continue
            hkeys = [("hT", tt) for tt in range(t0 // 128, (t0 + n) // 128)]
            for j in range(8):
                p, kp = pp[ip % 4], f"opp{ip % 4}"
                ip += 1
                for k in range(8):
                    S.op("pe", lambda e, p=p, j=j, k=k, t0=t0, n=n: e.matmul(p[:, :n], lhsT=w[:, k, j * 128:(j + 1) * 128], rhs=aT[:, k, t0:t0 + n],
                                                                         start=(k == 0), stop=(k == 7)),
                         reads=["wout"] + akeys, writes=[kp])
                S.op("dve", lambda e, p=p, j=j, t0=t0, n=n, s=s: e.scalar_tensor_tensor(
                    out=hT[:, j, t0:t0 + n], in0=p[:, :n], scalar=modc[:, l, 16 + j, s:s + 1], in1=hT[:, j, t0:t0 + n], op0=ALU.mult, op1=ALU.add),
                    reads=[kp, "modc"] + hkeys, writes=hkeys)


def spill_h(C, G, to_dram):
    S = C.S
    with C.phase():
        for k in range(8):
            if to_dram:
                S.dma("sp", G["hT_d"][:, k * T:(k + 1) * T], G["hT"][:, k, :])
            else:
                S.dma("sp", G["hT"][:, k, :], G["hT_d"][:, k * T:(k + 1) * T])


def build_program(stages=("load", "mods", "l0mix", "l0moe", "l1mix", "l1moe", "store"), debug=False, dbg_moe=None, EG=2, odd_parts="rg", l0parts="iafc"):
    nc = bass.Bass("TRN2", target_bir_lowering=False)
    C = Ctx(nc)
    G = {"dbg_moe": dbg_moe, "EG": EG, "dbg_qk": bool(debug), "odd_parts": odd_parts, "l0parts": l0parts}
    dbgk = {"kind": "ExternalOutput"} if debug else {}

    def din(name, shape, dtype=F32):
        G[name] = nc.dram_tensor(name, list(shape), dtype, kind="ExternalInput").ap()

    def dscr(name, shape, dtype):
        G[name] = nc.dram_tensor(name, list(shape), dtype, **dbgk).ap()

    din("x", [SEQ, D]); din("ctx", [CTX, D]); din("c2", [2, D])
    din("ada_w", [2, D, 6 * D]); din("ada_b", [2, 6 * D])
    din("e_w_in", [1, D, 3072]); din("e_w_out", [1, D, D])
    din("hy_conv_w", [1, 3, 1536]); din("hy_conv_b", [1, 1536])
    din("hy_f_w1", [1, 33, 64]); din("hy_f_b1", [1, 64]); din("hy_f_w2", [1, 64, 64]); din("hy_f_b2", [1, 64])
    din("hy_f_w3", [1, 64, 2048]); din("hy_f_freq", [1, 2, 64]); din("hy_bias", [1, 2, 512])
    din("da_q_norm", [1, 64]); din("da_k_norm", [1, 64]); din("da_lam", [1, 4, 64]); din("da_subln", [1, 128])
    din("o_w_in", [1, D, 2816]); din("o_w_out", [1, D, D])
    din("ret_decay", [1, 2, 4]); din("ret_gn", [1, 512]); din("gq_q_norm", [1, 64]); din("gq_k_norm", [1, 64]); din("gq_sink", [1, 8])
    din("moe_w_grp", [2, D, 4]); din("moe_b_grp", [2, 4]); din("moe_w_rt", [2, D, 32]); din("moe_b_rt", [2, 32])
    din("moe_w_gate", [2, 4, 8, D, DEXP]); din("moe_w_up", [2, 4, 8, D, DEXP]); din("moe_w_down", [2, 4, 8, DEXP, D])
    for name, (shape, dtype) in CONST_SPECS.items():
        din(name, shape, dtype)
    G["out"] = nc.dram_tensor("out", [SEQ, D], F32, kind="ExternalOutput").ap()
    dscr("combT_d", [NEXP, T], BF16)
    dscr("hyc_d", [1536, T], BF16)
    dscr("vtok_d", [T, 512], BF16)
    dscr("filt_d", [2, 2, SEQ, 512], BF16)
    dscr("filtc_d", [2, 2, CTX, 512], BF16)
    dscr("z1_d", [512, T], BF16)
    dscr("mix_d", [D, T], BF16)
    dscr("hT_d", [128, 8 * T], F32)

    S = C.S
    with contextlib.ExitStack() as st0:
        C.stack = st0
        G["modc"] = C.sb("modc", [128, 2, 48, 2], F32)
        G["ident_f"] = C.sb("ident_f", [128, 128], F32)
        G["ident_b"] = C.sb("ident_b", [128, 128], BF16)
        G["ones_f"] = C.sb("ones_f", [128, 128], F32)
        S.dma("sp", G["ident_f"][:], G["c_ident_f"], writes=["ident_f"])
        S.dma("sp", G["ident_b"][:], G["c_ident_b"], writes=["ident_b"])
        S.op("pool", lambda e: e.memset(G["ones_f"][:], 1.0), writes=["ones_f"])

        def open_scopes():
            stA = contextlib.ExitStack()
            C.stack = stA
            G["aT"] = C.sb("aT", [128, 8, T], BF16)
            stH = contextlib.ExitStack()
            C.stack = stH
            G["hT"] = C.sb("hT", [128, 8, T], F32)
            return stA, stH

        l0mix = "l0mix" in stages
        l1mix = "l1mix" in stages
        stA, stH = open_scopes()
        if "mods" in stages:
            phase_mods(C, G, mid=(lambda: phase_load(C, G)) if "load" in stages else None)
        elif "load" in stages:
            phase_load(C, G)
        if l0mix:
            phase_norm(C, G, 0, 0)
        spill_h(C, G, True)
        stH.close()
        C.stack = stA
        lp = G.get("l0parts", "iafc")
        if l0mix:
            if "i" in lp:
                phase_hy_inproj(C, G)
            if "a" in lp:
                phase_diff_attn(C, G)
            if "f" in lp:
                phase_hy_filter(C, G, SEQ, "c_zemb_2048", "filt_d")
                phase_hy_filter(C, G, CTX, "c_zemb_256", "filtc_d")
        S.barrier()
        stA.close()
        if l0mix and "c" in lp:
            C.stack = st0
            phase_hyena_conv(C, G, SEQ, 0, "filt_d", "c_F2048", "c_G2048", "vtok_d")
            phase_hyena_conv(C, G, CTX, SEQ, "filtc_d", "c_F256", "c_G256", "vtok_d")
        stA, stH = open_scopes()
        spill_h(C, G, False)
        if "dbgnorm" in stages:
            phase_norm(C, G, 0, 0)
        if l0mix:
            phase_outproj(C, G, 0, "e_w_out", T)
        if "l0moe" in stages:
            phase_norm(C, G, 0, 1, router=True)
            phase_moe(C, G, 0, T)
        fin = []
        dbg_done = False

        def dbg_dump():
            if debug:
                for name in debug:
                    t = G[name]
                    shp = list(t.shape)
                    dd = nc.dram_tensor("dbg_" + name, [shp[0], int(np.prod(shp[1:]))], t.dtype, kind="ExternalOutput").ap()
                    src = t[:]
                    if len(shp) == 3:
                        src = src.rearrange("p a b -> p (a b)")
                    elif len(shp) == 4:
                        src = src.rearrange("p a b c -> p (a b c)")
                    fin.append(S.dma("sp", dd, src))
                S.barrier()

        if l1mix:
            phase_norm(C, G, 1, 0)
            spill_h(C, G, True)
            stH.close()
            C.stack = stA
            phase_odd_mixer(C, G)
            S.barrier()
            stA.close()
            stA, stH = open_scopes()
            spill_h(C, G, False)
            phase_outproj(C, G, 1, "o_w_out", SEQ)
        if "l1moe" in stages:
            phase_norm(C, G, 1, 1, router=True, ntok=SEQ)
            phase_moe(C, G, 1, SEQ)
        if "store" in stages:
            phase_store(C, G)
        dbg_dump()
        fin = list(G.get("final_ops", ())) + list(G.get("final_ops2", ())) + fin
        S.barrier()
        stH.close()
        stA.close()
        counts = S.emit(final_wait_ops=fin)
    return nc, counts


def _dft_consts(L):
    N = 2 * L
    k = np.arange(L, dtype=np.float64)
    s = np.arange(L, dtype=np.float64)
    th = 2.0 * np.pi * (k + 0.5) / N
    ang = np.outer(s, th)
    F = np.concatenate([np.cos(ang), -np.sin(ang)], axis=1)
    nsc = L // 128
    Ft = F.reshape(nsc, 128, 2 * nsc, 128).transpose(2, 1, 0, 3)
    Gm = F.T / L
    tb = min(L, 512)
    ntb = L // tb
    KG = 4
    ng = 2 * nsc // KG
    Gt = Gm.reshape(ng, KG, 128, ntb, tb).transpose(3, 0, 2, 1, 4)
    return np.ascontiguousarray(Ft).astype(ml_dtypes.bfloat16), np.ascontiguousarray(Gt).astype(ml_dtypes.bfloat16)


def _zemb(L):
    t = np.linspace(0.0, 1.0, L)[:, None]
    w = (2.0 * np.pi / L) * np.arange(L)[:, None]
    fb = np.linspace(1e-4, 15.0, 16)[None, :]
    z = np.concatenate([t, np.cos(fb * w), -np.sin(fb * w)], axis=-1)
    return np.ascontiguousarray(z.T).astype(np.float32)


def _negt(L):
    t = np.linspace(0.0, 1.0, L)
    return np.ascontiguousarray(-t.reshape(L // 128, 128).T).astype(np.float32)


CONST_SPECS = {
    "c_ident_f": ([128, 128], F32), "c_ident_b": ([128, 128], BF16),
    "c_cos": ([SEQ, 32], F32), "c_sin": ([SEQ, 32], F32),
    "c_delta": ([1, 512], F32), "c_negt_2048": ([128, 16], F32), "c_negt_256": ([128, 2], F32), "c_mask0": ([128, 1], F32),
    "c_zemb_2048": ([33, SEQ], F32), "c_zemb_256": ([33, CTX], F32),
    "c_F2048": ([32, 128, 16, 128], BF16), "c_G2048": ([4, 8, 128, 4, 512], BF16),
    "c_F256": ([4, 128, 2, 128], BF16), "c_G256": ([1, 1, 128, 4, 256], BF16),
    "c_cos_rt": ([SEQ, 64], F32), "c_sin_rt": ([SEQ, 64], F32), "c_ret": ([5, 128, 128], F32), "c_retcol": ([128, 2], F32),
    "c_gmask": ([2, 128, 128], BF16),
}
_CONSTS = None


def host_constants():
    global _CONSTS
    if _CONSTS is not None:
        return _CONSTS
    c = {"c_ident_f": np.eye(128, dtype=np.float32), "c_ident_b": np.eye(128).astype(ml_dtypes.bfloat16)}
    tok = np.arange(SEQ)
    inv = 10000.0 ** (-np.arange(16, dtype=np.float64) / 16)
    ang = np.concatenate([(tok // 64)[:, None] * inv[None, :], (tok % 64)[:, None] * inv[None, :]], axis=1)
    c["c_cos"] = np.cos(ang).astype(np.float32)
    c["c_sin"] = np.sin(ang).astype(np.float32)
    mx, mn = math.log(1e-2) / 0.3, math.log(1e-2) / 1.5
    c["c_delta"] = np.abs(np.linspace(mn, mx, 512)).astype(np.float32)[None, :]
    c["c_negt_2048"] = _negt(SEQ)
    c["c_negt_256"] = _negt(CTX)
    m0 = np.ones((128, 1), np.float32); m0[0, 0] = 0.0
    c["c_mask0"] = m0
    c["c_zemb_2048"] = _zemb(SEQ)
    c["c_zemb_256"] = _zemb(CTX)
    c["c_F2048"], c["c_G2048"] = _dft_consts(SEQ)
    c["c_F256"], c["c_G256"] = _dft_consts(CTX)
    inv_rt = 1.0 / (10000.0 ** np.linspace(0.0, 1.0, 64))
    ang_rt = np.arange(SEQ, dtype=np.float64)[:, None] * inv_rt[None, :]
    c["c_cos_rt"] = np.cos(ang_rt).astype(np.float32)
    c["c_sin_rt"] = np.sin(ang_rt).astype(np.float32)
    j = np.arange(128)[:, None]; i = np.arange(128)[None, :]
    relf = np.maximum(i - j, 0); maskf = (i >= j)
    relb = np.maximum(j - i, 0); maskb = (j >= i)
    iota1 = np.broadcast_to(np.arange(1, 129)[None, :], (128, 128))
    c["c_ret"] = np.stack([relf, maskf, relb, maskb, iota1]).astype(np.float32)
    c["c_retcol"] = np.stack([127 - np.arange(128), np.arange(128)], axis=1).astype(np.float32)
    c["c_gmask"] = np.stack([(i <= j), (j <= i)]).astype(ml_dtypes.bfloat16)
    _CONSTS = c
    return c


WEIGHT_KEYS = ["ada_w", "ada_b", "e_w_in", "e_w_out", "hy_conv_w", "hy_conv_b", "hy_f_w1", "hy_f_b1", "hy_f_w2", "hy_f_b2", "hy_f_w3",
               "hy_f_freq", "hy_bias", "da_q_norm", "da_k_norm", "da_lam", "da_subln", "o_w_in", "o_w_out", "ret_decay", "ret_gn",
               "gq_q_norm", "gq_k_norm", "gq_sink", "moe_w_grp", "moe_b_grp", "moe_w_rt", "moe_b_rt", "moe_w_gate", "moe_w_up", "moe_w_down"]


def make_in_maps(inputs, ncores=8):
    consts = host_constants()
    maps = []
    for b in range(ncores):
        m = {"x": np.ascontiguousarray(inputs["x"][b]), "ctx": np.ascontiguousarray(inputs["ctx"][b]),
             "c2": np.ascontiguousarray(np.stack([inputs["c"][b], inputs["c_ctx"]], axis=0))}
        for k in WEIGHT_KEYS:
            m[k] = np.ascontiguousarray(inputs[k])
        m.update(consts)
        maps.append(m)
    return maps


def kernel(**inputs):
    inputs = {k: np.asarray(v) for k, v in inputs.items()}
    nc, _ = build_program()
    in_maps = make_in_maps(inputs, 8)
    res = run_bass_kernel_spmd(nc, in_maps, core_ids=list(range(8)))
    return np.stack([np.asarray(r["out"]) for r in res.results], axis=0).astype(np.float32)


def phase_odd_mixer(C, G):
    parts = G.get("odd_parts", "rg")
    if "r" in parts:
        phase_retention(C, G)
    if "g" in parts:
        phase_gqa(C, G)


def phase_retention(C, G):
    S = C.S
    aT = G["aT"]
    RS = 128 ** -0.5
    with C.phase():
        qT = C.sb("rqT", [128, 4, SEQ], BF16)
        kT = C.sb("rkT", [128, 4, T], BF16)
        ktok = C.sb("rktok", [128, NT, 4, 128], BF16)
        vv = C.sb("rv", [128, NT, 512], BF16)
        sg = C.sb("rsg", [128, 16, 512], BF16)
        with C.phase():
            w = C.sb("wret", [128, 8, 2048], BF16)
            S.dma("pool", w[:], G["o_w_in"][0, :, 0:2048].rearrange("(k p) n -> p k n", p=128), writes=["wret"])
            cs = C.sb("rtc", [128, 16, 64], F32)
            sn = C.sb("rts", [128, 16, 64], F32)
            S.dma("sp", cs[:], G["c_cos_rt"].rearrange("(t p) f -> p t f", p=128), writes=["rtrope"])
            S.dma("sp", sn[:], G["c_sin_rt"].rearrange("(t p) f -> p t f", p=128), writes=["rtrope"])
            pp = [C.ps(f"rpp{i}", [128, 512], F32) for i in range(4)]
            tp = [C.ps(f"rtp{i}", [128, 4, 128], BF16) for i in range(2)]
            xs = [[C.sb(f"rxs{p}{i}", [128, 512], F32) for i in range(2)] for p in range(2)]
            t1 = [[C.sb(f"rt1{p}{i}", [128, 256], F32) for i in range(2)] for p in range(2)]
            t2 = [[C.sb(f"rt2{p}{i}", [128, 256], F32) for i in range(2)] for p in range(2)]
            qn = [C.sb(f"rqn{p}", [128, 512], BF16) for p in range(2)]

            def chain(tt, which):
                lat = tt < 16
                p = tt % 2
                x_, kx = xs[p][which], f"rxs{p}{which}"
                a_, ka = t1[p][which], f"rt1{p}{which}"
                b_, kb = t2[p][which], f"rt2{p}{which}"
                dst = qn[p][:] if which == 0 else ktok[:, tt].rearrange("p h d -> p (h d)")
                kdst = f"rqn{p}" if which == 0 else ("rktok", tt)
                if lat:
                    S.op("act", lambda e: e.activation(out=x_[:], in_=pp[which][:], func=AF.Identity, scale=(1.0 if which == 0 else RS)),
                         reads=[f"rpp{which}"], writes=[kx])
                    yield
                    xv = x_[:].rearrange("p (h a f) -> p h a f", a=2, f=64)
                    x1, x2 = xv[:, :, 0, :], xv[:, :, 1, :]
                    cb = cs[:, tt, None, :].to_broadcast([128, 4, 64])
                    sb_ = sn[:, tt, None, :].to_broadcast([128, 4, 64])
                    av = a_[:].rearrange("p (h f) -> p h f", f=64)
                    bv = b_[:].rearrange("p (h f) -> p h f", f=64)
                    dv_ = dst.rearrange("p (h a f) -> p h a f", a=2, f=64)
                    S.op("dve", lambda e: e.tensor_tensor(out=av, in0=x1, in1=cb, op=ALU.mult), reads=[kx, "rtrope"], writes=[ka])
                    yield
                    S.op("pool", lambda e: e.tensor_tensor(out=bv, in0=x2, in1=sb_, op=ALU.mult), reads=[kx, "rtrope"], writes=[kb])
                    yield
                    S.op("dve", lambda e: e.tensor_tensor(out=dv_[:, :, 0, :], in0=av, in1=bv, op=ALU.subtract), reads=[ka, kb], writes=[kdst])
                    yield
                    S.op("pool", lambda e: e.tensor_tensor(out=av, in0=x2, in1=cb, op=ALU.mult), reads=[kx, "rtrope"], writes=[ka])
                    yield
                    S.op("dve", lambda e: e.tensor_tensor(out=bv, in0=x1, in1=sb_, op=ALU.mult), reads=[kx, "rtrope"], writes=[kb])
                    yield
                    S.op("pool", lambda e: e.tensor_tensor(out=dv_[:, :, 1, :], in0=av, in1=bv, op=ALU.add), reads=[ka, kb], writes=[kdst])
                    yield
                else:
                    S.op("act", lambda e: e.activation(out=dst, in_=pp[1][:], func=AF.Identity, scale=RS), reads=["rpp1"], writes=[kdst])
                    yield
                tp_, ktp = tp[which], f"rtp{which}"
                for j in range(4):
                    S.op("pe", lambda e, j=j: e.transpose(out=tp_[:, j, :], in_=dst[:, j * 128:(j + 1) * 128], identity=G["ident_b"][:]),
                         reads=[kdst, "ident_b"], writes=[ktp])
                dT, kdT = (qT, "rqT") if which == 0 else (kT, "rkT")
                S.op("act", lambda e: e.copy(out=dT[:, :, tt * 128:(tt + 1) * 128], in_=tp_[:]), reads=[ktp], writes=[kdT])
                yield

            for t2_ in range(0, NT, 2):
                gens = []
                for tt in (t2_, t2_ + 1):
                    lat = tt < 16
                    cols = ((0, 0), (1, 512), (2, 1024), (3, 1536)) if lat else ((1, 512), (2, 1024))
                    for (pi, c0) in cols:
                        for k in range(8):
                            S.op("pe", lambda e, pi=pi, c0=c0, k=k, tt=tt: e.matmul(pp[pi][:], lhsT=aT[:, k, tt * 128:(tt + 1) * 128], rhs=w[:, k, c0:c0 + 512],
                                                                               start=(k == 0), stop=(k == 7)),
                                 reads=["wret"], writes=[f"rpp{pi}"])
                    S.op("act", lambda e, tt=tt: e.copy(out=vv[:, tt, :], in_=pp[2][:]), reads=["rpp2"], writes=["rv"])
                    if lat:
                        S.op("act", lambda e, tt=tt: e.activation(out=sg[:, tt, :], in_=pp[3][:], func=AF.Silu), reads=["rpp3"], writes=["rsg"])
                    for which in ((0, 1) if lat else (1,)):
                        g_ = chain(tt, which)
                        next(g_)
                        gens.append(g_)
                run_rr(gens)
        with C.phase():
            lg = C.sb("rlg", [128, 8], F32)
            S.dma("sp", lg[:], G["ret_decay"].rearrange("o a b -> o (a b)").to_broadcast([128, 8]), writes=["rlg"])
            S.op("act", lambda e: e.activation(out=lg[:], in_=lg[:], func=AF.Exp, scale=-1.0), reads=["rlg"], writes=["rlg"])
            S.op("act", lambda e: e.activation(out=lg[:], in_=lg[:], func=AF.Ln, bias=1.0), reads=["rlg"], writes=["rlg"])
            S.op("dve", lambda e: e.tensor_scalar(out=lg[:], in0=lg[:], scalar1=-1.0, scalar2=None, op0=ALU.mult), reads=["rlg"], writes=["rlg"])
            cst = C.sb("rcst", [128, 5, 128], F32)
            S.dma("sp", cst[:], G["c_ret"].rearrange("a p f -> p a f"), writes=["rcst"])
            ccol = C.sb("rccol", [128, 2], F32)
            S.dma("sp", ccol[:], G["c_retcol"], writes=["rccol"])
            DT = C.sb("rDT", [128, 8, 128], F32)
            XI = C.sb("rXI", [128, 8, 128], F32)
            zc = C.sb("rzc", [128, 8], F32)
            g128 = C.sb("rg128", [128, 8], F32)
            xib = C.sb("rxib", [128, 128], F32)
            S.op("dve", lambda e: e.tensor_scalar(out=xib[:], in0=cst[:, 4, :], scalar1=-1.0, scalar2=129.0, op0=ALU.mult, op1=ALU.add), reads=["rcst"], writes=["rxib"])
            for dr in range(2):
                for h in range(4):
                    c = dr * 4 + h
                    S.op("act", lambda e, c=c, dr=dr: e.activation(out=DT[:, c, :], in_=cst[:, 2 * dr, :], func=AF.Exp, scale=lg[:, c:c + 1]),
                         reads=["rlg", "rcst"], writes=["rDT"])
                    S.op("dve", lambda e, c=c, dr=dr: e.tensor_tensor(out=DT[:, c, :], in0=DT[:, c, :], in1=cst[:, 2 * dr + 1, :], op=ALU.mult),
                         reads=["rDT", "rcst"], writes=["rDT"])
                    src = cst[:, 4, :] if dr == 0 else xib[:]
                    S.op("act", lambda e, c=c, src=src: e.activation(out=XI[:, c, :], in_=src, func=AF.Exp, scale=lg[:, c:c + 1]),
                         reads=["rlg", "rcst", "rxib"], writes=["rXI"])
                    S.op("act", lambda e, c=c, dr=dr: e.activation(out=zc[:, c:c + 1], in_=ccol[:, dr:dr + 1], func=AF.Exp, scale=lg[:, c:c + 1]),
                         reads=["rlg", "rccol"], writes=["rzc"])
            S.op("act", lambda e: e.activation(out=g128[:], in_=lg[:], func=AF.Exp, scale=128.0), reads=["rlg"], writes=["rg128"])
            oall = C.sb("roall", [128, 16, 512], F32)
            Sf = [C.sb(f"rSf{c}", [128, 128], F32) for c in range(8)]
            Sb = [[C.sb(f"rSb{c}_{i}", [128, 128], BF16) for i in range(2)] for c in range(8)]
            ap_ = [C.ps(f"rap{i}", [128, 128], F32) for i in range(3)]
            op_ = [C.ps(f"rop{i}", [128, 128], F32) for i in range(2)]
            up_ = [C.ps(f"rup{i}", [128, 128], F32) for i in range(2)]
            At = [C.sb(f"rAt{i}", [128, 128], BF16) for i in range(8)]
            qx = [C.sb(f"rqx{i}", [128, 128], BF16) for i in range(8)]
            kz = [C.sb(f"rkz{i}", [128, 128], BF16) for i in range(8)]
            for c in range(8):
                S.op("pool", lambda e, c=c: e.memset(Sf[c][:], 0.0), writes=[f"rSf{c}"])
                S.op("pool", lambda e, c=c: e.memset(Sb[c][0][:], 0.0), writes=[f"rSb{c}_0"])
            orders = {0: [16, 17] + list(range(16)), 1: [17, 16] + list(range(15, -1, -1))}
            ia = 0
            io = 0
            for step in range(18):
                chains = [(dr, h) for dr in range(2) for h in range(4)]
                for (dr, h) in chains:
                    ch = orders[dr][step]
                    c = dr * 4 + h
                    if ch < 16:
                        a3 = ia % 3
                        ia += 1
                        S.op("pe", lambda e, a3=a3, h=h, ch=ch: e.matmul(ap_[a3][:], lhsT=kT[:, h, ch * 128:(ch + 1) * 128], rhs=qT[:, h, ch * 128:(ch + 1) * 128],
                                                                   start=True, stop=True), reads=["rkT", "rqT"], writes=[f"rap{a3}"])
                        S.op("dve", lambda e, a3=a3, c=c: e.tensor_tensor(out=At[c][:], in0=ap_[a3][:], in1=DT[:, c, :], op=ALU.mult),
                             reads=[f"rap{a3}", "rDT"], writes=[f"rAt{c}"])
                        S.op("pool", lambda e, c=c, h=h, ch=ch: e.tensor_tensor(out=qx[c][:], in0=qT[:, h, ch * 128:(ch + 1) * 128], in1=XI[:, c, :], op=ALU.mult),
                             reads=["rqT", "rXI"], writes=[f"rqx{c}"])
                    if step < 17:
                        S.op("pool", lambda e, ch=ch, h=h, c=c: e.tensor_scalar(out=kz[c][:], in0=ktok[:, ch, h, :], scalar1=zc[:, c:c + 1], scalar2=None, op0=ALU.mult),
                             reads=[("rktok", ch), "rzc"], writes=[f"rkz{c}"])
                for (dr, h) in chains:
                    ch = orders[dr][step]
                    c = dr * 4 + h
                    cur, nxt = Sb[c][step % 2], Sb[c][(step + 1) % 2]
                    kcur, knxt = f"rSb{c}_{step % 2}", f"rSb{c}_{(step + 1) % 2}"
                    i2 = io % 2
                    io += 1
                    if ch < 16:
                        S.op("pe", lambda e, i2=i2, c=c, h=h, ch=ch: e.matmul(op_[i2][:], lhsT=At[c][:], rhs=vv[:, ch, h * 128:(h + 1) * 128], start=True, stop=False),
                             reads=[f"rAt{c}", "rv"], writes=[f"rop{i2}"])
                        S.op("pe", lambda e, i2=i2, c=c, cur=cur: e.matmul(op_[i2][:], lhsT=qx[c][:], rhs=cur[:], start=False, stop=True),
                             reads=[f"rqx{c}", kcur], writes=[f"rop{i2}"])
                        first = (ch + 2 < 17 - ch) if dr == 0 else (17 - ch < ch + 2)
                        if first:
                            S.op("act", lambda e, i2=i2, h=h, ch=ch: e.copy(out=oall[:, ch, h * 128:(h + 1) * 128], in_=op_[i2][:]),
                                 reads=[f"rop{i2}"], writes=[("roall", ch, h)])
                        else:
                            S.op("dve", lambda e, i2=i2, h=h, ch=ch: e.tensor_tensor(out=oall[:, ch, h * 128:(h + 1) * 128], in0=op_[i2][:],
                                                                                   in1=oall[:, ch, h * 128:(h + 1) * 128], op=ALU.add),
                                 reads=[f"rop{i2}", ("roall", ch, h)], writes=[("roall", ch, h)])
                    if step < 17:
                        S.op("pe", lambda e, i2=i2, c=c, ch=ch, h=h: e.matmul(up_[i2][:], lhsT=kz[c][:], rhs=vv[:, ch, h * 128:(h + 1) * 128], start=True, stop=True),
                             reads=[f"rkz{c}", "rv"], writes=[f"rup{i2}"])
                        S.op("dve", lambda e, i2=i2, c=c: e.scalar_tensor_tensor(out=Sf[c][:], in0=Sf[c][:], scalar=g128[:, c:c + 1], in1=up_[i2][:], op0=ALU.mult, op1=ALU.add),
                             reads=[f"rup{i2}", f"rSf{c}", "rg128"], writes=[f"rSf{c}"])
                        S.op("act", lambda e, c=c, nxt=nxt: e.copy(out=nxt[:], in_=Sf[c][:]), reads=[f"rSf{c}"], writes=[knxt])
            gn = C.sb("rgn", [128, 512], F32)
            S.dma("sp", gn[:], G["ret_gn"][0:1, :].to_broadcast([128, 512]), writes=["rgn"])
            st2 = [C.sb(f"rst{i}", [128, 8], F32) for i in range(2)]
            xc2 = [C.sb(f"rxc{i}", [128, 512], F32) for i in range(2)]
            sq2 = [C.sb(f"rsq{i}", [128, 512], F32) for i in range(2)]
            yb = [C.sb(f"ryb{i}", [128, 512], BF16) for i in range(2)]
            yT = [C.sb(f"ryT{i}", [128, 4, 128], BF16) for i in range(2)]
            tpo = C.ps("rtpo", [128, 4, 128], BF16)

            def gn_chain(ch):
                p = ch % 2
                st, xc, sq = st2[p], xc2[p], sq2[p]
                kst, kxc, ksq = f"rst{p}", f"rxc{p}", f"rsq{p}"
                ok = [("roall", ch, h) for h in range(4)]
                o3 = oall[:, ch, :].rearrange("p (h d) -> p h d", d=128)
                y_, ky = yb[p], f"ryb{p}"
                t_, kt_ = yT[p], f"ryT{p}"
                S.op("dve", lambda e: e.reduce_sum(out=st[:, 0:4], in_=o3, axis=AX.X), reads=ok, writes=[kst])
                yield
                S.op("dve", lambda e: e.tensor_scalar(out=st[:, 0:4], in0=st[:, 0:4], scalar1=-1.0 / 128, scalar2=None, op0=ALU.mult), reads=[kst], writes=[kst])
                yield
                S.op("dve", lambda e: e.tensor_tensor(out=xc[:].rearrange("p (h d) -> p h d", d=128), in0=o3,
                                                      in1=st[:, 0:4, None].to_broadcast([128, 4, 128]), op=ALU.add), reads=ok + [kst], writes=[kxc])
                yield
                S.op("act", lambda e: e.activation(out=sq[:], in_=xc[:], func=AF.Square), reads=[kxc], writes=[ksq])
                yield
                S.op("dve", lambda e: e.reduce_sum(out=st[:, 4:8], in_=sq[:].rearrange("p (h d) -> p h d", d=128), axis=AX.X), reads=[ksq], writes=[kst])
                yield
                S.op("act", lambda e: e.activation(out=st[:, 4:8], in_=st[:, 4:8], func=AF.Sqrt, scale=1.0 / 128, bias=EPS), reads=[kst], writes=[kst])
                yield
                S.op("dve", lambda e: e.reciprocal(out=st[:, 4:8], in_=st[:, 4:8]), reads=[kst], writes=[kst])
                yield
                S.op("dve", lambda e: e.tensor_tensor(out=xc[:].rearrange("p (h d) -> p h d", d=128), in0=xc[:].rearrange("p (h d) -> p h d", d=128),
                                                      in1=st[:, 4:8, None].to_broadcast([128, 4, 128]), op=ALU.mult), reads=[kxc, kst], writes=[kxc])
                yield
                S.op("pool", lambda e: e.tensor_tensor(out=xc[:], in0=xc[:], in1=gn[:], op=ALU.mult), reads=[kxc, "rgn"], writes=[kxc])
                yield
                S.op("pool", lambda e: e.tensor_tensor(out=y_[:], in0=xc[:], in1=sg[:, ch, :], op=ALU.mult), reads=[kxc, "rsg"], writes=[ky])
                yield
                for j in range(4):
                    S.op("pe", lambda e, j=j: e.transpose(out=tpo[:, j, :], in_=y_[:, j * 128:(j + 1) * 128], identity=G["ident_b"][:]),
                         reads=[ky, "ident_b"], writes=["rtpo"])
                S.op("act", lambda e: e.copy(out=t_[:], in_=tpo[:]), reads=["rtpo"], writes=[kt_])
                S.dma("sp", G["mix_d"][0:512, ch * 128:(ch + 1) * 128].rearrange("(j p) t -> p j t", p=128), t_[:], reads=[kt_], writes=[("mix_d", "r", ch)])
                yield

            for c2 in range(0, 16, 2):
                run_rr([gn_chain(c2), gn_chain(c2 + 1)])


def phase_gqa(C, G):
    S = C.S
    aT = G["aT"]
    with C.phase():
        qT = C.sb("gqT", [128, 4, SEQ], BF16)
        kT = C.sb("gkT", [128, 2, T], BF16)
        vx = C.sb("gvx", [128, NT, 2, 65], BF16)
        gq = C.sb("ggq", [128, 64], F32)
        gk = C.sb("ggk", [128, 64], F32)
        cs = C.sb("gropec", [128, 16, 32], F32)
        sn = C.sb("gropes", [128, 16, 32], F32)
        sk = C.sb("gsink", [128, 8], F32)
        msk = C.sb("gmask", [128, 2, 128], BF16)
        S.dma("sp", gq[:], G["gq_q_norm"][0:1, :].to_broadcast([128, 64]), writes=["ggq"])
        S.dma("sp", gk[:], G["gq_k_norm"][0:1, :].to_broadcast([128, 64]), writes=["ggk"])
        S.dma("sp", sk[:], G["gq_sink"][0:1, :].to_broadcast([128, 8]), writes=["gsink"])
        S.dma("sp", cs[:], G["c_cos"].rearrange("(t p) f -> p t f", p=128), writes=["rope"])
        S.dma("sp", sn[:], G["c_sin"].rearrange("(t p) f -> p t f", p=128), writes=["rope"])
        S.dma("sp", msk[:], G["c_gmask"].rearrange("a p f -> p a f"), writes=["gmask"])
        S.op("dve", lambda e: e.tensor_scalar(out=gq[:], in0=gq[:], scalar1=0.125, scalar2=None, op0=ALU.mult), reads=["ggq"], writes=["ggq"])
        S.op("act", lambda e: e.activation(out=sk[:], in_=sk[:], func=AF.Exp), reads=["gsink"], writes=["gsink"])
        S.op("pool", lambda e: e.memset(vx[:, :, :, 64:65], 1.0), writes=["gvx1"])
        with C.phase():
            w = C.sb("wgqa", [128, 8, 896], BF16)
            base = 2048
            S.dma("pool", w[:, :, 0:512], G["o_w_in"][0, :, base:base + 512].rearrange("(k p) n -> p k n", p=128), writes=["wgqa0"])
            for i, kv in enumerate((0, 0, 1, 1)):
                S.dma("pool", w[:, :, 512 + i * 64:512 + (i + 1) * 64],
                      G["o_w_in"][0, :, base + 512 + kv * 64:base + 512 + (kv + 1) * 64].rearrange("(k p) n -> p k n", p=128), writes=[f"wgqa1{i}"])
            S.dma("pool", w[:, :, 768:896], G["o_w_in"][0, :, base + 640:base + 768].rearrange("(k p) n -> p k n", p=128), writes=["wgqa2"])
            wk_ = ["wgqa0", "wgqa10", "wgqa11", "wgqa12", "wgqa13", "wgqa2"]
            pq = [C.ps(f"gpq{i}", [128, 512], F32) for i in range(2)]
            pk = [C.ps(f"gpk{i}", [128, 512], F32) for i in range(2)]
            tps_ = [C.ps(f"gqktp{i}", [128, 4, 128], BF16) for i in range(2)]
            Wq = [qk_work(C, f"gwq{i}", tps_[i]) for i in range(2)]
            Wk = [qk_work(C, f"gwk{i}", tps_[i]) for i in range(2)]
            for i in range(2):
                Wq[i]["tpkey"] = Wk[i]["tpkey"] = f"gqktp{i}"
            for t2 in range(0, NT, 2):
                gens = []
                for tt in (t2, t2 + 1):
                    lat = tt < 16
                    q_, k_ = pq[tt % 2], pk[tt % 2]
                    if lat:
                        for k in range(8):
                            S.op("pe", lambda e, q_=q_, k=k, tt=tt: e.matmul(q_[:], lhsT=aT[:, k, tt * 128:(tt + 1) * 128], rhs=w[:, k, 0:512], start=(k == 0), stop=(k == 7)),
                                 reads=wk_, writes=[f"gpq{tt % 2}"])
                    for k in range(8):
                        S.op("pe", lambda e, k_=k_, k=k, tt=tt: e.matmul(k_[:, 0:384], lhsT=aT[:, k, tt * 128:(tt + 1) * 128], rhs=w[:, k, 512:896], start=(k == 0), stop=(k == 7)),
                             reads=wk_, writes=[f"gpk{tt % 2}"])
                    S.op("act", lambda e, k_=k_, tt=tt: e.copy(out=vx[:, tt, :, 0:64], in_=k_[:, 256:384].rearrange("p (h d) -> p h d", d=64)), reads=[f"gpk{tt % 2}"], writes=["gvx"])
                    if lat:
                        gens.append(qk_prep(C, G, q_[:], f"gpq{tt % 2}", gq, "ggq", cs, sn, tt, True, qT, "gqT", 8, Wq[tt % 2], f"gwq{tt % 2}"))
                    gens.append(qk_prep(C, G, k_[:, 0:256], f"gpk{tt % 2}", gk, "ggk", cs, sn, tt, lat, kT, "gkT", 4, Wk[tt % 2], f"gwk{tt % 2}"))
                run_rr(gens)
        with C.phase():
            sp = [C.ps(f"gsp{i}", [128, 512], F32) for i in range(4)]
            oacc = [C.ps(f"goacc{i}", [128, 4, 65], F32) for i in range(2)]
            pt = [C.sb(f"gpt{i}", [128, 512], BF16) for i in range(3)]
            den = [C.sb(f"gden{i}", [128, 4], F32) for i in range(2)]
            sks = C.sb("gsks", [128, 2, 4], F32)
            for kv_ in range(2):
                for sl_ in range(4):
                    hd_ = kv_ * 4 + 2 * (sl_ % 2) + sl_ // 2
                    S.op("dve", lambda e, kv_=kv_, sl_=sl_, hd_=hd_: e.tensor_copy(out=sks[:, kv_, sl_:sl_ + 1], in_=sk[:, hd_:hd_ + 1]), reads=["gsink"], writes=["gsks"])
            on = [C.sb(f"gon{i}", [128, 512], F32) for i in range(2)]
            oT = [C.sb(f"goT{i}", [128, 4, 128], BF16) for i in range(2)]
            its = []
            for qt in range(16):
                for kv in range(2):
                    tiles = [(16, None), (17, None)]
                    if qt > 0:
                        tiles.append((qt - 1, 0))
                    tiles.append((qt, None))
                    if qt < 15:
                        tiles.append((qt + 1, 1))
                    for ti, (kt, mk) in enumerate(tiles):
                        its.append(dict(qt=qt, kv=kv, kt=kt, mk=mk, first=(ti == 0), last=(ti == len(tiles) - 1)))

            def emit_s(i):
                d_ = its[i]
                qt, kv, kt, mk = d_["qt"], d_["kv"], d_["kt"], d_["mk"]
                p_, kp_ = pt[i % 3], f"gpt{i % 3}"
                for half in range(2):
                    sb_i = (i % 2) * 2 + half
                    pb = slice(half * 64, (half + 1) * 64)
                    S.op("pe", lambda e, pb=pb, sb_i=sb_i, kv=kv, kt=kt, qt=qt: e.matmul(
                        sp[sb_i][:, 0:256], lhsT=kT[pb, kv, kt * 128:(kt + 1) * 128],
                        rhs=qT[pb, kv * 2:kv * 2 + 2, qt * 128:(qt + 1) * 128], start=True, stop=True),
                        reads=["gqT", "gkT"], writes=[f"gsp{sb_i}"])
                    S.op("act", lambda e, p_=p_, half=half, sb_i=sb_i: e.activation(out=p_[:, half * 256:(half + 1) * 256], in_=sp[sb_i][:, 0:256], func=AF.Exp),
                         reads=[f"gsp{sb_i}"], writes=[kp_])
                if mk is not None:
                    S.op("pool", lambda e, p_=p_, mk=mk: e.tensor_tensor(out=p_[:].rearrange("p (a b) -> p a b", b=128), in0=p_[:].rearrange("p (a b) -> p a b", b=128),
                                                                         in1=msk[:, mk, None, :].to_broadcast([128, 4, 128]), op=ALU.mult),
                         reads=[kp_, "gmask"], writes=[kp_])

            emit_s(0)
            for i in range(len(its)):
                if i + 1 < len(its):
                    emit_s(i + 1)
                d_ = its[i]
                qt, kv, kt = d_["qt"], d_["kv"], d_["kt"]
                p_, kp_ = pt[i % 3], f"gpt{i % 3}"
                o_n, kon = on[qt % 2], f"gon{qt % 2}"
                ab = (qt * 2 + kv) % 2
                acc, kacc = oacc[ab], f"goacc{ab}"
                for sl in range(4):
                    S.op("pe", lambda e, sl=sl, p_=p_, kt=kt, kv=kv, acc=acc, st_=(d_["first"] and sl == 0), last=d_["last"]: e.matmul(
                        acc[:, sl, :], lhsT=p_[:, sl * 128:(sl + 1) * 128], rhs=vx[:, kt, kv, :], start=st_, stop=last, skip_group_check=True),
                        reads=[kp_, "gvx", "gvx1"], writes=[kacc])
                if not d_["last"]:
                    continue
                dn_, kdn = den[ab], f"gden{ab}"
                S.op("dve", lambda e, acc=acc, dn_=dn_, kv=kv: e.tensor_tensor(out=dn_[:], in0=acc[:, :, 64], in1=sks[:, kv, :], op=ALU.add),
                     reads=[kacc, "gsks"], writes=[kdn])
                S.op("dve", lambda e, dn_=dn_: e.reciprocal(out=dn_[:], in_=dn_[:]), reads=[kdn], writes=[kdn])
                S.op("dve", lambda e, acc=acc, dn_=dn_, o_n=o_n, kv=kv: e.tensor_tensor(
                    out=o_n[:, kv * 256:(kv + 1) * 256].rearrange("p (i h d) -> p h i d", i=2, h=2),
                    in0=acc[:, :, 0:64].rearrange("p (h i) d -> p h i d", h=2),
                    in1=dn_[:, :, None].rearrange("p (h i) o -> p h i o", h=2).to_broadcast([128, 2, 2, 64]), op=ALU.mult),
                    reads=[kacc, kdn], writes=[kon])
                if kv == 0:
                    continue
                t_, kt_ = oT[qt % 2], f"goT{qt % 2}"
                tb_i = ((i + 1) % 2) * 2 + 1 if i + 1 < len(its) else 1
                tb_i = (i % 2) * 2
                tps = sp[tb_i]
                for j in range(4):
                    S.op("pe", lambda e, j=j, o_n=o_n, tps=tps: e.transpose(out=tps[:, j * 128:(j + 1) * 128], in_=o_n[:, j * 128:(j + 1) * 128], identity=G["ident_f"][:]),
                         reads=[kon, "ident_f"], writes=[f"gsp{tb_i}"])
                S.op("act", lambda e, t_=t_, tps=tps: e.copy(out=t_[:], in_=tps[:].rearrange("p (a b) -> p a b", b=128)), reads=[f"gsp{tb_i}"], writes=[kt_])
                S.dma("sp", G["mix_d"][512:1024, qt * 128:(qt + 1) * 128].rearrange("(j p) t -> p j t", p=128), t_[:], reads=[kt_], writes=[("mix_d", "g", qt)])
```

```python
import contextlib
import math
import numpy as np
import ml_dtypes
import concourse.bass as bass
import concourse.mybir as mybir
from concourse.bass_utils import run_bass_kernel_spmd

F32 = mybir.dt.float32
BF16 = mybir.dt.bfloat16
I32 = mybir.dt.int32
AF = mybir.ActivationFunctionType
ALU = mybir.AluOpType
AX = mybir.AxisListType

D = 1024
SEQ = 2048
CTX = 256
T = SEQ + CTX
NT = T // 128
EPS = 1e-6
NEXP = 32
DEXP = 256

ENGS = ("pe", "act", "dve", "pool", "sp")
NDMA_SEM = 8


class _Op:
    __slots__ = ("eng", "fn", "deps", "need_inc", "sem", "val", "is_dma", "rot_dep")

    def __init__(self, eng, fn, is_dma):
        self.eng = eng
        self.fn = fn
        self.deps = []
        self.need_inc = False
        self.sem = None
        self.val = 0
        self.is_dma = is_dma
        self.rot_dep = None


class Sched:
    def __init__(self, nc):
        self.nc = nc
        self.ops = {e: [] for e in ENGS}
        self.last_writer = {}
        self.readers = {}
        self.dma_hist = {e: [] for e in ENGS}

    def _track(self, op, reads, writes):
        deps = []
        for k in reads:
            w = self.last_writer.get(k)
            if w is not None:
                deps.append(w)
        for k in writes:
            w = self.last_writer.get(k)
            if w is not None:
                deps.append(w)
            deps.extend(self.readers.get(k, ()))
        seen = set()
        for dp in deps:
            if dp is op or id(dp) in seen:
                continue
            if dp.eng == "pe" and op.eng == "pe" and not dp.is_dma and not op.is_dma:
                continue
            seen.add(id(dp))
            op.deps.append(dp)
            dp.need_inc = True
        for k in writes:
            self.last_writer[k] = op
            self.readers[k] = []
        for k in reads:
            self.readers.setdefault(k, []).append(op)

    def op(self, eng, fn, reads=(), writes=()):
        o = _Op(eng, fn, False)
        self._track(o, reads, writes)
        self.ops[eng].append(o)
        return o

    def dma(self, eng, out, in_, reads=(), writes=(), **kw):
        def fn(e, out=out, in_=in_, kw=kw):
            return e.dma_start(out=out, in_=in_, **kw)
        o = _Op(eng, fn, True)
        o.need_inc = True
        self._track(o, reads, writes)
        hist = self.dma_hist[eng]
        if len(hist) >= NDMA_SEM:
            o.rot_dep = hist[-NDMA_SEM]
        hist.append(o)
        self.ops[eng].append(o)
        return o

    def barrier(self):
        lasts = []
        for e in ENGS:
            for o in reversed(self.ops[e]):
                if not o.is_dma and o.fn is not None:
                    lasts.append(o)
                    break
            lasts.extend(self.dma_hist[e][-NDMA_SEM:])
        for e in ENGS:
            o = _Op(e, None, False)
            for dp in lasts:
                o.deps.append(dp)
                dp.need_inc = True
            self.ops[e].append(o)
        self.last_writer = {}
        self.readers = {}

    def emit(self, final_wait_ops=()):
        nc = self.nc
        sems = {e: nc.alloc_semaphore(f"s_{e}") for e in ENGS}
        dsems = {e: [nc.alloc_semaphore(f"d_{e}{i}") for i in range(NDMA_SEM)] for e in ENGS
                 if self.dma_hist[e]}
        for e in ENGS:
            cnt = 0
            dcnt = [0] * NDMA_SEM
            di = 0
            for o in self.ops[e]:
                if o.is_dma:
                    j = di % NDMA_SEM
                    dcnt[j] += 16
                    o.sem = dsems[e][j]
                    o.val = dcnt[j]
                    di += 1
                elif o.need_inc and o.fn is not None:
                    cnt += 1
                    o.sem = sems[e]
                    o.val = cnt
        engobj = {"pe": "tensor", "act": "scalar", "dve": "vector", "pool": "gpsimd", "sp": "sync"}
        final_wait_ops = list(final_wait_ops)
        with nc.Block() as block:
            for e in ENGS:
                def body(eng, e=e):
                    waited = {}

                    def wait(dp):
                        if dp.sem is None:
                            return
                        key = id(dp.sem)
                        if waited.get(key, 0) >= dp.val:
                            return
                        waited[key] = dp.val
                        eng.wait_ge(dp.sem, dp.val)
                    for o in self.ops[e]:
                        for dp in o.deps:
                            wait(dp)
                        if o.rot_dep is not None:
                            wait(o.rot_dep)
                        if o.fn is None:
                            continue
                        ins = o.fn(eng)
                        if o.is_dma:
                            ins.then_inc(o.sem, 16)
                        elif o.need_inc:
                            ins.then_inc(o.sem, 1)
                    if e == "sp":
                        for dp in final_wait_ops:
                            wait(dp)
                getattr(block, engobj[e])(body)
        return {e: len(self.ops[e]) for e in ENGS}


class Ctx:
    def __init__(self, nc):
        self.nc = nc
        self.S = Sched(nc)
        self.stack = None
        self.uid = 0

    @contextlib.contextmanager
    def phase(self):
        prev = self.stack
        with contextlib.ExitStack() as st:
            self.stack = st
            yield
            self.S.barrier()
        self.stack = prev

    def sb(self, name, shape, dtype):
        self.uid += 1
        return self.stack.enter_context(self.nc.sbuf_tensor(f"{name}_{self.uid}", list(shape), dtype))

    def ps(self, name, shape, dtype=F32):
        self.uid += 1
        return self.stack.enter_context(self.nc.psum_tensor(f"{name}_{self.uid}", list(shape), dtype))


def token_blocks():
    return [(0, 512, 0), (512, 512, 0), (1024, 512, 0), (1536, 512, 0), (2048, 256, 1)]


def phase_load(C, G):
    S = C.S
    hT = G["hT"]
    with C.phase():
        xt = [C.sb(f"ld_x{i}", [128, D], F32) for i in range(3)]
        tp = [C.ps(f"ld_tp{i}", [128, 8, 128], F32) for i in range(2)]
        for t in range(NT):
            src = G["x"][t * 128:(t + 1) * 128, :] if t < 16 else G["ctx"][(t - 16) * 128:(t - 15) * 128, :]
            xb, pb = xt[t % 3], tp[t % 2]
            kx, kp = f"ldx{t % 3}", f"ldp{t % 2}"
            S.dma("sp", xb[:], src, writes=[kx])
            for k in range(8):
                S.op("pe", lambda e, k=k, xb=xb, pb=pb: e.transpose(out=pb[:, k, :], in_=xb[:, k * 128:(k + 1) * 128],
                                                                 identity=G["ident_f"][:]),
                     reads=[kx, "ident_f"], writes=[kp])
            eng = "act" if t % 2 == 0 else "dve"
            if eng == "act":
                S.op("act", lambda e, pb=pb, t=t: e.copy(out=hT[:, :, t * 128:(t + 1) * 128], in_=pb[:]),
                     reads=[kp], writes=[("hT", t)])
            else:
                S.op("dve", lambda e, pb=pb, t=t: e.tensor_copy(out=hT[:, :, t * 128:(t + 1) * 128], in_=pb[:]),
                     reads=[kp], writes=[("hT", t)])


def phase_store(C, G):
    S = C.S
    hT = G["hT"]
    outs = []
    with C.phase():
        ot = [C.sb(f"st_o{i}", [128, D], F32) for i in range(3)]
        tp = [C.ps(f"st_tp{i}", [128, 8, 128], F32) for i in range(2)]
        for t in range(16):
            ob, pb = ot[t % 3], tp[t % 2]
            ko, kp = f"sto{t % 3}", f"stp{t % 2}"
            for k in range(8):
                S.op("pe", lambda e, k=k, pb=pb, t=t: e.transpose(out=pb[:, k, :], in_=hT[:, k, t * 128:(t + 1) * 128],
                                                                identity=G["ident_f"][:]),
                     reads=[("hT", t), "ident_f"], writes=[kp])
            if t % 2 == 0:
                S.op("act", lambda e, pb=pb, ob=ob: e.copy(out=ob[:].rearrange("p (k d) -> p k d", k=8), in_=pb[:]),
                     reads=[kp], writes=[ko])
            else:
                S.op("dve", lambda e, pb=pb, ob=ob: e.tensor_copy(out=ob[:].rearrange("p (k d) -> p k d", k=8), in_=pb[:]),
                     reads=[kp], writes=[ko])
            outs.append(S.dma("sp", G["out"][t * 128:(t + 1) * 128, :], ob[:], reads=[ko]))
        G["final_ops"] = outs


def phase_mods(C, G, mid=None):
    S = C.S
    nc = C.nc
    with C.phase():
        c2sb = C.sb("c2sb", [2, D], F32)
        bsb = C.sb("bsb", [2, 6 * D], F32)
        cs = C.sb("cs", [128, 8, 2], BF16)
        bcol = C.sb("bcol", [128, 48, 2], F32)
        cps = C.ps("cps", [128, 8, 2], F32)
        bps = C.ps("bps", [128, 48, 2], F32)
        S.dma("sp", c2sb[:], G["c2"], writes=["c2sb"])
        S.dma("sp", bsb[:], G["ada_b"], writes=["bsb"])
        for k in range(8):
            S.op("pe", lambda e, k=k: e.transpose(out=cps[:, k, :], in_=c2sb[0:2, k * 128:(k + 1) * 128], identity=G["ident_f"][0:2, 0:2]),
                 reads=["c2sb", "ident_f"], writes=["cps"])
        for j in range(48):
            S.op("pe", lambda e, j=j: e.transpose(out=bps[:, j, :], in_=bsb[0:2, j * 128:(j + 1) * 128], identity=G["ident_f"][0:2, 0:2]),
                 reads=["bsb", "ident_f"], writes=["bps"])
        S.op("act", lambda e: e.activation(out=cs[:], in_=cps[:], func=AF.Silu), reads=["cps"], writes=["cs"])
        S.op("act", lambda e: e.copy(out=bcol[:], in_=bps[:]), reads=["bps"], writes=["bcol"])
        wb = [C.sb(f"adaw{i}", [128, 8, 1536], BF16) for i in range(2)]
        mp = C.ps("modp", [128, 2, 48, 2], F32)
        pieces = [(l, q) for l in range(2) for q in range(4)]

        def issue(i):
            l, q = pieces[i]
            S.dma("pool", wb[i % 2][:], G["ada_w"][l, :, q * 1536:(q + 1) * 1536].rearrange("(k p) n -> p k n", p=128), writes=[f"adaw{i % 2}"])
        issue(0)
        issue(1)
        if mid is not None:
            mid()
        i = 0
        for l in range(2):
            for q in range(4):
                w, kw = wb[i % 2], f"adaw{i % 2}"
                if i >= 2:
                    issue(i)
                for jj in range(12):
                    j = q * 12 + jj
                    for k in range(8):
                        S.op("pe", lambda e, w=w, l=l, j=j, jj=jj, k=k: e.matmul(mp[:, l, j, :], lhsT=w[:, k, jj * 128:(jj + 1) * 128],
                                                                              rhs=cs[:, k, :], start=(k == 0), stop=(k == 7)),
                             reads=[kw, "cs"], writes=["modp"])
                i += 1
        for l in range(2):
            for s in range(2):
                S.op("dve", lambda e, l=l, s=s: e.tensor_tensor(out=G["modc"][:, l, :, s], in0=mp[:, l, :, s], in1=bcol[:, :, l], op=ALU.add),
                     reads=["modp", "bcol"], writes=["modc"])
        for l in range(2):
            for j0 in (8, 32):
                S.op("dve", lambda e, l=l, j0=j0: e.tensor_scalar(out=G["modc"][:, l, j0:j0 + 8, :], in0=G["modc"][:, l, j0:j0 + 8, :],
                                                                 scalar1=1.0, scalar2=None, op0=ALU.add),
                     reads=["modc"], writes=["modc"])


def phase_norm(C, G, l, which, router=False, ntok=T):
    S = C.S
    sh0 = 0 if which == 0 else 24
    sc0 = 8 if which == 0 else 32
    hT, aT, modc = G["hT"], G["aT"], G["modc"]
    with C.phase():
        sq = [C.sb(f"nsq{i}", [128, 512], F32) for i in range(3)]
        tmp = [C.sb(f"ntmp{i}", [128, 512], F32) for i in range(3)]
        rstd = [C.sb(f"nrstd{i}", [128, 512], F32) for i in range(2)]
        ssp = [C.ps(f"nss{i}", [128, 512], F32) for i in range(2)]
        if router:
            v32 = [C.sb(f"nv32{i}", [128, 512], F32) for i in range(8)]
            wr = C.sb("wr32", [128, 8, 36], F32)
            rb = C.sb("rbias", [128, 36], F32)
            lgp = [C.ps(f"lgp{i}", [128, 4, 64], F32) for i in range(2)]
            ctp = C.ps("combTp", [32, 128], F32)
            combT = C.sb("combT", [32, T], BF16)
            S.dma("sp", wr[:, :, 0:4], G["moe_w_grp"][l].rearrange("(k p) n -> p k n", p=128), writes=["wr32a"])
            S.dma("sp", wr[:, :, 4:36], G["moe_w_rt"][l].rearrange("(k p) n -> p k n", p=128), writes=["wr32b"])
            S.dma("sp", rb[:, 0:4], G["moe_b_grp"][l:l + 1, :].to_broadcast([128, 4]), writes=["rba"])
            S.dma("sp", rb[:, 4:36], G["moe_b_rt"][l:l + 1, :].to_broadcast([128, 32]), writes=["rbb"])
            rts = [C.sb(f"rt{i}", [128, 160], F32) for i in range(8)]
        cnt = 0
        tile_idx = 0
        for bi, (t0, n, s) in enumerate(token_blocks()):
            if t0 >= ntok:
                continue
            sp_, rs_ = ssp[bi % 2], rstd[bi % 2]
            ksp, krs = f"nss{bi % 2}", f"nrstd{bi % 2}"
            hkeys = [("hT", tt) for tt in range(t0 // 128, (t0 + n) // 128)]
            for k in range(8):
                b = sq[cnt % 3]
                kb = f"nsq{cnt % 3}"
                cnt += 1
                S.op("act", lambda e, b=b, k=k, t0=t0, n=n: e.activation(out=b[:, :n], in_=hT[:, k, t0:t0 + n], func=AF.Square),
                     reads=hkeys, writes=[kb])
                S.op("pe", lambda e, b=b, k=k, n=n, sp_=sp_: e.matmul(sp_[:, :n], lhsT=G["ones_f"][:], rhs=b[:, :n], start=(k == 0), stop=(k == 7)),
                     reads=[kb, "ones_f"], writes=[ksp])
            S.op("act", lambda e, sp_=sp_, rs_=rs_, n=n: e.activation(out=rs_[:, :n], in_=sp_[:, :n], func=AF.Sqrt, scale=1.0 / D, bias=EPS),
                 reads=[ksp], writes=[krs])
            S.op("dve", lambda e, rs_=rs_, n=n: e.reciprocal(out=rs_[:, :n], in_=rs_[:, :n]), reads=[krs], writes=[krs])
            akeys = [("aT", tt) for tt in range(t0 // 128, (t0 + n) // 128)]
            lg = lgp[bi % 2] if router else None
            klg = f"lgp{bi % 2}"
            for k in range(8):
                tb = tmp[k % 3]
                ktb = f"ntmp{k % 3}"
                S.op("dve", lambda e, tb=tb, k=k, t0=t0, n=n, rs_=rs_: e.tensor_tensor(out=tb[:, :n], in0=hT[:, k, t0:t0 + n], in1=rs_[:, :n], op=ALU.mult),
                     reads=hkeys + [krs], writes=[ktb])
                if not router:
                    S.op("act", lambda e, tb=tb, k=k, t0=t0, n=n, s=s: e.activation(
                        out=aT[:, k, t0:t0 + n], in_=tb[:, :n], func=AF.Identity,
                        scale=modc[:, l, sc0 + k, s:s + 1], bias=modc[:, l, sh0 + k, s:s + 1]),
                        reads=[ktb, "modc"], writes=akeys)
                else:
                    vb = v32[k]
                    kvb = f"nv32{k}"
                    S.op("act", lambda e, tb=tb, vb=vb, k=k, n=n, s=s: e.activation(
                        out=vb[:, :n], in_=tb[:, :n], func=AF.Identity,
                        scale=modc[:, l, sc0 + k, s:s + 1], bias=modc[:, l, sh0 + k, s:s + 1]),
                        reads=[ktb, "modc"], writes=[kvb])
                    S.op("pool", lambda e, vb=vb, k=k, t0=t0, n=n: e.tensor_copy(out=aT[:, k, t0:t0 + n], in_=vb[:, :n]),
                         reads=[kvb], writes=akeys)
            if router:
                for st in range(n // 128):
                    for k in range(8):
                        S.op("pe", lambda e, k=k, st=st, lg=lg: e.matmul(lg[:, st, 0:36], lhsT=v32[k][:, st * 128:(st + 1) * 128], rhs=wr[:, k, :],
                                                                     start=(k == 0), stop=(k == 7)),
                             reads=[f"nv32{k}", "wr32a", "wr32b"], writes=[klg])
                gens = []
                for st in range(n // 128):
                    tt = t0 // 128 + st
                    gens.append(route_tile(C, G, lg[:, st, 0:36], klg, rb, rts[tile_idx % 8], f"rt{tile_idx % 8}", ctp, combT, tt))
                    tile_idx += 1
                while gens:
                    for g_ in list(gens):
                        try:
                            next(g_)
                        except StopIteration:
                            gens.remove(g_)
        if router:
            S.dma("sp", G["combT_d"][:, 0:ntok], combT[:, 0:ntok], reads=["combT"], writes=["combT_d"])


def route_tile(C, G, lgp, klg, rb, rt, krt, ctp, combT, tt):
    S = C.S
    LG = rt[:, 0:36]
    mg, nmg, sumg, pmax = rt[:, 36:37], rt[:, 37:38], rt[:, 38:39], rt[:, 39:40]
    ohg = rt[:, 40:44]
    eg = rt[:, 44:48]
    les = rt[:, 48:56]
    m1, m2, dm, ed = rt[:, 56:57], rt[:, 57:58], rt[:, 58:59], rt[:, 59:60]
    mk1 = rt[:, 60:68]
    les2 = rt[:, 68:76]
    mk2 = rt[:, 76:84]
    w1, w2 = rt[:, 84:85], rt[:, 85:86]
    ce = rt[:, 88:96]
    comb = rt[:, 96:128]
    R, W = [krt], [krt]

    def dv(fn, extra_r=()):
        S.op("dve", fn, reads=R + list(extra_r), writes=W)

    dv(lambda e: e.tensor_tensor(out=LG, in0=lgp, in1=rb[:], op=ALU.add), extra_r=[klg, "rba", "rbb"])
    yield
    dv(lambda e: e.reduce_max(out=mg, in_=rt[:, 0:4], axis=AX.X))
    yield
    dv(lambda e: e.tensor_scalar(out=ohg, in0=rt[:, 0:4], scalar1=mg, scalar2=None, op0=ALU.is_equal))
    yield
    dv(lambda e: e.tensor_scalar(out=nmg, in0=mg, scalar1=-1.0, scalar2=None, op0=ALU.mult))
    yield
    S.op("act", lambda e: e.activation(out=eg, in_=rt[:, 0:4], func=AF.Exp, bias=nmg, scale=1.0, accum_out=sumg), reads=R, writes=W)
    yield
    dv(lambda e: e.reciprocal(out=pmax, in_=sumg))
    yield
    dv(lambda e: e.tensor_scalar(out=les, in0=rt[:, 4:12], scalar1=rt[:, 40:41], scalar2=None, op0=ALU.mult))
    yield
    for g in range(1, 4):
        dv(lambda e, g=g: e.scalar_tensor_tensor(out=les, in0=rt[:, 4 + 8 * g:12 + 8 * g], scalar=rt[:, 40 + g:41 + g], in1=les,
                                                 op0=ALU.mult, op1=ALU.add))
        yield
    dv(lambda e: e.reduce_max(out=m1, in_=les, axis=AX.X))
    yield
    dv(lambda e: e.tensor_scalar(out=mk1, in0=les, scalar1=m1, scalar2=None, op0=ALU.is_equal))
    yield
    dv(lambda e: e.scalar_tensor_tensor(out=les2, in0=mk1, scalar=-1e30, in1=les, op0=ALU.mult, op1=ALU.add))
    yield
    dv(lambda e: e.reduce_max(out=m2, in_=les2, axis=AX.X))
    yield
    dv(lambda e: e.tensor_scalar(out=mk2, in0=les2, scalar1=m2, scalar2=None, op0=ALU.is_equal))
    yield
    dv(lambda e: e.tensor_tensor(out=dm, in0=m2, in1=m1, op=ALU.subtract))
    yield
    S.op("act", lambda e: e.activation(out=ed, in_=dm, func=AF.Exp), reads=R, writes=W)
    yield
    dv(lambda e: e.tensor_scalar(out=w1, in0=ed, scalar1=1.0, scalar2=None, op0=ALU.add))
    yield
    dv(lambda e: e.reciprocal(out=w1, in_=w1))
    yield
    dv(lambda e: e.tensor_tensor(out=w2, in0=ed, in1=w1, op=ALU.mult))
    yield
    dv(lambda e: e.tensor_scalar(out=rt[:, 84:86], in0=rt[:, 84:86], scalar1=pmax, scalar2=None, op0=ALU.mult))
    yield
    dv(lambda e: e.tensor_scalar(out=ce, in0=mk1, scalar1=w1, scalar2=None, op0=ALU.mult))
    yield
    dv(lambda e: e.scalar_tensor_tensor(out=ce, in0=mk2, scalar=w2, in1=ce, op0=ALU.mult, op1=ALU.add))
    yield
    for g in range(4):
        dv(lambda e, g=g: e.tensor_scalar(out=rt[:, 96 + 8 * g:104 + 8 * g], in0=ce, scalar1=rt[:, 40 + g:41 + g], scalar2=None, op0=ALU.mult))
        yield
    S.op("pe", lambda e: e.transpose(out=ctp[:, :], in_=comb, identity=G["ident_f"][:]), reads=R + ["ident_f"], writes=["combTp"])
    S.op("act", lambda e: e.copy(out=combT[:, tt * 128:(tt + 1) * 128], in_=ctp[:, :]), reads=["combTp"], writes=["combT"])
    yield


def phase_moe(C, G, l, ntok):
    S = C.S
    hT, aT, modc = G["hT"], G["aT"], G["modc"]
    EG = 2
    NB = 2 * EG
    blocks = [(t0, n) for (t0, n, s_) in token_blocks() if t0 < ntok]
    with C.phase():
        wg = [C.sb(f"wg{i}", [128, 8, 256], BF16) for i in range(NB)]
        wu = [C.sb(f"wu{i}", [128, 8, 256], BF16) for i in range(NB)]
        wd = [C.sb(f"wd{i}", [128, 2, 1024], BF16) for i in range(NB)]
        cb = [C.sb(f"cb{i}", [128, T], BF16) for i in range(NB)]
        sg = [C.sb(f"sg{i}", [128, 512], F32) for i in range(2)]
        tg = [C.sb(f"tg{i}", [128, 512], F32) for i in range(2)]
        at = [[[C.sb(f"at{u}_{e}_{c}", [128, 512], BF16) for c in range(2)] for e in range(EG)] for u in range(2)]
        hp = [C.ps(f"hp{i}", [128, 2, 512], F32) for i in range(2)]
        yp = C.ps("yp", [128, 4, 512], F32)

        def load_group(g0):
            for e in range(g0, g0 + EG):
                w = e % NB
                gi, ei = e // 8, e % 8
                S.dma("pool", wg[w][:], G["moe_w_gate"][l, gi, ei].rearrange("(k p) f -> p k f", p=128), writes=[f"wg{w}"])
                S.dma("pool", wu[w][:], G["moe_w_up"][l, gi, ei].rearrange("(k p) f -> p k f", p=128), writes=[f"wu{w}"])
                S.dma("pool", wd[w][:], G["moe_w_down"][l, gi, ei].rearrange("(c p) d -> p c d", p=128), writes=[f"wd{w}"])
                S.dma("sp", cb[w][:, 0:ntok], G["combT_d"][e:e + 1, 0:ntok].to_broadcast([128, ntok]), writes=[f"cb{w}"])

        units = [(g0, bi) for g0 in range(0, NEXP, EG) for bi in range(len(blocks))]
        gcount = [0]

        def emit_g(u, el, c):
            g0, bi = units[u]
            t0, n = blocks[bi]
            e = g0 + el
            w = e % NB
            i = gcount[0]
            gcount[0] += 1
            h, kh = hp[i % 2], f"hp{i % 2}"
            for (wt, kw, q) in ((wg, f"wg{w}", 0), (wu, f"wu{w}", 1)):
                for k in range(8):
                    S.op("pe", lambda e_, h=h, wt=wt, w=w, c=c, k=k, t0=t0, n=n, q=q: e_.matmul(
                        h[:, q, :n], lhsT=wt[w][:, k, c * 128:(c + 1) * 128], rhs=aT[:, k, t0:t0 + n], start=(k == 0), stop=(k == 7)),
                        reads=[kw], writes=[kh])
            sgt, tgt = sg[i % 2], tg[i % 2]
            ksg, ktg = f"sg{i % 2}", f"tg{i % 2}"
            a_ = at[u % 2][el][c]
            ka = f"at{u % 2}_{el}_{c}"
            S.op("act", lambda e_, h=h, sgt=sgt, n=n: e_.activation(out=sgt[:, :n], in_=h[:, 0, :n], func=AF.Silu), reads=[kh], writes=[ksg])
            S.op("dve", lambda e_, h=h, tgt=tgt, w=w, t0=t0, n=n: e_.tensor_tensor(out=tgt[:, :n], in0=h[:, 1, :n], in1=cb[w][:, t0:t0 + n], op=ALU.mult),
                 reads=[kh, f"cb{w}"], writes=[ktg])
            S.op("pool", lambda e_, sgt=sgt, tgt=tgt, a_=a_, n=n: e_.tensor_tensor(out=a_[:, :n], in0=sgt[:, :n], in1=tgt[:, :n], op=ALU.mult),
                 reads=[ksg, ktg], writes=[ka])

        def emit_d(u, half):
            g0, bi = units[u]
            t0, n = blocks[bi]
            for jj in range(4):
                j = half * 4 + jj
                for el in range(EG):
                    w = (g0 + el) % NB
                    for c in range(2):
                        S.op("pe", lambda e_, jj=jj, j=j, el=el, w=w, c=c, n=n, u=u: e_.matmul(
                            yp[:, jj, :n], lhsT=wd[w][:, c, j * 128:(j + 1) * 128], rhs=at[u % 2][el][c][:, :n],
                            start=(el == 0 and c == 0), stop=(el == EG - 1 and c == 1)),
                            reads=[f"wd{w}", f"at{u % 2}_{el}_{c}"], writes=[("yp", jj)])
            s = 0 if t0 < SEQ else 1
            hkeys = [("hT", tt) for tt in range(t0 // 128, (t0 + n) // 128)]
            for jj in range(4):
                j = half * 4 + jj
                S.op("dve", lambda e_, j=j, jj=jj, t0=t0, n=n, s=s: e_.scalar_tensor_tensor(
                    out=hT[:, j, t0:t0 + n], in0=yp[:, jj, :n], scalar=modc[:, l, 40 + j, s:s + 1], in1=hT[:, j, t0:t0 + n],
                    op0=ALU.mult, op1=ALU.add),
                    reads=[("yp", jj), "modc"] + hkeys, writes=hkeys)

        load_group(0)
        for el in range(EG):
            for c in range(2):
                emit_g(0, el, c)
        for u in range(len(units)):
            g0, bi = units[u]
            if bi == 0 and g0 + EG < NEXP:
                load_group(g0 + EG)
            nxt = u + 1 < len(units)
            if nxt:
                emit_g(u + 1, 0, 0)
                emit_g(u + 1, 0, 1)
            emit_d(u, 0)
            if nxt:
                emit_g(u + 1, 1, 0)
                emit_g(u + 1, 1, 1)
            emit_d(u, 1)


def load_cols(C, G, name, srcs, nchunk, width=128):
    S = C.S
    R = sum(a.shape[0] for a in srcs)
    cols = C.sb(name + "_cols", [128, nchunk, R], F32)
    with C.phase():
        rows = C.sb(name + "_rows", [R, nchunk * width], F32)
        pp = C.ps(name + "_ps", [128, nchunk, R], F32)
        r0 = 0
        for i, a in enumerate(srcs):
            S.dma("sp", rows[r0:r0 + a.shape[0], :], a, writes=[(name, "rows", i)])
            r0 += a.shape[0]
        rk = [(name, "rows", i) for i in range(len(srcs))]
        for j in range(nchunk):
            S.op("pe", lambda e, j=j: e.transpose(out=pp[0:width, j, :], in_=rows[0:R, j * width:(j + 1) * width], identity=G["ident_f"][0:R, 0:R]),
                 reads=rk + ["ident_f"], writes=[(name, "ps")])
        S.op("act", lambda e: e.copy(out=cols[0:width], in_=pp[0:width]), reads=[(name, "ps")], writes=[name])
    return cols


def phase_hy_inproj(C, G):
    S = C.S
    aT = G["aT"]
    with C.phase():
        w = C.sb("why", [128, 8, 1536], BF16)
        S.dma("pool", w[:], G["e_w_in"][0, :, 0:1536].rearrange("(k p) n -> p k n", p=128), writes=["why"])
        cw = load_cols(C, G, "hycw", [G["hy_conv_w"][0], G["hy_conv_b"]], 12)
        raw = [C.sb(f"hyraw{i}", [128, T + 4], F32) for i in range(2)]
        tmp = [C.sb(f"hytmp{i}", [128, T], F32) for i in range(2)]
        ob = [C.sb(f"hyob{i}", [128, T], BF16) for i in range(2)]
        vt = [C.sb(f"hyvt{i}", [128, 4, 128], BF16) for i in range(2)]
        pp = [C.ps(f"hypp{i}", [128, 512], F32) for i in range(3)]
        tp = [C.ps(f"hytp{i}", [128, 4, 128], BF16) for i in range(2)]
        for i in range(2):
            for c0 in (0, SEQ + 1, SEQ + 2, T + 3):
                S.op("pool", lambda e, i=i, c0=c0: e.memset(raw[i][:, c0:c0 + 1], 0.0), writes=[f"hyraw{i}"])
        ip = 0
        for cc in range(12):
            rw, tm, o = raw[cc % 2], tmp[cc % 2], ob[cc % 2]
            krw, ktm, ko = f"hyraw{cc % 2}", f"hytmp{cc % 2}", f"hyob{cc % 2}"
            for (t0, n, s) in token_blocks():
                p = pp[ip % 3]
                kp = f"hypp{ip % 3}"
                ip += 1
                for k in range(8):
                    S.op("pe", lambda e, p=p, k=k, cc=cc, t0=t0, n=n: e.matmul(p[:, :n], lhsT=w[:, k, cc * 128:(cc + 1) * 128], rhs=aT[:, k, t0:t0 + n],
                                                                            start=(k == 0), stop=(k == 7)),
                         reads=["why", "aT_all"], writes=[kp])
                off = 1 + t0 if s == 0 else 3 + t0
                S.op("act", lambda e, p=p, rw=rw, off=off, n=n: e.copy(out=rw[:, off:off + n], in_=p[:, :n]), reads=[kp], writes=[krw])
            for (a0, L_, o0) in ((0, SEQ, 0), (SEQ + 2, CTX, SEQ)):
                S.op("act", lambda e, rw=rw, tm=tm, a0=a0, L_=L_, o0=o0, cc=cc: e.activation(
                    out=tm[:, o0:o0 + L_], in_=rw[:, a0 + 1:a0 + 1 + L_], func=AF.Identity, scale=cw[:, cc, 1:2], bias=cw[:, cc, 3:4]),
                    reads=[krw, "hycw"], writes=[ktm])
                S.op("dve", lambda e, rw=rw, tm=tm, a0=a0, L_=L_, o0=o0, cc=cc: e.scalar_tensor_tensor(
                    out=tm[:, o0:o0 + L_], in0=rw[:, a0:a0 + L_], scalar=cw[:, cc, 0:1], in1=tm[:, o0:o0 + L_], op0=ALU.mult, op1=ALU.add),
                    reads=[krw, ktm, "hycw"], writes=[ktm])
                S.op("dve", lambda e, rw=rw, tm=tm, o=o, a0=a0, L_=L_, o0=o0, cc=cc: e.scalar_tensor_tensor(
                    out=o[:, o0:o0 + L_], in0=rw[:, a0 + 2:a0 + 2 + L_], scalar=cw[:, cc, 2:3], in1=tm[:, o0:o0 + L_], op0=ALU.mult, op1=ALU.add),
                    reads=[krw, ktm, "hycw"], writes=[ko])
            S.dma("sp", G["hyc_d"][cc * 128:(cc + 1) * 128, :], o[:], reads=[ko], writes=[("hyc_d", cc)])
            if cc < 4:
                for tg in range(0, NT, 4):
                    nt = min(4, NT - tg)
                    t_ps, t_sb = tp[(tg // 4) % 2], vt[(tg // 4) % 2]
                    kps, ksb = f"hytp{(tg // 4) % 2}", f"hyvt{(tg // 4) % 2}"
                    for i in range(nt):
                        S.op("pe", lambda e, i=i, tg=tg, o=o, t_ps=t_ps: e.transpose(out=t_ps[:, i, :], in_=o[:, (tg + i) * 128:(tg + i + 1) * 128],
                                                                                identity=G["ident_b"][:]),
                             reads=[ko, "ident_b"], writes=[kps])
                    S.op("dve", lambda e, t_ps=t_ps, t_sb=t_sb, nt=nt: e.tensor_copy(out=t_sb[:, :nt, :], in_=t_ps[:, :nt, :]), reads=[kps], writes=[ksb])
                    S.dma("sp", G["vtok_d"][tg * 128:(tg + nt) * 128, cc * 128:(cc + 1) * 128].rearrange("(i p) c -> p i c", p=128),
                          t_sb[:, :nt, :], reads=[ksb], writes=[("vtok_d", cc, tg)])


def phase_hy_filter(C, G, L, zemb_name, out_name):
    S = C.S
    TWO_PI = 2.0 * math.pi
    with C.phase():
        zT = C.sb("zT", [33, L], F32)
        w1 = C.sb("fw1", [33, 64], F32)
        w2 = C.sb("fw2", [64, 64], F32)
        w3 = C.sb("fw3", [64, 2048], F32)
        S.dma("sp", zT[:], G[zemb_name], writes=["zT"])
        S.dma("sp", w1[:], G["hy_f_w1"][0], writes=["fw1"])
        S.dma("sp", w2[:], G["hy_f_w2"][0], writes=["fw2"])
        S.dma("sp", w3[:], G["hy_f_w3"][0], writes=["fw3"])
        pc = load_cols(C, G, "hyfp", [G["hy_f_b1"], G["hy_f_b2"], G["hy_f_freq"][0]], 1, width=64)
        dl = C.sb("delta_bc", [128, 512], F32)
        negt = C.sb("negt", [128, L // 128], F32)
        mask0 = C.sb("mask0", [128, 1], F32)
        S.dma("sp", dl[:], G["c_delta"][0:1, :].to_broadcast([128, 512]), writes=["delta_bc"])
        S.dma("sp", negt[:], G["c_negt_%d" % L], writes=["negt"])
        S.dma("sp", mask0[:], G["c_mask0"], writes=["mask0"])
        h1 = C.sb("fh1", [64, L], F32)
        h2 = C.sb("fh2", [64, L], F32)
        a = C.sb("fa", [64, 512], F32)
        ki = C.sb("fki", [64, 512], I32)
        kf = C.sb("fkf", [64, 512], F32)
        hp = C.ps("fhp", [64, 512], F32)
        nb = max(1, L // 512)
        bs = min(L, 512)
        for layer, (wt, kw, src, ksrc, dst, kdst, bcol, fcol) in enumerate((
                (w1, "fw1", zT, "zT", h1, "fh1", 0, 2), (w2, "fw2", h1, "fh1", h2, "fh2", 1, 3))):
            for b in range(nb):
                sl = slice(b * bs, (b + 1) * bs)
                S.op("pe", lambda e, wt=wt, src=src, sl=sl: e.matmul(hp[:, :bs], lhsT=wt[:], rhs=src[:, sl], start=True, stop=True),
                     reads=[kw, ksrc], writes=["fhp"])
                S.op("dve", lambda e, bcol=bcol, fcol=fcol: e.tensor_scalar(out=a[:, :bs], in0=hp[:, :bs], scalar1=pc[0:64, 0, bcol:bcol + 1],
                                                                           scalar2=pc[0:64, 0, fcol:fcol + 1], op0=ALU.add, op1=ALU.mult),
                     reads=["fhp", "hyfp"], writes=["fa"])
                S.op("dve", lambda e: e.tensor_scalar(out=ki[:, :bs], in0=a[:, :bs], scalar1=1.0 / TWO_PI, scalar2=None, op0=ALU.mult),
                     reads=["fa"], writes=["fki"])
                S.op("dve", lambda e: e.tensor_copy(out=kf[:, :bs], in_=ki[:, :bs]), reads=["fki"], writes=["fkf"])
                S.op("dve", lambda e: e.scalar_tensor_tensor(out=a[:, :bs], in0=kf[:, :bs], scalar=-TWO_PI, in1=a[:, :bs], op0=ALU.mult, op1=ALU.add),
                     reads=["fkf", "fa"], writes=["fa"])
                S.op("dve", lambda e: e.tensor_scalar(out=a[:, :bs], in0=a[:, :bs], scalar1=3.1415925, scalar2=-3.1415925, op0=ALU.min, op1=ALU.max),
                     reads=["fa"], writes=["fa"])
                S.op("act", lambda e, dst=dst, sl=sl: e.activation(out=dst[:, sl], in_=a[:, :bs], func=AF.Sin), reads=["fa"], writes=[kdst])
        p3 = [C.ps(f"fp3_{i}", [128, 512], F32) for i in range(4)]
        dec = [C.sb(f"fdec{i}", [128, 512], F32) for i in range(2)]
        bw = [C.sb(f"fbw{i}", [128, 512], F32) for i in range(2)]
        sm = [C.sb(f"fsm{i}", [128, 512], F32) for i in range(2)]
        df = [C.sb(f"fdf{i}", [128, 512], F32) for i in range(2)]
        fo = [C.sb(f"ffo{i}", [128, 2, 512], BF16) for i in range(2)]
        it = 0
        for lt in range(L // 128):
            dc, kdc = dec[lt % 2], f"fdec{lt % 2}"
            S.op("act", lambda e, dc=dc, lt=lt: e.activation(out=dc[:], in_=dl[:], func=AF.Exp, scale=negt[:, lt:lt + 1]),
                 reads=["delta_bc", "negt"], writes=[kdc])
            for n in range(2):
                for dr in range(2):
                    q = n * 2 + dr
                    S.op("pe", lambda e, q=q, lt=lt: e.matmul(p3[q][:], lhsT=h2[:, lt * 128:(lt + 1) * 128], rhs=w3[:, q * 512:(q + 1) * 512],
                                                           start=True, stop=True),
                         reads=["fh2", "fw3"], writes=[f"fp3_{q}"])
            for n in range(2):
                b_, s_, d_, o_ = bw[it % 2], sm[it % 2], df[it % 2], fo[it % 2]
                kb, ks, kd, kfo = f"fbw{it % 2}", f"fsm{it % 2}", f"fdf{it % 2}", f"ffo{it % 2}"
                it += 1
                pf, pb = p3[n * 2], p3[n * 2 + 1]
                if lt == 0:
                    S.op("act", lambda e, b_=b_, pb=pb: e.activation(out=b_[:], in_=pb[:], func=AF.Identity, scale=mask0[:, 0:1]),
                         reads=[f"fp3_{n * 2 + 1}", "mask0"], writes=[kb])
                else:
                    S.op("act", lambda e, b_=b_, pb=pb: e.copy(out=b_[:], in_=pb[:]), reads=[f"fp3_{n * 2 + 1}"], writes=[kb])
                S.op("dve", lambda e, s_=s_, pf=pf, b_=b_: e.tensor_tensor(out=s_[:], in0=pf[:], in1=b_[:], op=ALU.add),
                     reads=[f"fp3_{n * 2}", kb], writes=[ks])
                S.op("dve", lambda e, d_=d_, pf=pf, b_=b_: e.tensor_tensor(out=d_[:], in0=pf[:], in1=b_[:], op=ALU.subtract),
                     reads=[f"fp3_{n * 2}", kb], writes=[kd])
                S.op("pool", lambda e, o_=o_, s_=s_, dc=dc: e.tensor_tensor(out=o_[:, 0, :], in0=s_[:], in1=dc[:], op=ALU.mult),
                     reads=[ks, kdc], writes=[kfo])
                S.op("pool", lambda e, o_=o_, d_=d_, dc=dc: e.tensor_tensor(out=o_[:, 1, :], in0=d_[:], in1=dc[:], op=ALU.mult),
                     reads=[kd, kdc], writes=[kfo])
                S.dma("sp", G[out_name][n, :, lt * 128:(lt + 1) * 128, :].rearrange("s p c -> p s c"), o_[:], reads=[kfo],
                      writes=[(out_name, n, lt)])


def phase_hyena_conv(C, G, L, tok0, filt_name, Fname, Gname, vtok_src):
    S = C.S
    nsc = L // 128
    npair = L // 128
    tb = min(L, 512)
    ntb = L // tb
    with C.phase():
        ztok = C.sb("ztok", [128, nsc, 512], BF16)
        hb = load_cols(C, G, "hybias", [G["hy_bias"][0]], 4)
        S.dma("sp", ztok[:], G[vtok_src][tok0:tok0 + L, :].rearrange("(c p) n -> p c n", p=128), writes=["ztok"])
        for order in range(2):
            with C.phase():
                fs = C.sb("fs", [128, nsc, 512], BF16)
                fd = C.sb("fd", [128, nsc, 512], BF16)
                S.dma("sp", fs[:], G[filt_name][order, 0].rearrange("(c p) n -> p c n", p=128), writes=["fs"])
                S.dma("sp", fd[:], G[filt_name][order, 1].rearrange("(c p) n -> p c n", p=128), writes=["fd"])
                Y = C.sb("Y", [128, 2 * npair, 512], BF16)
                with C.phase():
                    Ft = [C.sb(f"Ft{i}", [128, 2, nsc, 128], BF16) for i in range(2)]
                    zp = [C.ps(f"zp{i}", [128, 4, 512], F32) for i in range(2)]
                    hs = [C.sb(f"hs{i}", [128, 2, 512], F32) for i in range(2)]
                    t1 = [C.sb(f"t1_{i}", [128, 2, 512], F32) for i in range(2)]
                    t2 = [C.sb(f"t2_{i}", [128, 2, 512], F32) for i in range(2)]
                    for j in range(npair):
                        F_, kF = Ft[j % 2], f"Ft{j % 2}"
                        z, kz = zp[j % 2], f"zp{j % 2}"
                        h_, kh = hs[j % 2], f"hs{j % 2}"
                        a_, ka = t1[j % 2], f"t1_{j % 2}"
                        b_, kb = t2[j % 2], f"t2_{j % 2}"
                        S.dma("sp", F_[:, 0], G[Fname][j], writes=[kF])
                        S.dma("sp", F_[:, 1], G[Fname][npair + j], writes=[kF])
                        for q, (ri, mv, kmv) in enumerate(((0, ztok, "ztok"), (1, ztok, "ztok"), (0, fs, "fs"), (1, fd, "fd"))):
                            for c in range(nsc):
                                S.op("pe", lambda e, z=z, q=q, ri=ri, c=c, mv=mv, F_=F_: e.matmul(z[:, q, :], lhsT=F_[:, ri, c, :], rhs=mv[:, c, :],
                                                                                             start=(c == 0), stop=(c == nsc - 1)),
                                     reads=[kF, kmv], writes=[kz])
                        S.op("act", lambda e, z=z, h_=h_: e.copy(out=h_[:], in_=z[:, 2:4, :]), reads=[kz], writes=[kh])
                        S.op("dve", lambda e, z=z, h_=h_, a_=a_: e.tensor_tensor(out=a_[:], in0=z[:, 0:2, :], in1=h_[:], op=ALU.mult),
                             reads=[kz, kh], writes=[ka])
                        S.op("dve", lambda e, z=z, h_=h_, b_=b_: e.tensor_tensor(out=b_[:, 0, :], in0=z[:, 0, :], in1=h_[:, 1, :], op=ALU.mult),
                             reads=[kz, kh], writes=[kb])
                        S.op("dve", lambda e, z=z, h_=h_, b_=b_: e.tensor_tensor(out=b_[:, 1, :], in0=z[:, 1, :], in1=h_[:, 0, :], op=ALU.mult),
                             reads=[kz, kh], writes=[kb])
                        S.op("pool", lambda e, a_=a_, j=j, Y=Y: e.tensor_tensor(out=Y[:, j, :], in0=a_[:, 0, :], in1=a_[:, 1, :], op=ALU.subtract),
                             reads=[ka], writes=["Y"])
                        S.op("pool", lambda e, b_=b_, j=j, Y=Y: e.tensor_tensor(out=Y[:, npair + j, :], in0=b_[:, 0, :], in1=b_[:, 1, :], op=ALU.add),
                             reads=[kb], writes=["Y"])
                with C.phase():
                    KG = 4
                    Gt = [C.sb(f"Gt{i}", [128, KG, tb], BF16) for i in range(3)]
                    op_ = [C.ps(f"op{i}", [128, tb], F32) for i in range(4)]
                    zprev = [C.sb(f"zprev{i}", [128, tb], BF16) for i in range(4)]
                    gate = [C.sb(f"gate{i}", [128, tb], BF16) for i in range(4)]
                    tmpf = [C.sb(f"tmpf{i}", [128, tb], F32) for i in range(2)]
                    zn = [C.sb(f"zn{i}", [128, tb], BF16) for i in range(2)]
                    ttp = [C.ps(f"ttp{i}", [128, 4, 128], BF16) for i in range(2)]
                    ig = 0
                    ie = 0
                    for b in range(ntb):
                        for kg in range(2 * npair // KG):
                            g_, kg_ = Gt[ig % 3], f"Gt{ig % 3}"
                            ig += 1
                            S.dma("sp", g_[:], G[Gname][b, kg], writes=[kg_])
                            for kk in range(KG):
                                kc = kg * KG + kk
                                for cc in range(4):
                                    S.op("pe", lambda e, cc=cc, kc=kc, kk=kk, g_=g_, Y=Y, op_=op_: e.matmul(op_[cc][:], lhsT=Y[:, kc, cc * 128:(cc + 1) * 128], rhs=g_[:, kk, :],
                                                                                         start=(kc == 0), stop=(kc == 2 * npair - 1)),
                                         reads=["Y", kg_], writes=[f"op{cc}"])
                        t_lo = tok0 + b * tb
                        for cc in range(4):
                            zp_, kzp = zprev[ie % 4], f"zprev{ie % 4}"
                            gt_, kgt = gate[ie % 4], f"gate{ie % 4}"
                            tf_, ktf = tmpf[ie % 2], f"tmpf{ie % 2}"
                            zn_, kzn = zn[ie % 2], f"zn{ie % 2}"
                            tp_, ktp = ttp[ie % 2], f"ttp{ie % 2}"
                            ie += 1
                            if order == 0:
                                S.dma("sp", zp_[:], G["hyc_d"][cc * 128:(cc + 1) * 128, t_lo:t_lo + tb], writes=[kzp])
                            else:
                                S.dma("sp", zp_[:], G["z1_d"][cc * 128:(cc + 1) * 128, t_lo:t_lo + tb], reads=[("z1_d", cc, t_lo)], writes=[kzp])
                            grow = (1 + order) * 512 + cc * 128
                            S.dma("sp", gt_[:], G["hyc_d"][grow:grow + 128, t_lo:t_lo + tb], writes=[kgt])
                            S.op("dve", lambda e, tf_=tf_, zp_=zp_, cc=cc, order=order, op_=op_: e.scalar_tensor_tensor(
                                out=tf_[:], in0=zp_[:], scalar=hb[:, cc, order:order + 1], in1=op_[cc][:], op0=ALU.mult, op1=ALU.add),
                                reads=[kzp, f"op{cc}", "hybias"], writes=[ktf])
                            S.op("pool", lambda e, zn_=zn_, tf_=tf_, gt_=gt_: e.tensor_tensor(out=zn_[:], in0=tf_[:], in1=gt_[:], op=ALU.mult),
                                 reads=[ktf, kgt], writes=[kzn])
                            if order == 0:
                                S.dma("pool", G["z1_d"][cc * 128:(cc + 1) * 128, t_lo:t_lo + tb], zn_[:], reads=[kzn], writes=[("z1_d", cc, t_lo)])
                                for i in range(tb // 128):
                                    S.op("pe", lambda e, i=i, zn_=zn_, tp_=tp_: e.transpose(out=tp_[:, i, :], in_=zn_[:, i * 128:(i + 1) * 128],
                                                                                       identity=G["ident_b"][:]),
                                         reads=[kzn, "ident_b"], writes=[ktp])
                                c0 = b * (tb // 128)
                                S.op("act", lambda e, tp_=tp_, cc=cc, c0=c0: e.copy(out=ztok[:, c0:c0 + tb // 128, cc * 128:(cc + 1) * 128],
                                                                                in_=tp_[:, 0:tb // 128, :]),
                                     reads=[ktp], writes=["ztok"])
                            else:
                                S.dma("pool", G["mix_d"][cc * 128:(cc + 1) * 128, t_lo:t_lo + tb], zn_[:], reads=[kzn], writes=[("mix_d", cc, t_lo)])


def qk_prep(C, G, pq, kpq, gain, kgain, cs, sn, tt, rope, dstT, kdst, nh, W, tag):
    S = C.S
    n = nh * 64
    sq, ss, xn, xg, t1, t2, qn, tp = W["sq"], W["ss"], W["xn"], W["xg"], W["t1"], W["t2"], W["qn"], W["tp"]
    k = lambda nm: (W.get("tpkey", (tag, "tp")) if nm == "tp" else (tag, nm))
    S.op("act", lambda e: e.activation(out=sq[:, :n], in_=pq, func=AF.Square), reads=[kpq], writes=[k("sq")])
    yield
    S.op("dve", lambda e: e.reduce_sum(out=ss[:, :nh], in_=sq[:, :n].rearrange("p (h d) -> p h d", d=64), axis=AX.X), reads=[k("sq")], writes=[k("ss")])
    yield
    S.op("act", lambda e: e.activation(out=ss[:, :nh], in_=ss[:, :nh], func=AF.Sqrt, scale=1.0 / 64, bias=EPS), reads=[k("ss")], writes=[k("ss")])
    yield
    S.op("dve", lambda e: e.reciprocal(out=ss[:, :nh], in_=ss[:, :nh]), reads=[k("ss")], writes=[k("ss")])
    yield
    S.op("dve", lambda e: e.tensor_tensor(out=xn[:, :n].rearrange("p (h d) -> p h d", d=64), in0=pq.rearrange("p (h d) -> p h d", d=64),
                                          in1=ss[:, :nh, None].to_broadcast([128, nh, 64]), op=ALU.mult), reads=[kpq, k("ss")], writes=[k("xn")])
    yield
    if rope:
        S.op("pool", lambda e: e.tensor_tensor(out=xg[:, :n].rearrange("p (h d) -> p h d", d=64), in0=xn[:, :n].rearrange("p (h d) -> p h d", d=64),
                                               in1=gain[:, None, :].to_broadcast([128, nh, 64]), op=ALU.mult), reads=[k("xn"), kgain], writes=[k("xg")])
        yield
        xv = xg[:, :n].rearrange("p (h a f) -> p h a f", a=2, f=32)
        x1, x2 = xv[:, :, :, 0:16], xv[:, :, :, 16:32]
        cb = cs[:, tt, None, :].rearrange("p o (a f) -> p o a f", a=2).to_broadcast([128, nh, 2, 16])
        sb_ = sn[:, tt, None, :].rearrange("p o (a f) -> p o a f", a=2).to_broadcast([128, nh, 2, 16])
        h2 = n // 2
        t1v = t1[:, :h2].rearrange("p (h a f) -> p h a f", a=2, f=16)
        t2v = t2[:, :h2].rearrange("p (h a f) -> p h a f", a=2, f=16)
        qv = qn[:, :n].rearrange("p (h a f) -> p h a f", a=2, f=32)
        S.op("dve", lambda e: e.tensor_tensor(out=t1v, in0=x1, in1=cb, op=ALU.mult), reads=[k("xg"), "rope"], writes=[k("t1")])
        yield
        S.op("pool", lambda e: e.tensor_tensor(out=t2v, in0=x2, in1=sb_, op=ALU.mult), reads=[k("xg"), "rope"], writes=[k("t2")])
        yield
        S.op("dve", lambda e: e.tensor_tensor(out=qv[:, :, :, 0:16], in0=t1v, in1=t2v, op=ALU.subtract), reads=[k("t1"), k("t2")], writes=[k("qn")])
        yield
        S.op("pool", lambda e: e.tensor_tensor(out=t1v, in0=x2, in1=cb, op=ALU.mult), reads=[k("xg"), "rope", k("qn")], writes=[k("t1")])
        yield
        S.op("dve", lambda e: e.tensor_tensor(out=t2v, in0=x1, in1=sb_, op=ALU.mult), reads=[k("xg"), "rope", k("qn")], writes=[k("t2")])
        yield
        S.op("pool", lambda e: e.tensor_tensor(out=qv[:, :, :, 16:32], in0=t1v, in1=t2v, op=ALU.add), reads=[k("t1"), k("t2")], writes=[k("qn")])
        yield
    else:
        S.op("pool", lambda e: e.tensor_tensor(out=qn[:, :n].rearrange("p (h d) -> p h d", d=64), in0=xn[:, :n].rearrange("p (h d) -> p h d", d=64),
                                               in1=gain[:, None, :].to_broadcast([128, nh, 64]), op=ALU.mult), reads=[k("xn"), kgain], writes=[k("qn")])
        yield
    nj = n // 128
    for j in range(nj):
        S.op("pe", lambda e, j=j: e.transpose(out=tp[:, j, :], in_=qn[:, j * 128:(j + 1) * 128], identity=G["ident_b"][:]),
             reads=[k("qn"), "ident_b"], writes=[k("tp")])
    S.op("act", lambda e: e.copy(out=dstT[:, 0:nj, tt * 128:(tt + 1) * 128], in_=tp[:, 0:nj, :]), reads=[k("tp")], writes=[kdst])
    yield


def run_rr(gens):
    gens = list(gens)
    while gens:
        for g_ in list(gens):
            try:
                next(g_)
            except StopIteration:
                gens.remove(g_)

def qk_work(C, tag, tp=None):
    return {"sq": C.sb(tag + "sq", [128, 512], F32), "ss": C.sb(tag + "ss", [128, 8], F32), "xn": C.sb(tag + "xn", [128, 512], F32),
            "xg": C.sb(tag + "xg", [128, 512], F32), "t1": C.sb(tag + "t1", [128, 256], F32), "t2": C.sb(tag + "t2", [128, 256], F32),
            "qn": C.sb(tag + "qn", [128, 512], BF16), "tp": tp if tp is not None else C.ps(tag + "tp", [128, 4, 128], BF16)}


def phase_diff_attn(C, G):
    S = C.S
    aT = G["aT"]
    LAM_INIT = 0.8 - 0.6 * math.exp(0.0)
    with C.phase():
        qT = C.sb("qT", [128, 4, T], BF16)
        kT = C.sb("kT", [128, 4, T], BF16)
        vx = C.sb("vx", [128, NT, 4, 129], BF16)
        gq = C.sb("gq", [128, 64], F32)
        gk = C.sb("gk", [128, 64], F32)
        cs = C.sb("ropec", [128, 16, 32], F32)
        sn = C.sb("ropes", [128, 16, 32], F32)
        sub = C.sb("subln", [128, 128], F32)
        lamb = C.sb("lamb", [128, 4, 64], F32)
        lw = C.sb("lamw", [128, 8], F32)
        S.dma("sp", gq[:], G["da_q_norm"][0:1, :].to_broadcast([128, 64]), writes=["gq"])
        S.dma("sp", gk[:], G["da_k_norm"][0:1, :].to_broadcast([128, 64]), writes=["gk"])
        S.dma("sp", sub[:], G["da_subln"][0:1, :].to_broadcast([128, 128]), writes=["subln"])
        S.dma("sp", lamb[:].rearrange("p a b -> p (a b)"), G["da_lam"].rearrange("o a b -> o (a b)").to_broadcast([128, 256]), writes=["lamb"])
        S.dma("sp", cs[:], G["c_cos"].rearrange("(t p) f -> p t f", p=128), writes=["rope"])
        S.dma("sp", sn[:], G["c_sin"].rearrange("(t p) f -> p t f", p=128), writes=["rope"])
        S.op("dve", lambda e: e.tensor_scalar(out=gq[:], in0=gq[:], scalar1=0.125, scalar2=None, op0=ALU.mult), reads=["gq"], writes=["gq"])
        S.op("dve", lambda e: e.tensor_scalar(out=sub[:], in0=sub[:], scalar1=1.0 - LAM_INIT, scalar2=None, op0=ALU.mult), reads=["subln"], writes=["subln"])
        S.op("dve", lambda e: e.tensor_tensor(out=lamb[:, 0, :], in0=lamb[:, 0, :], in1=lamb[:, 1, :], op=ALU.mult), reads=["lamb"], writes=["lamb"])
        S.op("dve", lambda e: e.tensor_tensor(out=lamb[:, 2, :], in0=lamb[:, 2, :], in1=lamb[:, 3, :], op=ALU.mult), reads=["lamb"], writes=["lamb"])
        S.op("dve", lambda e: e.reduce_sum(out=lw[:, 0:1], in_=lamb[:, 0, :], axis=AX.X), reads=["lamb"], writes=["lamw"])
        S.op("dve", lambda e: e.reduce_sum(out=lw[:, 1:2], in_=lamb[:, 2, :], axis=AX.X), reads=["lamb"], writes=["lamw"])
        S.op("act", lambda e: e.activation(out=lw[:, 2:4], in_=lw[:, 0:2], func=AF.Exp), reads=["lamw"], writes=["lamw"])
        S.op("dve", lambda e: e.tensor_tensor(out=lw[:, 4:5], in0=lw[:, 3:4], in1=lw[:, 2:3], op=ALU.subtract), reads=["lamw"], writes=["lamw"])
        S.op("dve", lambda e: e.tensor_scalar(out=lw[:, 5:6], in0=lw[:, 4:5], scalar1=-LAM_INIT, scalar2=None, op0=ALU.add), reads=["lamw"], writes=["lamw"])
        S.op("pool", lambda e: e.memset(vx[:, :, :, 128:129], 1.0), writes=["vx1"])
        with C.phase():
            w = C.sb("wqkv", [128, 8, 1536], BF16)
            S.dma("pool", w[:], G["e_w_in"][0, :, 1536:3072].rearrange("(k p) n -> p k n", p=128), writes=["wqkv"])
            pq = [C.ps(f"pq{i}", [128, 512], F32) for i in range(2)]
            pk = [C.ps(f"pk{i}", [128, 512], F32) for i in range(2)]
            pv = C.ps("pv", [128, 512], F32)
            tps_ = [C.ps(f"qktp{i}", [128, 4, 128], BF16) for i in range(2)]
            Wq = [qk_work(C, f"wq{i}", tps_[i]) for i in range(2)]
            Wk = [qk_work(C, f"wk{i}", tps_[i]) for i in range(2)]
            for i in range(2):
                Wq[i]["tpkey"] = Wk[i]["tpkey"] = f"qktp{i}"
            for t2 in range(0, NT, 2):
                gens = []
                for tt in (t2, t2 + 1):
                    q_, k_ = pq[tt % 2], pk[tt % 2]
                    for (dst, kd, c0) in ((q_, f"pq{tt % 2}", 0), (k_, f"pk{tt % 2}", 512), (pv, "pv", 1024)):
                        for k in range(8):
                            S.op("pe", lambda e, dst=dst, k=k, c0=c0, tt=tt: e.matmul(dst[:], lhsT=aT[:, k, tt * 128:(tt + 1) * 128], rhs=w[:, k, c0:c0 + 512],
                                                                                 start=(k == 0), stop=(k == 7)),
                                 reads=["wqkv"], writes=[kd])
                    S.op("act", lambda e, tt=tt: e.copy(out=vx[:, tt, :, 0:128], in_=pv[:].rearrange("p (h d) -> p h d", d=128)), reads=["pv"], writes=["vx"])
                    rope = tt < 16
                    gens.append(qk_prep(C, G, q_[:], f"pq{tt % 2}", gq, "gq", cs, sn, tt, rope, qT, "qT", 8, Wq[tt % 2], f"wq{tt % 2}"))
                    gens.append(qk_prep(C, G, k_[:], f"pk{tt % 2}", gk, "gk", cs, sn, tt, rope, kT, "kT", 8, Wk[tt % 2], f"wk{tt % 2}"))
                run_rr(gens)
        if G.get("dbg_qk"):
            for nm, tl in (("qT", qT), ("kT", kT)):
                dd = C.nc.dram_tensor("dbgq_" + nm, [128, 4 * T], BF16, kind="ExternalOutput").ap()
                G.setdefault("final_ops2", []).append(S.dma("sp", dd, tl[:].rearrange("p a b -> p (a b)"), reads=[nm]))
        with C.phase():
            sp = [C.ps(f"sp{i}", [128, 512], F32) for i in range(2)]
            oacc = [[C.ps(f"oacc{s_}_{b}", [128, 2, 129], F32) for b in range(2)] for s_ in range(2)]
            tpo = C.ps("tpo", [128, 4, 128], BF16)
            pt = [C.sb(f"pt{i}", [128, 512], BF16) for i in range(3)]
            osb2 = [[C.sb(f"osb{b}_{m}", [128, 4, 128], F32) for m in range(2)] for b in range(2)]
            rc2 = [C.sb(f"rc{b}", [128, 8], F32) for b in range(2)]
            dsc = C.sb("dsc", [128, 128], F32)
            on2 = [C.sb(f"on{b}", [128, 4, 128], BF16) for b in range(2)]
            pending = []
            oT = [C.sb(f"oT{i}", [128, 512], BF16) for i in range(2)]
            its = []
            for h in range(4):
                for (q0, nq, kts) in ((0, 512, range(NT)), (512, 512, range(NT)), (1024, 512, range(NT)), (1536, 512, range(NT)),
                                      (2048, 256, (16, 17))):
                    kts = list(kts)
                    for m in range(2):
                        for kt in kts:
                            its.append(dict(h=h, q0=q0, nq=nq, m=m, kt=kt, first=(kt == kts[0]), last=(kt == kts[-1])))

            def emit_s(i):
                d_ = its[i]
                s_, ks_ = sp[i % 2], f"sp{i % 2}"
                p_, kp_ = pt[i % 3], f"pt{i % 3}"
                pb = slice(d_["m"] * 64, (d_["m"] + 1) * 64)
                h, kt, q0, nq = d_["h"], d_["kt"], d_["q0"], d_["nq"]
                S.op("pe", lambda e, s_=s_, pb=pb, h=h, kt=kt, q0=q0, nq=nq: e.matmul(
                    s_[:, :nq], lhsT=kT[pb, h, kt * 128:(kt + 1) * 128], rhs=qT[pb, h, q0:q0 + nq], start=True, stop=True),
                    reads=["qT", "kT"], writes=[ks_])
                S.op("act", lambda e, s_=s_, p_=p_, nq=nq: e.activation(out=p_[:, :nq], in_=s_[:, :nq], func=AF.Exp), reads=[ks_], writes=[kp_])

            ib = 0
            rnd = [-1]
            emit_s(0)
            for i in range(len(its)):
                if i + 1 < len(its):
                    emit_s(i + 1)
                d_ = its[i]
                h, kt, q0, nq, m = d_["h"], d_["kt"], d_["q0"], d_["nq"], d_["m"]
                nqs = nq // 128
                p_, kp_ = pt[i % 3], f"pt{i % 3}"
                if d_["first"]:
                    rnd[0] += 1
                ss_ = rnd[0] % 2
                for qs in range(nqs):
                    S.op("pe", lambda e, qs=qs, p_=p_, kt=kt, h=h, ss_=ss_, f_=(d_["first"] and qs % 2 == 0), l_=d_["last"]: e.matmul(
                        oacc[ss_][qs // 2][:, qs % 2, :], lhsT=p_[:, qs * 128:(qs + 1) * 128], rhs=vx[:, kt, h, :], start=f_, stop=l_, skip_group_check=True),
                        reads=[kp_, "vx", "vx1"], writes=[f"oacc{ss_}_{qs // 2}"])
                if pending and pending[0][0] <= i:
                    pending.pop(0)[1]()
                if not d_["last"]:
                    continue
                bsel = ib % 2
                osb, rc, on = osb2[bsel], rc2[bsel], on2[bsel]
                ko = [f"osb{bsel}_0", f"osb{bsel}_1"]
                krc, kon = f"rc{bsel}", f"on{bsel}"
                for b in range(nqs // 2):
                    acc, kacc = oacc[ss_][b], f"oacc{ss_}_{b}"
                    c0 = m * 4 + 2 * b
                    S.op("dve", lambda e, acc=acc, rc=rc, c0=c0: e.reciprocal(out=rc[:, c0:c0 + 2], in_=acc[:, :, 128]),
                         reads=[kacc], writes=[krc])
                    S.op("dve", lambda e, acc=acc, rc=rc, c0=c0, osb=osb, m=m, b=b: e.tensor_tensor(
                        out=osb[m][:, 2 * b:2 * b + 2, :], in0=acc[:, :, 0:128], in1=rc[:, c0:c0 + 2, None].to_broadcast([128, 2, 128]), op=ALU.mult),
                        reads=[kacc, krc], writes=[ko[m]])
                if m == 0:
                    continue
                o_T, ko_T = oT[ib % 2], f"oT{ib % 2}"
                ib += 1
                for qs in range(nqs):
                    S.op("dve", lambda e, qs=qs, osb=osb: e.scalar_tensor_tensor(out=osb[0][:, qs, :], in0=osb[1][:, qs, :], scalar=lw[:, 5:6], in1=osb[0][:, qs, :],
                                                                                op0=ALU.mult, op1=ALU.add),
                         reads=ko + ["lamw"], writes=[ko[0]])
                    S.op("act", lambda e, qs=qs, osb=osb, rc=rc: e.activation(out=dsc[:], in_=osb[0][:, qs, :], func=AF.Square, accum_out=rc[:, qs:qs + 1]),
                         reads=[ko[0], krc], writes=["dsc", krc])
                    S.op("act", lambda e, qs=qs, rc=rc: e.activation(out=rc[:, qs:qs + 1], in_=rc[:, qs:qs + 1], func=AF.Sqrt, scale=1.0 / 128, bias=EPS),
                         reads=[krc], writes=[krc])
                    S.op("dve", lambda e, qs=qs, rc=rc: e.reciprocal(out=rc[:, qs:qs + 1], in_=rc[:, qs:qs + 1]), reads=[krc], writes=[krc])
                    S.op("dve", lambda e, qs=qs, rc=rc, osb=osb, on=on: e.scalar_tensor_tensor(out=on[:, qs, :], in0=osb[0][:, qs, :], scalar=rc[:, qs:qs + 1], in1=sub[:],
                                                                                            op0=ALU.mult, op1=ALU.mult),
                         reads=[ko[0], krc, "subln"], writes=[kon])

                def fin_(nqs=nqs, on=on, kon=kon, o_T=o_T, ko_T=ko_T, h=h, q0=q0, nq=nq):
                    for qs in range(nqs):
                        S.op("pe", lambda e, qs=qs, on=on: e.transpose(out=tpo[:, qs, :], in_=on[:, qs, :], identity=G["ident_b"][:]), reads=[kon, "ident_b"], writes=["tpo"])
                    S.op("act", lambda e, o_T=o_T, nqs=nqs: e.copy(out=o_T[:, :nqs * 128].rearrange("p (a b) -> p a b", b=128), in_=tpo[:, 0:nqs, :]),
                         reads=["tpo"], writes=[ko_T])
                    S.dma("sp", G["mix_d"][512 + h * 128:512 + (h + 1) * 128, q0:q0 + nq], o_T[:, :nq], reads=[ko_T], writes=[("mix_d", "a", h, q0)])
                pending.append((i + 6, fin_))
            for _, f_ in pending:
                f_()


def phase_outproj(C, G, l, w_name, ntok):
    S = C.S
    hT, aT, modc = G["hT"], G["aT"], G["modc"]
    with C.phase():
        w = C.sb("wout", [128, 8, D], BF16)
        S.dma("pool", w[:], G[w_name][0].rearrange("(k p) n -> p k n", p=128), writes=["wout"])
        for k in range(8):
            S.dma("sp", aT[:, k, 0:ntok], G["mix_d"][k * 128:(k + 1) * 128, 0:ntok], writes=[("aTm", k)])
        akeys = [("aTm", k) for k in range(8)]
        pp = [C.ps(f"opp{i}", [128, 512], F32) for i in range(4)]
        ip = 0
        for (t0, n, s) in token_blocks():
            if t0 >= ntok:
                continue
            hkeys = [("hT", tt) for tt in range(t0 // 128, (t0 + n) // 128)]
            for j in range(8):
                p, kp = pp[ip % 4], f"opp{ip % 4}"
                ip += 1
                for k in range(8):
                    S.op("pe", lambda e, p=p, j=j, k=k, t0=t0, n=n: e.matmul(p[:, :n], lhsT=w[:, k, j * 128:(j + 1) * 128], rhs=aT[:, k, t0:t0 + n],
                                                                         start=(k == 0), stop=(k == 7)),
                         reads=["wout"] + akeys, writes=[kp])
                S.op("dve", lambda e, p=p, j=j, t0=t0, n=n, s=s: e.scalar_tensor_tensor(
                    out=hT[:, j, t0:t0 + n], in0=p[:, :n], scalar=modc[:, l, 16 + j, s:s + 1], in1=hT[:, j, t0:t0 + n], op0=ALU.mult, op1=ALU.add),
                    reads=[kp, "modc"] + hkeys, writes=hkeys)


def spill_h(C, G, to_dram):
    S = C.S
    with C.phase():
        for k in range(8):
            if to_dram:
                S.dma("sp", G["hT_d"][:, k * T:(k + 1) * T], G["hT"][:, k, :])
            else:
                S.dma("sp", G["hT"][:, k, :], G["hT_d"][:, k * T:(k + 1) * T])


def build_program(stages=("load", "mods", "l0mix", "l0moe", "l1mix", "l1moe", "store"), debug=False, dbg_moe=None, EG=2, odd_parts="rg", l0parts="iafc"):
    nc = bass.Bass("TRN2", target_bir_lowering=False)
    C = Ctx(nc)
    G = {"dbg_moe": dbg_moe, "EG": EG, "dbg_qk": bool(debug), "odd_parts": odd_parts, "l0parts": l0parts}
    dbgk = {"kind": "ExternalOutput"} if debug else {}

    def din(name, shape, dtype=F32):
        G[name] = nc.dram_tensor(name, list(shape), dtype, kind="ExternalInput").ap()

    def dscr(name, shape, dtype):
        G[name] = nc.dram_tensor(name, list(shape), dtype, **dbgk).ap()

    din("x", [SEQ, D]); din("ctx", [CTX, D]); din("c2", [2, D])
    din("ada_w", [2, D, 6 * D]); din("ada_b", [2, 6 * D])
    din("e_w_in", [1, D, 3072]); din("e_w_out", [1, D, D])
    din("hy_conv_w", [1, 3, 1536]); din("hy_conv_b", [1, 1536])
    din("hy_f_w1", [1, 33, 64]); din("hy_f_b1", [1, 64]); din("hy_f_w2", [1, 64, 64]); din("hy_f_b2", [1, 64])
    din("hy_f_w3", [1, 64, 2048]); din("hy_f_freq", [1, 2, 64]); din("hy_bias", [1, 2, 512])
    din("da_q_norm", [1, 64]); din("da_k_norm", [1, 64]); din("da_lam", [1, 4, 64]); din("da_subln", [1, 128])
    din("o_w_in", [1, D, 2816]); din("o_w_out", [1, D, D])
    din("ret_decay", [1, 2, 4]); din("ret_gn", [1, 512]); din("gq_q_norm", [1, 64]); din("gq_k_norm", [1, 64]); din("gq_sink", [1, 8])
    din("moe_w_grp", [2, D, 4]); din("moe_b_grp", [2, 4]); din("moe_w_rt", [2, D, 32]); din("moe_b_rt", [2, 32])
    din("moe_w_gate", [2, 4, 8, D, DEXP]); din("moe_w_up", [2, 4, 8, D, DEXP]); din("moe_w_down", [2, 4, 8, DEXP, D])
    for name, (shape, dtype) in CONST_SPECS.items():
        din(name, shape, dtype)
    G["out"] = nc.dram_tensor("out", [SEQ, D], F32, kind="ExternalOutput").ap()
    dscr("combT_d", [NEXP, T], BF16)
    dscr("hyc_d", [1536, T], BF16)
    dscr("vtok_d", [T, 512], BF16)
    dscr("filt_d", [2, 2, SEQ, 512], BF16)
    dscr("filtc_d", [2, 2, CTX, 512], BF16)
    dscr("z1_d", [512, T], BF16)
    dscr("mix_d", [D, T], BF16)
    dscr("hT_d", [128, 8 * T], F32)

    S = C.S
    with contextlib.ExitStack() as st0:
        C.stack = st0
        G["modc"] = C.sb("modc", [128, 2, 48, 2], F32)
        G["ident_f"] = C.sb("ident_f", [128, 128], F32)
        G["ident_b"] = C.sb("ident_b", [128, 128], BF16)
        G["ones_f"] = C.sb("ones_f", [128, 128], F32)
        S.dma("sp", G["ident_f"][:], G["c_ident_f"], writes=["ident_f"])
        S.dma("sp", G["ident_b"][:], G["c_ident_b"], writes=["ident_b"])
        S.op("pool", lambda e: e.memset(G["ones_f"][:], 1.0), writes=["ones_f"])

        def open_scopes():
            stA = contextlib.ExitStack()
            C.stack = stA
            G["aT"] = C.sb("aT", [128, 8, T], BF16)
            stH = contextlib.ExitStack()
            C.stack = stH
            G["hT"] = C.sb("hT", [128, 8, T], F32)
            return stA, stH

        l0mix = "l0mix" in stages
        l1mix = "l1mix" in stages
        stA, stH = open_scopes()
        if "mods" in stages:
            phase_mods(C, G, mid=(lambda: phase_load(C, G)) if "load" in stages else None)
        elif "load" in stages:
            phase_load(C, G)
        if l0mix:
            phase_norm(C, G, 0, 0)
        spill_h(C, G, True)
        stH.close()
        C.stack = stA
        lp = G.get("l0parts", "iafc")
        if l0mix:
            if "i" in lp:
                phase_hy_inproj(C, G)
            if "a" in lp:
                phase_diff_attn(C, G)
            if "f" in lp:
                phase_hy_filter(C, G, SEQ, "c_zemb_2048", "filt_d")
                phase_hy_filter(C, G, CTX, "c_zemb_256", "filtc_d")
        S.barrier()
        stA.close()
        if l0mix and "c" in lp:
            C.stack = st0
            phase_hyena_conv(C, G, SEQ, 0, "filt_d", "c_F2048", "c_G2048", "vtok_d")
            phase_hyena_conv(C, G, CTX, SEQ, "filtc_d", "c_F256", "c_G256", "vtok_d")
        stA, stH = open_scopes()
        spill_h(C, G, False)
        if "dbgnorm" in stages:
            phase_norm(C, G, 0, 0)
        if l0mix:
            phase_outproj(C, G, 0, "e_w_out", T)
        if "l0moe" in stages:
            phase_norm(C, G, 0, 1, router=True)
            phase_moe(C, G, 0, T)
        fin = []
        dbg_done = False

        def dbg_dump():
            if debug:
                for name in debug:
                    t = G[name]
                    shp = list(t.shape)
                    dd = nc.dram_tensor("dbg_" + name, [shp[0], int(np.prod(shp[1:]))], t.dtype, kind="ExternalOutput").ap()
                    src = t[:]
                    if len(shp) == 3:
                        src = src.rearrange("p a b -> p (a b)")
                    elif len(shp) == 4:
                        src = src.rearrange("p a b c -> p (a b c)")
                    fin.append(S.dma("sp", dd, src))
                S.barrier()

        if l1mix:
            phase_norm(C, G, 1, 0)
            spill_h(C, G, True)
            stH.close()
            C.stack = stA
            phase_odd_mixer(C, G)
            S.barrier()
            stA.close()
            stA, stH = open_scopes()
            spill_h(C, G, False)
            phase_outproj(C, G, 1, "o_w_out", SEQ)
        if "l1moe" in stages:
            phase_norm(C, G, 1, 1, router=True, ntok=SEQ)
            phase_moe(C, G, 1, SEQ)
        if "store" in stages:
            phase_store(C, G)
        dbg_dump()
        fin = list(G.get("final_ops", ())) + list(G.get("final_ops2", ())) + fin
        S.barrier()
        stH.close()
        stA.close()
        counts = S.emit(final_wait_ops=fin)
    return nc, counts


def _dft_consts(L):
    N = 2 * L
    k = np.arange(L, dtype=np.float64)
    s = np.arange(L, dtype=np.float64)
    th = 2.0 * np.pi * (k + 0.5) / N
    ang = np.outer(s, th)
    F = np.concatenate([np.cos(ang), -np.sin(ang)], axis=1)
    nsc = L // 128
    Ft = F.reshape(nsc, 128, 2 * nsc, 128).transpose(2, 1, 0, 3)
    Gm = F.T / L
    tb = min(L, 512)
    ntb = L // tb
    KG = 4
    ng = 2 * nsc // KG
    Gt = Gm.reshape(ng, KG, 128, ntb, tb).transpose(3, 0, 2, 1, 4)
    return np.ascontiguousarray(Ft).astype(ml_dtypes.bfloat16), np.ascontiguousarray(Gt).astype(ml_dtypes.bfloat16)


def _zemb(L):
    t = np.linspace(0.0, 1.0, L)[:, None]
    w = (2.0 * np.pi / L) * np.arange(L)[:, None]
    fb = np.linspace(1e-4, 15.0, 16)[None, :]
    z = np.concatenate([t, np.cos(fb * w), -np.sin(fb * w)], axis=-1)
    return np.ascontiguousarray(z.T).astype(np.float32)


def _negt(L):
    t = np.linspace(0.0, 1.0, L)
    return np.ascontiguousarray(-t.reshape(L // 128, 128).T).astype(np.float32)


CONST_SPECS = {
    "c_ident_f": ([128, 128], F32), "c_ident_b": ([128, 128], BF16),
    "c_cos": ([SEQ, 32], F32), "c_sin": ([SEQ, 32], F32),
    "c_delta": ([1, 512], F32), "c_negt_2048": ([128, 16], F32), "c_negt_256": ([128, 2], F32), "c_mask0": ([128, 1], F32),
    "c_zemb_2048": ([33, SEQ], F32), "c_zemb_256": ([33, CTX], F32),
    "c_F2048": ([32, 128, 16, 128], BF16), "c_G2048": ([4, 8, 128, 4, 512], BF16),
    "c_F256": ([4, 128, 2, 128], BF16), "c_G256": ([1, 1, 128, 4, 256], BF16),
    "c_cos_rt": ([SEQ, 64], F32), "c_sin_rt": ([SEQ, 64], F32), "c_ret": ([5, 128, 128], F32), "c_retcol": ([128, 2], F32),
    "c_gmask": ([2, 128, 128], BF16),
}
_CONSTS = None


def host_constants():
    global _CONSTS
    if _CONSTS is not None:
        return _CONSTS
    c = {"c_ident_f": np.eye(128, dtype=np.float32), "c_ident_b": np.eye(128).astype(ml_dtypes.bfloat16)}
    tok = np.arange(SEQ)
    inv = 10000.0 ** (-np.arange(16, dtype=np.float64) / 16)
    ang = np.concatenate([(tok // 64)[:, None] * inv[None, :], (tok % 64)[:, None] * inv[None, :]], axis=1)
    c["c_cos"] = np.cos(ang).astype(np.float32)
    c["c_sin"] = np.sin(ang).astype(np.float32)
    mx, mn = math.log(1e-2) / 0.3, math.log(1e-2) / 1.5
    c["c_delta"] = np.abs(np.linspace(mn, mx, 512)).astype(np.float32)[None, :]
    c["c_negt_2048"] = _negt(SEQ)
    c["c_negt_256"] = _negt(CTX)
    m0 = np.ones((128, 1), np.float32); m0[0, 0] = 0.0
    c["c_mask0"] = m0
    c["c_zemb_2048"] = _zemb(SEQ)
    c["c_zemb_256"] = _zemb(CTX)
    c["c_F2048"], c["c_G2048"] = _dft_consts(SEQ)
    c["c_F256"], c["c_G256"] = _dft_consts(CTX)
    inv_rt = 1.0 / (10000.0 ** np.linspace(0.0, 1.0, 64))
    ang_rt = np.arange(SEQ, dtype=np.float64)[:, None] * inv_rt[None, :]
    c["c_cos_rt"] = np.cos(ang_rt).astype(np.float32)
    c["c_sin_rt"] = np.sin(ang_rt).astype(np.float32)
    j = np.arange(128)[:, None]; i = np.arange(128)[None, :]
    relf = np.maximum(i - j, 0); maskf = (i >= j)
    relb = np.maximum(j - i, 0); maskb = (j >= i)
    iota1 = np.broadcast_to(np.arange(1, 129)[None, :], (128, 128))
    c["c_ret"] = np.stack([relf, maskf, relb, maskb, iota1]).astype(np.float32)
    c["c_retcol"] = np.stack([127 - np.arange(128), np.arange(128)], axis=1).astype(np.float32)
    c["c_gmask"] = np.stack([(i <= j), (j <= i)]).astype(ml_dtypes.bfloat16)
    _CONSTS = c
    return c


WEIGHT_KEYS = ["ada_w", "ada_b", "e_w_in", "e_w_out", "hy_conv_w", "hy_conv_b", "hy_f_w1", "hy_f_b1", "hy_f_w2", "hy_f_b2", "hy_f_w3",
               "hy_f_freq", "hy_bias", "da_q_norm", "da_k_norm", "da_lam", "da_subln", "o_w_in", "o_w_out", "ret_decay", "ret_gn",
               "gq_q_norm", "gq_k_norm", "gq_sink", "moe_w_grp", "moe_b_grp", "moe_w_rt", "moe_b_rt", "moe_w_gate", "moe_w_up", "moe_w_down"]


def make_in_maps(inputs, ncores=8):
    consts = host_constants()
    maps = []
    for b in range(ncores):
        m = {"x": np.ascontiguousarray(inputs["x"][b]), "ctx": np.ascontiguousarray(inputs["ctx"][b]),
             "c2": np.ascontiguousarray(np.stack([inputs["c"][b], inputs["c_ctx"]], axis=0))}
        for k in WEIGHT_KEYS:
            m[k] = np.ascontiguousarray(inputs[k])
        m.update(consts)
        maps.append(m)
    return maps


def kernel(**inputs):
    inputs = {k: np.asarray(v) for k, v in inputs.items()}
    nc, _ = build_program()
    in_maps = make_in_maps(inputs, 8)
    res = run_bass_kernel_spmd(nc, in_maps, core_ids=list(range(8)))
    return np.stack([np.asarray(r["out"]) for r in res.results], axis=0).astype(np.float32)


def phase_odd_mixer(C, G):
    parts = G.get("odd_parts", "rg")
    if "r" in parts:
        phase_retention(C, G)
    if "g" in parts:
        phase_gqa(C, G)


def phase_retention(C, G):
    S = C.S
    aT = G["aT"]
    RS = 128 ** -0.5
    with C.phase():
        qT = C.sb("rqT", [128, 4, SEQ], BF16)
        kT = C.sb("rkT", [128, 4, T], BF16)
        ktok = C.sb("rktok", [128, NT, 4, 128], BF16)
        vv = C.sb("rv", [128, NT, 512], BF16)
        sg = C.sb("rsg", [128, 16, 512], BF16)
        with C.phase():
            w = C.sb("wret", [128, 8, 2048], BF16)
            S.dma("pool", w[:], G["o_w_in"][0, :, 0:2048].rearrange("(k p) n -> p k n", p=128), writes=["wret"])
            cs = C.sb("rtc", [128, 16, 64], F32)
            sn = C.sb("rts", [128, 16, 64], F32)
            S.dma("sp", cs[:], G["c_cos_rt"].rearrange("(t p) f -> p t f", p=128), writes=["rtrope"])
            S.dma("sp", sn[:], G["c_sin_rt"].rearrange("(t p) f -> p t f", p=128), writes=["rtrope"])
            pp = [C.ps(f"rpp{i}", [128, 512], F32) for i in range(4)]
            tp = [C.ps(f"rtp{i}", [128, 4, 128], BF16) for i in range(2)]
            xs = [[C.sb(f"rxs{p}{i}", [128, 512], F32) for i in range(2)] for p in range(2)]
            t1 = [[C.sb(f"rt1{p}{i}", [128, 256], F32) for i in range(2)] for p in range(2)]
            t2 = [[C.sb(f"rt2{p}{i}", [128, 256], F32) for i in range(2)] for p in range(2)]
            qn = [C.sb(f"rqn{p}", [128, 512], BF16) for p in range(2)]

            def chain(tt, which):
                lat = tt < 16
                p = tt % 2
                x_, kx = xs[p][which], f"rxs{p}{which}"
                a_, ka = t1[p][which], f"rt1{p}{which}"
                b_, kb = t2[p][which], f"rt2{p}{which}"
                dst = qn[p][:] if which == 0 else ktok[:, tt].rearrange("p h d -> p (h d)")
                kdst = f"rqn{p}" if which == 0 else ("rktok", tt)
                if lat:
                    S.op("act", lambda e: e.activation(out=x_[:], in_=pp[which][:], func=AF.Identity, scale=(1.0 if which == 0 else RS)),
                         reads=[f"rpp{which}"], writes=[kx])
                    yield
                    xv = x_[:].rearrange("p (h a f) -> p h a f", a=2, f=64)
                    x1, x2 = xv[:, :, 0, :], xv[:, :, 1, :]
                    cb = cs[:, tt, None, :].to_broadcast([128, 4, 64])
                    sb_ = sn[:, tt, None, :].to_broadcast([128, 4, 64])
                    av = a_[:].rearrange("p (h f) -> p h f", f=64)
                    bv = b_[:].rearrange("p (h f) -> p h f", f=64)
                    dv_ = dst.rearrange("p (h a f) -> p h a f", a=2, f=64)
                    S.op("dve", lambda e: e.tensor_tensor(out=av, in0=x1, in1=cb, op=ALU.mult), reads=[kx, "rtrope"], writes=[ka])
                    yield
                    S.op("pool", lambda e: e.tensor_tensor(out=bv, in0=x2, in1=sb_, op=ALU.mult), reads=[kx, "rtrope"], writes=[kb])
                    yield
                    S.op("dve", lambda e: e.tensor_tensor(out=dv_[:, :, 0, :], in0=av, in1=bv, op=ALU.subtract), reads=[ka, kb], writes=[kdst])
                    yield
                    S.op("pool", lambda e: e.tensor_tensor(out=av, in0=x2, in1=cb, op=ALU.mult), reads=[kx, "rtrope"], writes=[ka])
                    yield
                    S.op("dve", lambda e: e.tensor_tensor(out=bv, in0=x1, in1=sb_, op=ALU.mult), reads=[kx, "rtrope"], writes=[kb])
                    yield
                    S.op("pool", lambda e: e.tensor_tensor(out=dv_[:, :, 1, :], in0=av, in1=bv, op=ALU.add), reads=[ka, kb], writes=[kdst])
                    yield
                else:
                    S.op("act", lambda e: e.activation(out=dst, in_=pp[1][:], func=AF.Identity, scale=RS), reads=["rpp1"], writes=[kdst])
                    yield
                tp_, ktp = tp[which], f"rtp{which}"
                for j in range(4):
                    S.op("pe", lambda e, j=j: e.transpose(out=tp_[:, j, :], in_=dst[:, j * 128:(j + 1) * 128], identity=G["ident_b"][:]),
                         reads=[kdst, "ident_b"], writes=[ktp])
                dT, kdT = (qT, "rqT") if which == 0 else (kT, "rkT")
                S.op("act", lambda e: e.copy(out=dT[:, :, tt * 128:(tt + 1) * 128], in_=tp_[:]), reads=[ktp], writes=[kdT])
                yield

            for t2_ in range(0, NT, 2):
                gens = []
                for tt in (t2_, t2_ + 1):
                    lat = tt < 16
                    cols = ((0, 0), (1, 512), (2, 1024), (3, 1536)) if lat else ((1, 512), (2, 1024))
                    for (pi, c0) in cols:
                        for k in range(8):
                            S.op("pe", lambda e, pi=pi, c0=c0, k=k, tt=tt: e.matmul(pp[pi][:], lhsT=aT[:, k, tt * 128:(tt + 1) * 128], rhs=w[:, k, c0:c0 + 512],
                                                                               start=(k == 0), stop=(k == 7)),
                                 reads=["wret"], writes=[f"rpp{pi}"])
                    S.op("act", lambda e, tt=tt: e.copy(out=vv[:, tt, :], in_=pp[2][:]), reads=["rpp2"], writes=["rv"])
                    if lat:
                        S.op("act", lambda e, tt=tt: e.activation(out=sg[:, tt, :], in_=pp[3][:], func=AF.Silu), reads=["rpp3"], writes=["rsg"])
                    for which in ((0, 1) if lat else (1,)):
                        g_ = chain(tt, which)
                        next(g_)
                        gens.append(g_)
                run_rr(gens)
        with C.phase():
            lg = C.sb("rlg", [128, 8], F32)
            S.dma("sp", lg[:], G["ret_decay"].rearrange("o a b -> o (a b)").to_broadcast([128, 8]), writes=["rlg"])
            S.op("act", lambda e: e.activation(out=lg[:], in_=lg[:], func=AF.Exp, scale=-1.0), reads=["rlg"], writes=["rlg"])
            S.op("act", lambda e: e.activation(out=lg[:], in_=lg[:], func=AF.Ln, bias=1.0), reads=["rlg"], writes=["rlg"])
            S.op("dve", lambda e: e.tensor_scalar(out=lg[:], in0=lg[:], scalar1=-1.0, scalar2=None, op0=ALU.mult), reads=["rlg"], writes=["rlg"])
            cst = C.sb("rcst", [128, 5, 128], F32)
            S.dma("sp", cst[:], G["c_ret"].rearrange("a p f -> p a f"), writes=["rcst"])
            ccol = C.sb("rccol", [128, 2], F32)
            S.dma("sp", ccol[:], G["c_retcol"], writes=["rccol"])
            DT = C.sb("rDT", [128, 8, 128], F32)
            XI = C.sb("rXI", [128, 8, 128], F32)
            zc = C.sb("rzc", [128, 8], F32)
            g128 = C.sb("rg128", [128, 8], F32)
            xib = C.sb("rxib", [128, 128], F32)
            S.op("dve", lambda e: e.tensor_scalar(out=xib[:], in0=cst[:, 4, :], scalar1=-1.0, scalar2=129.0, op0=ALU.mult, op1=ALU.add), reads=["rcst"], writes=["rxib"])
            for dr in range(2):
                for h in range(4):
                    c = dr * 4 + h
                    S.op("act", lambda e, c=c, dr=dr: e.activation(out=DT[:, c, :], in_=cst[:, 2 * dr, :], func=AF.Exp, scale=lg[:, c:c + 1]),
                         reads=["rlg", "rcst"], writes=["rDT"])
                    S.op("dve", lambda e, c=c, dr=dr: e.tensor_tensor(out=DT[:, c, :], in0=DT[:, c, :], in1=cst[:, 2 * dr + 1, :], op=ALU.mult),
                         reads=["rDT", "rcst"], writes=["rDT"])
                    src = cst[:, 4, :] if dr == 0 else xib[:]
                    S.op("act", lambda e, c=c, src=src: e.activation(out=XI[:, c, :], in_=src, func=AF.Exp, scale=lg[:, c:c + 1]),
                         reads=["rlg", "rcst", "rxib"], writes=["rXI"])
                    S.op("act", lambda e, c=c, dr=dr: e.activation(out=zc[:, c:c + 1], in_=ccol[:, dr:dr + 1], func=AF.Exp, scale=lg[:, c:c + 1]),
                         reads=["rlg", "rccol"], writes=["rzc"])
            S.op("act", lambda e: e.activation(out=g128[:], in_=lg[:], func=AF.Exp, scale=128.0), reads=["rlg"], writes=["rg128"])
            oall = C.sb("roall", [128, 16, 512], F32)
            Sf = [C.sb(f"rSf{c}", [128, 128], F32) for c in range(8)]
            Sb = [[C.sb(f"rSb{c}_{i}", [128, 128], BF16) for i in range(2)] for c in range(8)]
            ap_ = [C.ps(f"rap{i}", [128, 128], F32) for i in range(3)]
            op_ = [C.ps(f"rop{i}", [128, 128], F32) for i in range(2)]
            up_ = [C.ps(f"rup{i}", [128, 128], F32) for i in range(2)]
            At = [C.sb(f"rAt{i}", [128, 128], BF16) for i in range(8)]
            qx = [C.sb(f"rqx{i}", [128, 128], BF16) for i in range(8)]
            kz = [C.sb(f"rkz{i}", [128, 128], BF16) for i in range(8)]
            for c in range(8):
                S.op("pool", lambda e, c=c: e.memset(Sf[c][:], 0.0), writes=[f"rSf{c}"])
                S.op("pool", lambda e, c=c: e.memset(Sb[c][0][:], 0.0), writes=[f"rSb{c}_0"])
            orders = {0: [16, 17] + list(range(16)), 1: [17, 16] + list(range(15, -1, -1))}
            ia = 0
            io = 0
            for step in range(18):
                chains = [(dr, h) for dr in range(2) for h in range(4)]
                for (dr, h) in chains:
                    ch = orders[dr][step]
                    c = dr * 4 + h
                    if ch < 16:
                        a3 = ia % 3
                        ia += 1
                        S.op("pe", lambda e, a3=a3, h=h, ch=ch: e.matmul(ap_[a3][:], lhsT=kT[:, h, ch * 128:(ch + 1) * 128], rhs=qT[:, h, ch * 128:(ch + 1) * 128],
                                                                   start=True, stop=True), reads=["rkT", "rqT"], writes=[f"rap{a3}"])
                        S.op("dve", lambda e, a3=a3, c=c: e.tensor_tensor(out=At[c][:], in0=ap_[a3][:], in1=DT[:, c, :], op=ALU.mult),
                             reads=[f"rap{a3}", "rDT"], writes=[f"rAt{c}"])
                        S.op("pool", lambda e, c=c, h=h, ch=ch: e.tensor_tensor(out=qx[c][:], in0=qT[:, h, ch * 128:(ch + 1) * 128], in1=XI[:, c, :], op=ALU.mult),
                             reads=["rqT", "rXI"], writes=[f"rqx{c}"])
                    if step < 17:
                        S.op("pool", lambda e, ch=ch, h=h, c=c: e.tensor_scalar(out=kz[c][:], in0=ktok[:, ch, h, :], scalar1=zc[:, c:c + 1], scalar2=None, op0=ALU.mult),
                             reads=[("rktok", ch), "rzc"], writes=[f"rkz{c}"])
                for (dr, h) in chains:
                    ch = orders[dr][step]
                    c = dr * 4 + h
                    cur, nxt = Sb[c][step % 2], Sb[c][(step + 1) % 2]
                    kcur, knxt = f"rSb{c}_{step % 2}", f"rSb{c}_{(step + 1) % 2}"
                    i2 = io % 2
                    io += 1
                    if ch < 16:
                        S.op("pe", lambda e, i2=i2, c=c, h=h, ch=ch: e.matmul(op_[i2][:], lhsT=At[c][:], rhs=vv[:, ch, h * 128:(h + 1) * 128], start=True, stop=False),
                             reads=[f"rAt{c}", "rv"], writes=[f"rop{i2}"])
                        S.op("pe", lambda e, i2=i2, c=c, cur=cur: e.matmul(op_[i2][:], lhsT=qx[c][:], rhs=cur[:], start=False, stop=True),
                             reads=[f"rqx{c}", kcur], writes=[f"rop{i2}"])
                        first = (ch + 2 < 17 - ch) if dr == 0 else (17 - ch < ch + 2)
                        if first:
                            S.op("act", lambda e, i2=i2, h=h, ch=ch: e.copy(out=oall[:, ch, h * 128:(h + 1) * 128], in_=op_[i2][:]),
                                 reads=[f"rop{i2}"], writes=[("roall", ch, h)])
                        else:
                            S.op("dve", lambda e, i2=i2, h=h, ch=ch: e.tensor_tensor(out=oall[:, ch, h * 128:(h + 1) * 128], in0=op_[i2][:],
                                                                                   in1=oall[:, ch, h * 128:(h + 1) * 128], op=ALU.add),
                                 reads=[f"rop{i2}", ("roall", ch, h)], writes=[("roall", ch, h)])
                    if step < 17:
                        S.op("pe", lambda e, i2=i2, c=c, ch=ch, h=h: e.matmul(up_[i2][:], lhsT=kz[c][:], rhs=vv[:, ch, h * 128:(h + 1) * 128], start=True, stop=True),
                             reads=[f"rkz{c}", "rv"], writes=[f"rup{i2}"])
                        S.op("dve", lambda e, i2=i2, c=c: e.scalar_tensor_tensor(out=Sf[c][:], in0=Sf[c][:], scalar=g128[:, c:c + 1], in1=up_[i2][:], op0=ALU.mult, op1=ALU.add),
                             reads=[f"rup{i2}", f"rSf{c}", "rg128"], writes=[f"rSf{c}"])
                        S.op("act", lambda e, c=c, nxt=nxt: e.copy(out=nxt[:], in_=Sf[c][:]), reads=[f"rSf{c}"], writes=[knxt])
            gn = C.sb("rgn", [128, 512], F32)
            S.dma("sp", gn[:], G["ret_gn"][0:1, :].to_broadcast([128, 512]), writes=["rgn"])
            st2 = [C.sb(f"rst{i}", [128, 8], F32) for i in range(2)]
            xc2 = [C.sb(f"rxc{i}", [128, 512], F32) for i in range(2)]
            sq2 = [C.sb(f"rsq{i}", [128, 512], F32) for i in range(2)]
            yb = [C.sb(f"ryb{i}", [128, 512], BF16) for i in range(2)]
            yT = [C.sb(f"ryT{i}", [128, 4, 128], BF16) for i in range(2)]
            tpo = C.ps("rtpo", [128, 4, 128], BF16)

            def gn_chain(ch):
                p = ch % 2
                st, xc, sq = st2[p], xc2[p], sq2[p]
                kst, kxc, ksq = f"rst{p}", f"rxc{p}", f"rsq{p}"
                ok = [("roall", ch, h) for h in range(4)]
                o3 = oall[:, ch, :].rearrange("p (h d) -> p h d", d=128)
                y_, ky = yb[p], f"ryb{p}"
                t_, kt_ = yT[p], f"ryT{p}"
                S.op("dve", lambda e: e.reduce_sum(out=st[:, 0:4], in_=o3, axis=AX.X), reads=ok, writes=[kst])
                yield
                S.op("dve", lambda e: e.tensor_scalar(out=st[:, 0:4], in0=st[:, 0:4], scalar1=-1.0 / 128, scalar2=None, op0=ALU.mult), reads=[kst], writes=[kst])
                yield
                S.op("dve", lambda e: e.tensor_tensor(out=xc[:].rearrange("p (h d) -> p h d", d=128), in0=o3,
                                                      in1=st[:, 0:4, None].to_broadcast([128, 4, 128]), op=ALU.add), reads=ok + [kst], writes=[kxc])
                yield
                S.op("act", lambda e: e.activation(out=sq[:], in_=xc[:], func=AF.Square), reads=[kxc], writes=[ksq])
                yield
                S.op("dve", lambda e: e.reduce_sum(out=st[:, 4:8], in_=sq[:].rearrange("p (h d) -> p h d", d=128), axis=AX.X), reads=[ksq], writes=[kst])
                yield
                S.op("act", lambda e: e.activation(out=st[:, 4:8], in_=st[:, 4:8], func=AF.Sqrt, scale=1.0 / 128, bias=EPS), reads=[kst], writes=[kst])
                yield
                S.op("dve", lambda e: e.reciprocal(out=st[:, 4:8], in_=st[:, 4:8]), reads=[kst], writes=[kst])
                yield
                S.op("dve", lambda e: e.tensor_tensor(out=xc[:].rearrange("p (h d) -> p h d", d=128), in0=xc[:].rearrange("p (h d) -> p h d", d=128),
                                                      in1=st[:, 4:8, None].to_broadcast([128, 4, 128]), op=ALU.mult), reads=[kxc, kst], writes=[kxc])
                yield
                S.op("pool", lambda e: e.tensor_tensor(out=xc[:], in0=xc[:], in1=gn[:], op=ALU.mult), reads=[kxc, "rgn"], writes=[kxc])
                yield
                S.op("pool", lambda e: e.tensor_tensor(out=y_[:], in0=xc[:], in1=sg[:, ch, :], op=ALU.mult), reads=[kxc, "rsg"], writes=[ky])
                yield
                for j in range(4):
                    S.op("pe", lambda e, j=j: e.transpose(out=tpo[:, j, :], in_=y_[:, j * 128:(j + 1) * 128], identity=G["ident_b"][:]),
                         reads=[ky, "ident_b"], writes=["rtpo"])
                S.op("act", lambda e: e.copy(out=t_[:], in_=tpo[:]), reads=["rtpo"], writes=[kt_])
                S.dma("sp", G["mix_d"][0:512, ch * 128:(ch + 1) * 128].rearrange("(j p) t -> p j t", p=128), t_[:], reads=[kt_], writes=[("mix_d", "r", ch)])
                yield

            for c2 in range(0, 16, 2):
                run_rr([gn_chain(c2), gn_chain(c2 + 1)])


def phase_gqa(C, G):
    S = C.S
    aT = G["aT"]
    with C.phase():
        qT = C.sb("gqT", [128, 4, SEQ], BF16)
        kT = C.sb("gkT", [128, 2, T], BF16)
        vx = C.sb("gvx", [128, NT, 2, 65], BF16)
        gq = C.sb("ggq", [128, 64], F32)
        gk = C.sb("ggk", [128, 64], F32)
        cs = C.sb("gropec", [128, 16, 32], F32)
        sn = C.sb("gropes", [128, 16, 32], F32)
        sk = C.sb("gsink", [128, 8], F32)
        msk = C.sb("gmask", [128, 2, 128], BF16)
        S.dma("sp", gq[:], G["gq_q_norm"][0:1, :].to_broadcast([128, 64]), writes=["ggq"])
        S.dma("sp", gk[:], G["gq_k_norm"][0:1, :].to_broadcast([128, 64]), writes=["ggk"])
        S.dma("sp", sk[:], G["gq_sink"][0:1, :].to_broadcast([128, 8]), writes=["gsink"])
        S.dma("sp", cs[:], G["c_cos"].rearrange("(t p) f -> p t f", p=128), writes=["rope"])
        S.dma("sp", sn[:], G["c_sin"].rearrange("(t p) f -> p t f", p=128), writes=["rope"])
        S.dma("sp", msk[:], G["c_gmask"].rearrange("a p f -> p a f"), writes=["gmask"])
        S.op("dve", lambda e: e.tensor_scalar(out=gq[:], in0=gq[:], scalar1=0.125, scalar2=None, op0=ALU.mult), reads=["ggq"], writes=["ggq"])
        S.op("act", lambda e: e.activation(out=sk[:], in_=sk[:], func=AF.Exp), reads=["gsink"], writes=["gsink"])
        S.op("pool", lambda e: e.memset(vx[:, :, :, 64:65], 1.0), writes=["gvx1"])
        with C.phase():
            w = C.sb("wgqa", [128, 8, 896], BF16)
            base = 2048
            S.dma("pool", w[:, :, 0:512], G["o_w_in"][0, :, base:base + 512].rearrange("(k p) n -> p k n", p=128), writes=["wgqa0"])
            for i, kv in enumerate((0, 0, 1, 1)):
                S.dma("pool", w[:, :, 512 + i * 64:512 + (i + 1) * 64],
                      G["o_w_in"][0, :, base + 512 + kv * 64:base + 512 + (kv + 1) * 64].rearrange("(k p) n -> p k n", p=128), writes=[f"wgqa1{i}"])
            S.dma("pool", w[:, :, 768:896], G["o_w_in"][0, :, base + 640:base + 768].rearrange("(k p) n -> p k n", p=128), writes=["wgqa2"])
            wk_ = ["wgqa0", "wgqa10", "wgqa11", "wgqa12", "wgqa13", "wgqa2"]
            pq = [C.ps(f"gpq{i}", [128, 512], F32) for i in range(2)]
            pk = [C.ps(f"gpk{i}", [128, 512], F32) for i in range(2)]
            tps_ = [C.ps(f"gqktp{i}", [128, 4, 128], BF16) for i in range(2)]
            Wq = [qk_work(C, f"gwq{i}", tps_[i]) for i in range(2)]
            Wk = [qk_work(C, f"gwk{i}", tps_[i]) for i in range(2)]
            for i in range(2):
                Wq[i]["tpkey"] = Wk[i]["tpkey"] = f"gqktp{i}"
            for t2 in range(0, NT, 2):
                gens = []
                for tt in (t2, t2 + 1):
                    lat = tt < 16
                    q_, k_ = pq[tt % 2], pk[tt % 2]
                    if lat:
                        for k in range(8):
                            S.op("pe", lambda e, q_=q_, k=k, tt=tt: e.matmul(q_[:], lhsT=aT[:, k, tt * 128:(tt + 1) * 128], rhs=w[:, k, 0:512], start=(k == 0), stop=(k == 7)),
                                 reads=wk_, writes=[f"gpq{tt % 2}"])
                    for k in range(8):
                        S.op("pe", lambda e, k_=k_, k=k, tt=tt: e.matmul(k_[:, 0:384], lhsT=aT[:, k, tt * 128:(tt + 1) * 128], rhs=w[:, k, 512:896], start=(k == 0), stop=(k == 7)),
                             reads=wk_, writes=[f"gpk{tt % 2}"])
                    S.op("act", lambda e, k_=k_, tt=tt: e.copy(out=vx[:, tt, :, 0:64], in_=k_[:, 256:384].rearrange("p (h d) -> p h d", d=64)), reads=[f"gpk{tt % 2}"], writes=["gvx"])
                    if lat:
                        gens.append(qk_prep(C, G, q_[:], f"gpq{tt % 2}", gq, "ggq", cs, sn, tt, True, qT, "gqT", 8, Wq[tt % 2], f"gwq{tt % 2}"))
                    gens.append(qk_prep(C, G, k_[:, 0:256], f"gpk{tt % 2}", gk, "ggk", cs, sn, tt, lat, kT, "gkT", 4, Wk[tt % 2], f"gwk{tt % 2}"))
                run_rr(gens)
        with C.phase():
            sp = [C.ps(f"gsp{i}", [128, 512], F32) for i in range(4)]
            oacc = [C.ps(f"goacc{i}", [128, 4, 65], F32) for i in range(2)]
            pt = [C.sb(f"gpt{i}", [128, 512], BF16) for i in range(3)]
            den = [C.sb(f"gden{i}", [128, 4], F32) for i in range(2)]
            sks = C.sb("gsks", [128, 2, 4], F32)
            for kv_ in range(2):
                for sl_ in range(4):
                    hd_ = kv_ * 4 + 2 * (sl_ % 2) + sl_ // 2
                    S.op("dve", lambda e, kv_=kv_, sl_=sl_, hd_=hd_: e.tensor_copy(out=sks[:, kv_, sl_:sl_ + 1], in_=sk[:, hd_:hd_ + 1]), reads=["gsink"], writes=["gsks"])
            on = [C.sb(f"gon{i}", [128, 512], F32) for i in range(2)]
            oT = [C.sb(f"goT{i}", [128, 4, 128], BF16) for i in range(2)]
            its = []
            for qt in range(16):
                for kv in range(2):
                    tiles = [(16, None), (17, None)]
                    if qt > 0:
                        tiles.append((qt - 1, 0))
                    tiles.append((qt, None))
                    if qt < 15:
                        tiles.append((qt + 1, 1))
                    for ti, (kt, mk) in enumerate(tiles):
                        its.append(dict(qt=qt, kv=kv, kt=kt, mk=mk, first=(ti == 0), last=(ti == len(tiles) - 1)))

            def emit_s(i):
                d_ = its[i]
                qt, kv, kt, mk = d_["qt"], d_["kv"], d_["kt"], d_["mk"]
                p_, kp_ = pt[i % 3], f"gpt{i % 3}"
                for half in range(2):
                    sb_i = (i % 2) * 2 + half
                    pb = slice(half * 64, (half + 1) * 64)
                    S.op("pe", lambda e, pb=pb, sb_i=sb_i, kv=kv, kt=kt, qt=qt: e.matmul(
                        sp[sb_i][:, 0:256], lhsT=kT[pb, kv, kt * 128:(kt + 1) * 128],
                        rhs=qT[pb, kv * 2:kv * 2 + 2, qt * 128:(qt + 1) * 128], start=True, stop=True),
                        reads=["gqT", "gkT"], writes=[f"gsp{sb_i}"])
                    S.op("act", lambda e, p_=p_, half=half, sb_i=sb_i: e.activation(out=p_[:, half * 256:(half + 1) * 256], in_=sp[sb_i][:, 0:256], func=AF.Exp),
                         reads=[f"gsp{sb_i}"], writes=[kp_])
                if mk is not None:
                    S.op("pool", lambda e, p_=p_, mk=mk: e.tensor_tensor(out=p_[:].rearrange("p (a b) -> p a b", b=128), in0=p_[:].rearrange("p (a b) -> p a b", b=128),
                                                                         in1=msk[:, mk, None, :].to_broadcast([128, 4, 128]), op=ALU.mult),
                         reads=[kp_, "gmask"], writes=[kp_])

            emit_s(0)
            for i in range(len(its)):
                if i + 1 < len(its):
                    emit_s(i + 1)
                d_ = its[i]
                qt, kv, kt = d_["qt"], d_["kv"], d_["kt"]
                p_, kp_ = pt[i % 3], f"gpt{i % 3}"
                o_n, kon = on[qt % 2], f"gon{qt % 2}"
                ab = (qt * 2 + kv) % 2
                acc, kacc = oacc[ab], f"goacc{ab}"
                for sl in range(4):
                    S.op("pe", lambda e, sl=sl, p_=p_, kt=kt, kv=kv, acc=acc, st_=(d_["first"] and sl == 0), last=d_["last"]: e.matmul(
                        acc[:, sl, :], lhsT=p_[:, sl * 128:(sl + 1) * 128], rhs=vx[:, kt, kv, :], start=st_, stop=last, skip_group_check=True),
                        reads=[kp_, "gvx", "gvx1"], writes=[kacc])
                if not d_["last"]:
                    continue
                dn_, kdn = den[ab], f"gden{ab}"
                S.op("dve", lambda e, acc=acc, dn_=dn_, kv=kv: e.tensor_tensor(out=dn_[:], in0=acc[:, :, 64], in1=sks[:, kv, :], op=ALU.add),
                     reads=[kacc, "gsks"], writes=[kdn])
                S.op("dve", lambda e, dn_=dn_: e.reciprocal(out=dn_[:], in_=dn_[:]), reads=[kdn], writes=[kdn])
                S.op("dve", lambda e, acc=acc, dn_=dn_, o_n=o_n, kv=kv: e.tensor_tensor(
                    out=o_n[:, kv * 256:(kv + 1) * 256].rearrange("p (i h d) -> p h i d", i=2, h=2),
                    in0=acc[:, :, 0:64].rearrange("p (h i) d -> p h i d", h=2),
                    in1=dn_[:, :, None].rearrange("p (h i) o -> p h i o", h=2).to_broadcast([128, 2, 2, 64]), op=ALU.mult),
                    reads=[kacc, kdn], writes=[kon])
                if kv == 0:
                    continue
                t_, kt_ = oT[qt % 2], f"goT{qt % 2}"
                tb_i = ((i + 1) % 2) * 2 + 1 if i + 1 < len(its) else 1
                tb_i = (i % 2) * 2
                tps = sp[tb_i]
                for j in range(4):
                    S.op("pe", lambda e, j=j, o_n=o_n, tps=tps: e.transpose(out=tps[:, j * 128:(j + 1) * 128], in_=o_n[:, j * 128:(j + 1) * 128], identity=G["ident_f"][:]),
                         reads=[kon, "ident_f"], writes=[f"gsp{tb_i}"])
                S.op("act", lambda e, t_=t_, tps=tps: e.copy(out=t_[:], in_=tps[:].rearrange("p (a b) -> p a b", b=128)), reads=[f"gsp{tb_i}"], writes=[kt_])
                S.dma("sp", G["mix_d"][512:1024, qt * 128:(qt + 1) * 128].rearrange("(j p) t -> p j t", p=128), t_[:], reads=[kt_], writes=[("mix_d", "g", qt)])
```
